# Optimizing a Trainium2 kernel written in Bass

```python
import math
import jax, jax.numpy as jnp
from jax import lax
import numpy as np

D_MODEL = 1024
BATCH = 8
SEQ = 8192
DEPTH = 2

N_EVEN = (DEPTH + 1) // 2
N_ODD = DEPTH // 2

BLOCK = 128
RMS_EPS = 1e-6
GN_EPS = 1e-5
NEG = -1e30

A_HEADS = 8
A_KV_HEADS = 2
A_HEAD_DIM = 64
WINDOW = 128
ROPE_THETA = 500000.0
ROPE_DIM = A_HEAD_DIM // 4

B_HEADS = 8
B_HEAD_DIM = 64
RET_THETA = 10000.0

C_HEADS = 8
C_NOPE = 64
C_ROPE = 32
C_V = 64
C_Q_LORA = 512
C_KV_LORA = 256
MLA_THETA = 10000.0

D_HEADS = 4
D_HEAD_DIM = 128
D_CONV = 5

N_EXPERTS = 16
EC_CAPACITY_FACTOR = 2
D_FF_EXPERT = 2048

A_Q_W = A_HEADS * A_HEAD_DIM
A_KV_W = A_KV_HEADS * A_HEAD_DIM
B_W = B_HEADS * B_HEAD_DIM
D_W = D_HEADS * D_HEAD_DIM
EVEN_SPLITS = (A_Q_W, A_KV_W, A_KV_W, B_W, B_W, B_W, B_W)
ODD_SPLITS = (C_Q_LORA, C_KV_LORA, C_ROPE, D_W, D_W, D_W, D_W, 4 * D_HEADS)
EVEN_COLS = sum(EVEN_SPLITS)
ODD_COLS = sum(ODD_SPLITS)
EVEN_MIX_W = A_Q_W + B_W
ODD_MIX_W = C_HEADS * C_V + D_W

kernel_name = 'hybrid_bidir_swa_retention_mla_mlstm_ecmoe'


def rms_norm(x, g):
    xf = x.astype(jnp.float32)
    y = xf * lax.rsqrt(jnp.mean(xf * xf, axis=-1, keepdims=True) + RMS_EPS)
    return (y * g.astype(jnp.float32)).astype(x.dtype)


def head_group_norm(y):
    mu = jnp.mean(y, axis=-1, keepdims=True)
    yc = y - mu
    return yc * lax.rsqrt(jnp.mean(yc * yc, axis=-1, keepdims=True) + GN_EPS)


def split_cols(z, sizes):
    return jnp.split(z, [int(c) for c in np.cumsum(sizes)[:-1]], axis=-1)


def rope(x, positions, theta):
    d = x.shape[-1]
    half = d // 2
    inv_freq = theta ** (-jnp.arange(half, dtype=jnp.float32) * 2.0 / d)
    ang = positions.astype(jnp.float32)[:, :, None] * inv_freq
    cos = jnp.cos(ang)[:, :, None, :]
    sin = jnp.sin(ang)[:, :, None, :]
    xf = x.astype(jnp.float32)
    x1, x2 = xf[..., :half], xf[..., half:]
    out = jnp.concatenate([x1 * cos - x2 * sin, x2 * cos + x1 * sin], axis=-1)
    return out.astype(x.dtype)


def partial_rope(x, positions):
    return jnp.concatenate([rope(x[..., :ROPE_DIM], positions, ROPE_THETA), x[..., ROPE_DIM:]], axis=-1)


def window_gqa_sink(q, k, v, sink):
    B, S, Hq, d = q.shape
    Hkv = k.shape[2]
    G = Hq // Hkv
    N = S // BLOCK
    qb = q.reshape(B, N, BLOCK, Hkv, G, d)
    pad = ((0, 0), (BLOCK, BLOCK), (0, 0), (0, 0))
    kp = jnp.pad(k, pad).reshape(B, N + 2, BLOCK, Hkv, d)
    vp = jnp.pad(v, pad).reshape(B, N + 2, BLOCK, Hkv, d)
    kw = jnp.concatenate([kp[:, :-2], kp[:, 1:-1], kp[:, 2:]], axis=2)
    vw = jnp.concatenate([vp[:, :-2], vp[:, 1:-1], vp[:, 2:]], axis=2)
    s = jnp.einsum('bnqhgd,bnkhd->bhgnqk', qb, kw).astype(jnp.float32) * (d ** -0.5)
    qpos = jnp.arange(N)[:, None, None] * BLOCK + jnp.arange(BLOCK)[None, :, None]
    kpos = jnp.arange(N)[:, None, None] * BLOCK - BLOCK + jnp.arange(3 * BLOCK)[None, None, :]
    valid = (jnp.abs(qpos - kpos) <= WINDOW) & (kpos >= 0) & (kpos < S)
    s = jnp.where(valid, s, NEG)
    sink_col = jnp.broadcast_to(sink.astype(jnp.float32).reshape(1, Hkv, G, 1, 1, 1), s.shape[:-1] + (1,))
    p = jax.nn.softmax(jnp.concatenate([s, sink_col], axis=-1), axis=-1)[..., :-1]
    o = jnp.einsum('bhgnqk,bnkhd->bnqhgd', p.astype(v.dtype), vw)
    return o.reshape(B, S, Hq * d)


def retention_scan(q, k, v, log_gamma, strict):
    B, S, H, d = q.shape
    N = S // BLOCK
    L = BLOCK
    qc = q.reshape(B, N, L, H, d)
    kc = k.reshape(B, N, L, H, d)
    vc = v.reshape(B, N, L, H, d)
    idx = jnp.arange(L, dtype=jnp.float32)
    rel = idx[:, None] - idx[None, :]
    mask = (rel > 0) if strict else (rel >= 0)
    decay = jnp.where(mask[None], jnp.exp(jnp.maximum(rel, 0.0)[None] * log_gamma[:, None, None]), 0.0)
    s = jnp.einsum('bnihd,bnjhd->bnhij', qc, kc) * decay
    intra = jnp.einsum('bnhij,bnjhd->bnihd', s, vc)
    zeta = jnp.exp((L - 1 - idx)[:, None] * log_gamma[None, :])
    xi = jnp.exp((idx + 1)[:, None] * log_gamma[None, :])
    U = jnp.einsum('bnjhk,bnjhv->nbhkv', kc * zeta[:, :, None], vc)
    chunk_decay = jnp.exp(L * log_gamma)[:, None, None]

    def step(R, u):
        return chunk_decay * R + u, R

    _, R_prev = lax.scan(step, jnp.zeros((B, H, d, d), jnp.float32), U)
    inter = jnp.einsum('bnihk,nbhkv->bnihv', qc * xi[:, :, None], R_prev)
    return (intra + inter).reshape(B, S, H, d)


def bidirectional_retention(q, k, v, g, positions, decay_logit):
    B, S, _ = q.shape
    shp = (B, S, B_HEADS, B_HEAD_DIM)
    q = rope(q.reshape(shp), positions, RET_THETA).astype(jnp.float32)
    k = rope(k.reshape(shp), positions, RET_THETA).astype(jnp.float32) * (B_HEAD_DIM ** -0.5)
    v = v.reshape(shp).astype(jnp.float32)
    lg = jax.nn.log_sigmoid(decay_logit.astype(jnp.float32))
    y_f = retention_scan(q, k, v, lg[0], strict=False)
    y_b = jnp.flip(retention_scan(jnp.flip(q, 1), jnp.flip(k, 1), jnp.flip(v, 1), lg[1], strict=True), 1)
    y = head_group_norm(y_f + y_b).reshape(B, S, B_W)
    return jax.nn.silu(g.astype(jnp.float32)) * y


def dense_block_attention(q, k, v):
    B, S, H, dq = q.shape
    dv = v.shape[-1]
    N = S // BLOCK
    scale = dq ** -0.5
    qb = q.reshape(B, N, BLOCK, H, dq).transpose(1, 0, 2, 3, 4)

    def one_block(qblk):
        s = jnp.einsum('bqhd,bkhd->bhqk', qblk, k).astype(jnp.float32) * scale
        p = jax.nn.softmax(s, axis=-1)
        return jnp.einsum('bhqk,bkhd->bqhd', p.astype(v.dtype), v)

    o = lax.map(one_block, qb)
    return o.transpose(1, 0, 2, 3, 4).reshape(B, S, H * dv)


def mla_attention(cq, ckv, kr, positions, norm_q, norm_kv, w_uq, w_ukv):
    B, S, _ = cq.shape
    q = (rms_norm(cq, norm_q) @ w_uq).reshape(B, S, C_HEADS, C_NOPE + C_ROPE)
    kv = (rms_norm(ckv, norm_kv) @ w_ukv).reshape(B, S, C_HEADS, C_NOPE + C_V)
    q_rope = rope(q[..., C_NOPE:], positions, MLA_THETA)
    k_rope = rope(kr.reshape(B, S, 1, C_ROPE), positions, MLA_THETA)
    q_full = jnp.concatenate([q[..., :C_NOPE], q_rope], axis=-1)
    k_full = jnp.concatenate([kv[..., :C_NOPE], jnp.broadcast_to(k_rope, (B, S, C_HEADS, C_ROPE))], axis=-1)
    return dense_block_attention(q_full, k_full, kv[..., C_NOPE:])


def mlstm_scan(q, k, v, i_pre, f_pre, strict):
    B, S, H, d = q.shape
    N = S // BLOCK
    L = BLOCK
    qc = q.reshape(B, N, L, H, d)
    kc = k.reshape(B, N, L, H, d)
    vc = v.reshape(B, N, L, H, d)
    ic = i_pre.reshape(B, N, L, H)
    b = jnp.cumsum(jax.nn.log_sigmoid(f_pre).reshape(B, N, L, H), axis=2)
    g = b[:, :, -1]
    a = g[:, :, None] - b + ic
    a_max = jnp.max(a, axis=2)
    w = jnp.exp(a - a_max[:, :, None])
    U = jnp.einsum('bnjh,bnjhk,bnjhv->nbhkv', w, kc, vc)
    u = jnp.einsum('bnjh,bnjhk->nbhk', w, kc)

    def step(carry, xs):
        C, n, m = carry
        U_c, u_c, g_c, amax_c = xs
        m_new = jnp.maximum(g_c + m, amax_c)
        sp = jnp.exp(g_c + m - m_new)
        sc = jnp.exp(amax_c - m_new)
        C_new = sp[..., None, None] * C + sc[..., None, None] * U_c
        n_new = sp[..., None] * n + sc[..., None] * u_c
        return (C_new, n_new, m_new), (C, n, m)

    init = (jnp.zeros((B, H, d, d), jnp.float32), jnp.zeros((B, H, d), jnp.float32), jnp.zeros((B, H), jnp.float32))
    _, (C_prev, n_prev, m_prev) = lax.scan(step, init, (U, u, g.transpose(1, 0, 2), a_max.transpose(1, 0, 2)))
    m_prev = m_prev.transpose(1, 0, 2)
    bt = b.transpose(0, 1, 3, 2)
    it = ic.transpose(0, 1, 3, 2)
    logD = bt[..., :, None] - bt[..., None, :] + it[..., None, :]
    ti = jnp.arange(L)
    mask = (ti[:, None] > ti[None, :]) if strict else (ti[:, None] >= ti[None, :])
    logD = jnp.where(mask, logD, NEG)
    log_inter = bt + m_prev[..., None]
    m_t = jnp.maximum(jnp.max(logD, axis=-1), log_inter)
    Dw = jnp.exp(logD - m_t[..., None])
    s = jnp.einsum('bnthd,bnjhd->bnhtj', qc, kc) * Dw
    inter_w = jnp.exp(log_inter - m_t)
    inter_w_t = inter_w.transpose(0, 1, 3, 2)[..., None]
    num = jnp.einsum('bnhtj,bnjhd->bnthd', s, vc) + jnp.einsum('bnthk,nbhkv->bnthv', qc, C_prev) * inter_w_t
    den = jnp.sum(s, axis=-1) + inter_w * jnp.einsum('bnthk,nbhk->bnht', qc, n_prev)
    denom = jnp.maximum(jnp.abs(den), jnp.exp(-m_t)).transpose(0, 1, 3, 2)[..., None]
    return (num / denom).reshape(B, S, H, d)


def centred_depthwise_conv(x, w):
    W, C = w.shape
    return lax.conv_general_dilated(x, w[:, None, :].astype(x.dtype), window_strides=(1,),
                                    padding=((W // 2, W // 2),), dimension_numbers=('NWC', 'WIO', 'NWC'),
                                    feature_group_count=C)


def bidirectional_mlstm(q, k, v, o, gates, conv_w, gate_bias):
    B, S, _ = q.shape
    qk = jax.nn.silu(centred_depthwise_conv(jnp.concatenate([q, k], axis=-1), conv_w))
    shp = (B, S, D_HEADS, D_HEAD_DIM)
    qh = qk[..., :D_W].reshape(shp).astype(jnp.float32)
    kh = qk[..., D_W:].reshape(shp).astype(jnp.float32) * (D_HEAD_DIM ** -0.5)
    vh = v.reshape(shp).astype(jnp.float32)
    gt = gates.reshape(B, S, 4, D_HEADS).astype(jnp.float32) + gate_bias.astype(jnp.float32)
    h_f = mlstm_scan(qh, kh, vh, gt[:, :, 0], gt[:, :, 1], strict=False)
    h_b = jnp.flip(mlstm_scan(jnp.flip(qh, 1), jnp.flip(kh, 1), jnp.flip(vh, 1),
                              jnp.flip(gt[:, :, 2], 1), jnp.flip(gt[:, :, 3], 1), strict=True), 1)
    return jax.nn.sigmoid(o.astype(jnp.float32)) * (h_f + h_b).reshape(B, S, D_W)


def even_mixer(h, positions, w_in, w_out, sink, decay_logit):
    B, S, _ = h.shape
    aq, ak, av, bq, bk, bv, bg = split_cols(h @ w_in, EVEN_SPLITS)
    aq = partial_rope(aq.reshape(B, S, A_HEADS, A_HEAD_DIM), positions)
    ak = partial_rope(ak.reshape(B, S, A_KV_HEADS, A_HEAD_DIM), positions)
    av = av.reshape(B, S, A_KV_HEADS, A_HEAD_DIM)
    o_a = window_gqa_sink(aq, ak, av, sink)
    o_b = bidirectional_retention(bq, bk, bv, bg, positions, decay_logit)
    return jnp.concatenate([o_a.astype(h.dtype), o_b.astype(h.dtype)], axis=-1) @ w_out


def odd_mixer(h, positions, w_in, w_out, norm_q, norm_kv, w_uq, w_ukv, conv_w, gate_bias):
    cq, ckv, kr, dq, dk, dv, do, dg = split_cols(h @ w_in, ODD_SPLITS)
    o_c = mla_attention(cq, ckv, kr, positions, norm_q, norm_kv, w_uq, w_ukv)
    o_d = bidirectional_mlstm(dq, dk, dv, do, dg, conv_w, gate_bias)
    return jnp.concatenate([o_c.astype(h.dtype), o_d.astype(h.dtype)], axis=-1) @ w_out


def expert_choice_ffn(h, w_router, w_gate, w_up, w_down):
    B, S, D = h.shape
    cap = EC_CAPACITY_FACTOR * S // N_EXPERTS
    aff = jax.nn.softmax(jnp.einsum('bsd,de->bse', h, w_router).astype(jnp.float32), axis=-1)
    gate, idx = lax.top_k(aff.transpose(0, 2, 1), cap)
    bidx = jnp.arange(B)[:, None, None]
    xin = h[bidx, idx]
    hid = jax.nn.silu(jnp.einsum('becd,edf->becf', xin, w_gate)) * jnp.einsum('becd,edf->becf', xin, w_up)
    y = jnp.einsum('becf,efd->becd', hid, w_down) * gate[..., None].astype(h.dtype)
    return jnp.zeros((B, S, D), y.dtype).at[bidx, idx].add(y)


def setup_inputs(seed: int = 0) -> dict:
    key = jax.random.key(seed)
    ks = jax.random.split(key, 24)
    f32 = jnp.float32

    def nrm(k, shape, scale):
        return jax.random.normal(k, shape, f32) * scale

    x = jax.random.normal(ks[0], (BATCH, SEQ, D_MODEL), f32)
    positions = jnp.broadcast_to(jnp.arange(SEQ, dtype=jnp.int32), (BATCH, SEQ))
    norm_mix = 1.0 + nrm(ks[1], (DEPTH, D_MODEL), 0.05)
    norm_ffn = 1.0 + nrm(ks[2], (DEPTH, D_MODEL), 0.05)
    norm_final = 1.0 + nrm(ks[3], (D_MODEL,), 0.05)
    ev_w_in = nrm(ks[4], (N_EVEN, D_MODEL, EVEN_COLS), D_MODEL ** -0.5)
    ev_w_out = nrm(ks[5], (N_EVEN, EVEN_MIX_W, D_MODEL), EVEN_MIX_W ** -0.5)
    attn_sink = nrm(ks[6], (N_EVEN, A_HEADS), 0.5)
    gam = 1.0 - 2.0 ** (-5.0 - jnp.arange(B_HEADS, dtype=f32))
    base_logit = jnp.log(gam) - jnp.log1p(-gam)
    ret_decay_logit = base_logit[None, None, :] + nrm(ks[7], (N_EVEN, 2, B_HEADS), 0.1)
    od_w_in = nrm(ks[8], (N_ODD, D_MODEL, ODD_COLS), D_MODEL ** -0.5)
    od_w_out = nrm(ks[9], (N_ODD, ODD_MIX_W, D_MODEL), ODD_MIX_W ** -0.5)
    mla_norm_q = 1.0 + nrm(ks[10], (N_ODD, C_Q_LORA), 0.05)
    mla_norm_kv = 1.0 + nrm(ks[11], (N_ODD, C_KV_LORA), 0.05)
    mla_w_uq = nrm(ks[12], (N_ODD, C_Q_LORA, C_HEADS * (C_NOPE + C_ROPE)), C_Q_LORA ** -0.5)
    mla_w_ukv = nrm(ks[13], (N_ODD, C_KV_LORA, C_HEADS * (C_NOPE + C_V)), C_KV_LORA ** -0.5)
    mlstm_conv = nrm(ks[14], (N_ODD, D_CONV, 2 * D_W), D_CONV ** -0.5)
    fb = jnp.linspace(3.0, 6.0, D_HEADS, dtype=f32)
    zb = jnp.zeros((D_HEADS,), f32)
    mlstm_gate_bias = jnp.stack([zb, fb, zb, fb])[None] + nrm(ks[15], (N_ODD, 4, D_HEADS), 0.1)
    moe_router = nrm(ks[16], (DEPTH, D_MODEL, N_EXPERTS), D_MODEL ** -0.5)
    moe_w_gate = nrm(ks[17], (DEPTH, N_EXPERTS, D_MODEL, D_FF_EXPERT), D_MODEL ** -0.5)
    moe_w_up = nrm(ks[18], (DEPTH, N_EXPERTS, D_MODEL, D_FF_EXPERT), D_MODEL ** -0.5)
    moe_w_down = nrm(ks[19], (DEPTH, N_EXPERTS, D_FF_EXPERT, D_MODEL), D_FF_EXPERT ** -0.5)
    return {'x': x, 'positions': positions, 'norm_mix': norm_mix, 'norm_ffn': norm_ffn,
            'norm_final': norm_final, 'ev_w_in': ev_w_in, 'ev_w_out': ev_w_out, 'attn_sink': attn_sink,
            'ret_decay_logit': ret_decay_logit, 'od_w_in': od_w_in, 'od_w_out': od_w_out,
            'mla_norm_q': mla_norm_q, 'mla_norm_kv': mla_norm_kv, 'mla_w_uq': mla_w_uq,
            'mla_w_ukv': mla_w_ukv, 'mlstm_conv': mlstm_conv, 'mlstm_gate_bias': mlstm_gate_bias,
            'moe_router': moe_router, 'moe_w_gate': moe_w_gate, 'moe_w_up': moe_w_up,
            'moe_w_down': moe_w_down}


def reference(x, positions, norm_mix, norm_ffn, norm_final, ev_w_in, ev_w_out, attn_sink,
              ret_decay_logit, od_w_in, od_w_out, mla_norm_q, mla_norm_kv, mla_w_uq, mla_w_ukv,
              mlstm_conv, mlstm_gate_bias, moe_router, moe_w_gate, moe_w_up, moe_w_down):
    for l in range(DEPTH):
        j = l // 2
        h = rms_norm(x, norm_mix[l])
        if l % 2 == 0:
            mix = even_mixer(h, positions, ev_w_in[j], ev_w_out[j], attn_sink[j], ret_decay_logit[j])
        else:
            mix = odd_mixer(h, positions, od_w_in[j], od_w_out[j], mla_norm_q[j], mla_norm_kv[j],
                            mla_w_uq[j], mla_w_ukv[j], mlstm_conv[j], mlstm_gate_bias[j])
        x = x + mix.astype(x.dtype)
        ffn = expert_choice_ffn(rms_norm(x, norm_ffn[l]), moe_router[l], moe_w_gate[l], moe_w_up[l], moe_w_down[l])
        x = x + ffn.astype(x.dtype)
    return rms_norm(x, norm_final)
```

```python
import numpy as np
from contextlib import ExitStack
import concourse.bass as bass
import concourse.mybir as mybir

F32 = mybir.dt.float32
BF16 = mybir.dt.bfloat16
I32 = mybir.dt.int32
AF = mybir.ActivationFunctionType
ALU = mybir.AluOpType
AX = mybir.AxisListType


class Buf:
    __slots__ = ("t", "w", "r", "dsem", "name")

    def __init__(self, t, name=""):
        self.t = t
        self.w = None
        self.r = []
        self.dsem = None
        self.name = name

    def __getitem__(self, k):
        return self.t[k]


class Eng:
    def __init__(self, name, eng, sem, inorder=False):
        self.name, self.eng, self.sem = name, eng, sem
        self.count = 0
        self.seen = {}
        self.ops = []
        self.inorder = inorder


class Sched:
    def __init__(self, nc):
        self.nc = nc
        self.es = ExitStack()
        self.E = {}
        for name, eng, ino in (("pe", nc.tensor, True), ("act", nc.scalar, False),
                               ("dve", nc.vector, False), ("pool", nc.gpsimd, False),
                               ("sp", nc.sync, False)):
            sem = self.es.enter_context(nc.semaphore("e_" + name))
            self.E[name] = Eng(name, eng, sem, ino)
        self.dsems = []
        self.free_ds = {"hw": [], "sw": []}
        self.stage_es = None
        self.stage_ds = []
        self.nbuf = 0

    def _get_dsem(self, kind):
        if self.free_ds[kind]:
            i = self.free_ds[kind].pop()
        else:
            sem = self.es.enter_context(self.nc.semaphore("d%d" % len(self.dsems)))
            self.dsems.append([sem, 0])
            i = len(self.dsems) - 1
        self.stage_ds.append((kind, i))
        return i

    def begin(self):
        self.stage_es = ExitStack()
        self.stage_ds = []

    def sb(self, shape, dt, name=None):
        self.nbuf += 1
        name = (name or "b") + "_%d" % self.nbuf
        t = self.stage_es.enter_context(self.nc.sbuf_tensor(name, list(shape), dt))
        return Buf(t, name)

    def ps(self, shape, dt=F32, name=None):
        self.nbuf += 1
        name = (name or "p") + "_%d" % self.nbuf
        t = self.stage_es.enter_context(self.nc.psum_tensor(name, list(shape), dt))
        return Buf(t, name)

    def end(self):
        self.barrier()
        with self.nc.Block() as block:
            for name, sect in (("pe", block.tensor), ("act", block.scalar), ("dve", block.vector),
                               ("pool", block.gpsimd), ("sp", block.sync)):
                E = self.E[name]
                ops = E.ops
                E.ops = []

                def body(eng, ops=ops):
                    for waits, fn, inc in ops:
                        for (sem, val) in waits:
                            eng.wait_ge(sem, val)
                        if fn is not None:
                            ins = fn(eng)
                            if inc is not None:
                                ins.then_inc(inc[0], inc[1])
                sect(body)
        for kind, i in self.stage_ds:
            self.free_ds[kind].append(i)
        self.stage_ds = []
        self.stage_es.close()
        self.stage_es = None

    def barrier(self):
        toks = [(E.sem, E.count) for E in self.E.values() if E.count > 0]
        toks += [(s, v) for (s, v) in self.dsems if v > 0]
        for E in self.E.values():
            w = self._filter(E, toks, barrier=True)
            if w:
                E.ops.append((w, None, None))

    def _filter(self, E, toks, barrier=False):
        best = {}
        for (sem, val) in toks:
            if sem is E.sem and (E.inorder or barrier):
                continue
            k = id(sem)
            if E.seen.get(k, 0) >= val:
                continue
            if k not in best or best[k][1] < val:
                best[k] = (sem, val)
        out = []
        for k, (sem, val) in best.items():
            E.seen[k] = val
            out.append((sem, val))
        return out

    def _deps(self, R, W, skip_sem=None):
        toks = []
        for b in R:
            if b.w is not None:
                toks.append(b.w)
        for b in W:
            if b.w is not None and not (skip_sem is not None and b.w[0] is skip_sem):
                toks.append(b.w)
            toks.extend(b.r)
        return toks

    def op(self, ename, fn, R=(), W=()):
        E = self.E[ename]
        waits = self._filter(E, self._deps(R, W))
        E.count += 1
        tok = (E.sem, E.count)
        E.ops.append((waits, fn, (E.sem, 1)))
        for b in R:
            b.r.append(tok)
        for b in W:
            b.w = tok
            b.r = []
        return tok

    def dma(self, qname, out, in_, R=(), W=(), sem_buf=None, fn=None, extra=(), **kw):
        E = self.E[qname]
        sb = sem_buf if sem_buf is not None else (W[0] if W else R[0])
        kind = "sw" if qname == "pool" else "hw"
        if sb.dsem is None:
            sb.dsem = {}
        if kind not in sb.dsem:
            sb.dsem[kind] = self._get_dsem(kind)
        ds = self.dsems[sb.dsem[kind]]
        waits = self._filter(E, self._deps(R, W, skip_sem=ds[0]) + list(extra))
        ds[1] += 16
        tok = (ds[0], ds[1])
        if fn is None:
            def fn(eng, out=out, in_=in_, kw=kw):
                return eng.dma_start(out=out, in_=in_, **kw)
        E.ops.append((waits, fn, (ds[0], 16)))
        for b in R:
            b.r.append(tok)
        for b in W:
            b.w = tok
            b.r = []
        return tok

    def reg(self, ename, value):
        holder = {}

        def fn(eng):
            holder["r"] = eng.alloc_register("rg%d" % id(holder))
            return eng.reg_mov(holder["r"], value)
        self.E[ename].ops.append(([], fn, None))
        return holder

    def mm(self, out_b, out_ap, lhsT_b, lhsT, rhs_b, rhs, start, stop):
        self.op("pe", lambda e: e.matmul(out_ap, lhsT, rhs, start=start, stop=stop),
                R=[lhsT_b, rhs_b], W=[out_b])

    def tr(self, out_b, out_ap, in_b, in_ap, ident_b, ident_ap):
        self.op("pe", lambda e: e.transpose(out_ap, in_ap, ident_ap), R=[in_b, ident_b], W=[out_b])

    def act(self, out_b, out_ap, in_b, in_ap, func, R=(), eng="act", **kw):
        self.op(eng, lambda e: e.activation(out=out_ap, in_=in_ap, func=func, **kw),
                R=[in_b] + list(R), W=[out_b] + ([kw["accum_b"]] if "accum_b" in kw else []))

    def tt(self, eng, out_b, out_ap, a_b, a_ap, b_b, b_ap, op):
        self.op(eng, lambda e: e.tensor_tensor(out=out_ap, in0=a_ap, in1=b_ap, op=op),
                R=[a_b, b_b], W=[out_b])

    def ts(self, eng, out_b, out_ap, a_b, a_ap, s1, s2, op0, op1=None, R=()):
        if op1 is None:
            f = lambda e: e.tensor_scalar(out=out_ap, in0=a_ap, scalar1=s1, scalar2=None, op0=op0)
        else:
            f = lambda e: e.tensor_scalar(out=out_ap, in0=a_ap, scalar1=s1, scalar2=s2, op0=op0, op1=op1)
        self.op(eng, f, R=[a_b] + list(R), W=[out_b])

    def stt(self, eng, out_b, out_ap, a_b, a_ap, scalar, b_b, b_ap, op0, op1, R=()):
        self.op(eng, lambda e: e.scalar_tensor_tensor(out=out_ap, in0=a_ap, scalar=scalar, in1=b_ap,
                                                     op0=op0, op1=op1),
                R=[a_b, b_b] + list(R), W=[out_b])

    def copy(self, eng, out_b, out_ap, in_b, in_ap):
        if eng == "act":
            self.op(eng, lambda e: e.copy(out=out_ap, in_=in_ap), R=[in_b], W=[out_b])
        else:
            self.op(eng, lambda e: e.tensor_copy(out=out_ap, in_=in_ap), R=[in_b], W=[out_b])

    def memset(self, eng, out_b, out_ap, val):
        self.op(eng, lambda e: e.memset(out_ap, val), W=[out_b])

    def close(self):
        self.es.close()


import math
import numpy as np

D = 1024
NCH = 8
GT = 512
RMS_EPS = 1e-6
PI = math.pi


def rot_perm(n_heads, hd, rope_dim):
    half = rope_dim // 2
    perm = np.arange(n_heads * hd)
    for h in range(n_heads):
        for d in range(rope_dim):
            perm[h * hd + d] = h * hd + (d + half if d < half else d - half)
    return perm


def rope_consts(hd, rope_dim, theta, n=128):
    half = rope_dim // 2
    inv = np.zeros(n, np.float32)
    sgn = np.zeros(n, np.float32)
    fr = (theta ** (-np.arange(half, dtype=np.float32) * 2.0 / rope_dim)).astype(np.float32)
    for p in range(n):
        d = p % hd
        if d < rope_dim:
            inv[p] = fr[d % half]
            sgn[p] = -1.0 if d < half else 1.0
    return inv, sgn


class Ctx:
    pass


def load_weights_bf16(S, wdram, ncols, gvec_b, gcol, wsb, col0=0, kchunks=NCH, scale_cols=None, stg=None):
    CW = 1024
    if stg is None:
        stg = [S.sb([128, CW], F32, "wstg") for _ in range(2)]
    k = 0
    for c in range(kchunks):
        for c0 in range(0, ncols, CW):
            cw = min(CW, ncols - c0)
            st = stg[k % 2]
            S.dma("sp" if k % 2 == 0 else "act", st[:, 0:cw], wdram[c * 128:(c + 1) * 128, c0:c0 + cw], W=[st])
            eng = "dve" if k % 2 == 0 else "pool"
            if gvec_b is not None:
                S.ts(eng, wsb, wsb[:, c, col0 + c0:col0 + c0 + cw], st, st[:, 0:cw],
                     gvec_b[:, gcol + c:gcol + c + 1], None, ALU.mult, R=[gvec_b])
            else:
                S.copy(eng, wsb, wsb[:, c, col0 + c0:col0 + c0 + cw], st, st[:, 0:cw])
            k += 1
    return stg


def norm_transpose_group(S, C, x_dram, g, xt, junk, ssq, rstd, xn, pT, xnT, ident):
    S.dma("sp", xt[:], x_dram[g * GT:(g + 1) * GT, :].rearrange("(j p) d -> p j d", p=128), W=[xt])
    for j in range(4):
        S.op("act", lambda e, j=j: e.activation(out=junk[:], in_=xt[:, j, :], func=AF.Square,
                                                 accum_out=ssq[:, j:j + 1]),
             R=[xt], W=[junk, ssq])
    S.op("act", lambda e: e.activation(out=rstd[:], in_=ssq[:], func=AF.Sqrt, bias=C.epsb[:, 0:1], scale=1.0 / D),
         R=[ssq, C.epsb], W=[rstd])
    S.op("dve", lambda e: e.reciprocal(out=rstd[:], in_=rstd[:]), R=[rstd], W=[rstd])
    for j in range(4):
        S.ts("dve" if j % 2 == 0 else "pool", xn, xn[:, j, :], xt, xt[:, j, :], rstd[:, j:j + 1], None, ALU.mult,
             R=[rstd])
    for c in range(NCH):
        p = pT[c % 2]
        for j in range(4):
            S.tr(p, p[:, j * 128:(j + 1) * 128], xn, xn[:, j, c * 128:(c + 1) * 128], ident, ident[:])
        S.copy("act" if c % 2 == 0 else "dve", xnT, xnT[:, c, :], p, p[:])


def rope_tables(S, C, pos_dram, g, posi, posf, specs, tmp, bias_negpi):
    ang, ki, kf = C.rt_ang, C.rt_ki, C.rt_kf
    S.dma("act", posi[:], pos_dram[g * GT:(g + 1) * GT].partition_broadcast(128), W=[posi])
    S.copy("pool", posf, posf[:], posi, posi[:])
    for (cb, invf, sgn, outs) in specs:
        S.ts("pool", ang, ang[:], posf, posf[:], invf, None, ALU.mult, R=[cb])
        for which in (0, 1):
            if which == 1:
                S.ts("pool", ang, ang[:], ang, ang[:], PI / 2, None, ALU.add)
            S.ts("pool", tmp, tmp[:], ang, ang[:], 1.0 / (2 * PI), None, ALU.mult)
            S.copy("pool", ki, ki[:], tmp, tmp[:])
            S.copy("pool", kf, kf[:], ki, ki[:])
            S.stt("dve", tmp, tmp[:], kf, kf[:], -2 * PI, ang, ang[:], ALU.mult, ALU.add)
            S.ts("pool", tmp, tmp[:], tmp, tmp[:], -PI, PI, ALU.max, ALU.min)
            S.op("act", lambda e: e.activation(out=kf[:], in_=tmp[:], func=AF.Sin), R=[tmp], W=[kf])
            for (cosb, sinb, scale) in outs:
                if which == 0:
                    S.ts("pool", sinb, sinb[:], kf, kf[:], sgn, scale, ALU.mult, ALU.mult, R=[cb])
                else:
                    S.ts("pool", cosb, cosb[:], kf, kf[:], scale, None, ALU.mult)


def stage_P0(S, C, x_dram):
    nc = S.nc
    Sq = C.S
    NG = Sq // GT
    S.begin()
    ident = S.sb([128, 128], BF16, "ident")
    S.dma("sp", ident[:], C.ident_d[:, :], W=[ident])
    cst = S.sb([128, 8], F32, "cst")
    S.dma("sp", cst[:], C.ropec0_d[:, :], W=[cst])
    gv = S.sb([128, NCH], F32, "gv")
    S.dma("sp", gv[:], C.norm_mix_d[0, :].rearrange("(c p) -> p c", p=128), W=[gv], allow_slow_non_contiguous=True)
    negpi = None
    C.epsb = S.sb([128, 1], F32, "epsb")
    S.memset("dve", C.epsb, C.epsb[:], RMS_EPS)
    C.rt_ang = S.sb([128, GT], F32, "rt_ang")
    C.rt_ki = S.sb([128, GT], I32, "rt_ki")
    C.rt_kf = S.sb([128, GT], F32, "rt_kf")
    NFM = 14
    WC = NFM * 256 + 1152
    w = S.sb([128, NCH, WC], BF16, "w0")
    load_weights_bf16(S, C.w0_d, WC, gv, 0, w)

    xt = [S.sb([128, 4, D], F32, "xt")] * 2
    junk = S.sb([128, D], BF16, "junk")
    ssq = S.sb([128, 4], F32, "ssq")
    rstd = S.sb([128, 4], F32, "rstd")
    xn = S.sb([128, 4, D], BF16, "xn")
    xnT = [S.sb([128, NCH, GT], BF16, "xnT") for _ in range(2)]
    pT = [S.ps([128, GT], BF16, "pT") for _ in range(2)]
    pz = [S.ps([128, GT], F32, "pz") for _ in range(2)]
    pr = [S.ps([128, GT], F32, "pr") for _ in range(2)]
    pm = [S.ps([128, GT], F32, "pm") for _ in range(2)]
    posi = S.sb([128, GT], I32, "posi")
    posf = S.sb([128, GT], F32, "posf")
    tmp = S.sb([128, GT], F32, "tmp")
    tabs = {}
    for nm in ("A", "Ak", "B", "Bk"):
        tabs[nm] = (S.sb([128, GT], F32, "cos" + nm), S.sb([128, GT], F32, "sin" + nm))
    t1 = [S.sb([128, GT], F32, "t1") for _ in range(2)]
    t2 = [S.sb([128, GT], F32, "t2") for _ in range(2)]
    ofm = [S.sb([128, GT], BF16, "ofm") for _ in range(3)]
    ova = [S.sb([128, 2, 128], BF16, "ova") for _ in range(2)]
    for b in ova:
        S.memset("pool", b, b[:], 0.0)
        S.memset("pool", b, b[:, :, 0:1], 1.0)
    obv = [S.sb([128, 512], BF16, "obv") for _ in range(2)]
    obg = [S.sb([128, 512], F32, "obg") for _ in range(2)]
    tab_of = ["A"] * 4 + ["Ak"] * 2 + ["B"] * 4 + ["Bk"] * 4
    k = 0
    for g in range(NG):
        X = xnT[g % 2]
        norm_transpose_group(S, C, x_dram, g, xt[g % 2], junk, ssq, rstd, xn, pT, X, ident)
        specs = [(cst, cst[:, 0:1], cst[:, 1:2], [(tabs["A"][0], tabs["A"][1], 1.0), (tabs["Ak"][0], tabs["Ak"][1], 0.125)]),
                 (cst, cst[:, 2:3], cst[:, 3:4], [(tabs["B"][0], tabs["B"][1], 1.0), (tabs["Bk"][0], tabs["Bk"][1], 0.125)])]
        rope_tables(S, C, C.pos_d, g, posi, posf, specs, tmp, negpi)
        for blk in range(NFM):
            z, r = pz[blk % 2], pr[blk % 2]
            c0 = blk * 256
            for c in range(NCH):
                S.mm(z, z[:], w, w[:, c, c0:c0 + 128], X, X[:, c, :], c == 0, c == NCH - 1)
            for c in range(NCH):
                S.mm(r, r[:], w, w[:, c, c0 + 128:c0 + 256], X, X[:, c, :], c == 0, c == NCH - 1)
            cosb, sinb = tabs[tab_of[blk]]
            a, b2, o = t1[blk % 2], t2[blk % 2], ofm[blk % 3]
            S.tt("dve", a, a[:], z, z[:], cosb, cosb[:], ALU.mult)
            S.tt("dve", b2, b2[:], r, r[:], sinb, sinb[:], ALU.mult)
            S.tt("pool", o, o[:], a, a[:], b2, b2[:], ALU.add)
            S.dma("sp", C.fm0_d[blk, :, g * GT:(g + 1) * GT], o[:], R=[o])
        c0 = NFM * 256
        for j in range(4):
            tok0 = g * GT + j * 128
            p1, p2, p3 = pm[0], pm[1], pz[j % 2]
            for (pp, cc, nn) in ((p1, c0, 128), (p2, c0 + 128, 512), (p3, c0 + 640, 512)):
                for c in range(NCH):
                    S.mm(pp, pp[:, 0:nn], X, X[:, c, j * 128:(j + 1) * 128], w, w[:, c, cc:cc + nn], c == 0, c == NCH - 1)
            va, bv, bg = ova[j % 2], obv[j % 2], obg[j % 2]
            S.copy("act", va, va[:, :, 64:128], p1, p1[:, 0:128].rearrange("p (g d) -> p g d", g=2))
            S.copy("dve", bv, bv[:], p2, p2[:])
            S.op("act", lambda e, bg=bg, p3=p3: e.activation(out=bg[:], in_=p3[:], func=AF.Silu), R=[p3], W=[bg])
            S.dma("pool", C.va0_d[tok0:tok0 + 128, :], va[:].rearrange("p g d -> p (g d)"), R=[va])
            S.dma("pool", C.bv0_d[tok0:tok0 + 128, :], bv[:], R=[bv])
            S.dma("pool", C.gate0_d[tok0:tok0 + 128, :], bg[:], R=[bg])
    S.end()


def host_prep_P0(ev_w_in):
    w = np.asarray(ev_w_in, np.float32)
    aq, ak, av, bq, bk, bv, bg = np.split(w, np.cumsum([512, 128, 128, 512, 512, 512])[:], axis=1)
    pa8 = rot_perm(8, 64, 16)
    pa2 = rot_perm(2, 64, 16)
    pb8 = rot_perm(8, 64, 64)
    cols = []
    aqr = aq[:, pa8]
    for j in range(4):
        cols += [aq[:, j * 128:(j + 1) * 128], aqr[:, j * 128:(j + 1) * 128]]
    akr = ak[:, pa2]
    for gq in range(2):
        kk = ak[:, gq * 64:(gq + 1) * 64]
        kr = akr[:, gq * 64:(gq + 1) * 64]
        cols += [kk, kk, kr, kr]
    bqr = bq[:, pb8]
    for j in range(4):
        cols += [bq[:, j * 128:(j + 1) * 128], bqr[:, j * 128:(j + 1) * 128]]
    bkr = bk[:, pb8]
    for j in range(4):
        cols += [bk[:, j * 128:(j + 1) * 128], bkr[:, j * 128:(j + 1) * 128]]
    cols += [av, bv, bg]
    return np.ascontiguousarray(np.concatenate(cols, axis=1))


def bcast_row_to_parts(S, C, src_b, src_ap, dst_b, dst_ap, ps_b, ncol):
    S.mm(ps_b, ps_b[:, 0:ncol], C.onesf, C.onesf[0:1, :], src_b, src_ap, True, True)
    S.copy("dve", dst_b, dst_ap, ps_b, ps_b[:, 0:ncol])


def load_consts(S, C):
    C.ident = S.sb([128, 128], BF16, "ident")
    S.dma("sp", C.ident[:], C.ident_d[:, :], W=[C.ident])
    C.onesf = S.sb([128, 128], F32, "onesf")
    S.memset("pool", C.onesf, C.onesf[:], 1.0)
    C.epsb = S.sb([128, 1], F32, "epsb")
    S.memset("dve", C.epsb, C.epsb[:], RMS_EPS)


def stage_A(S, C):
    Sq = C.S
    N = Sq // 128
    NG = Sq // GT
    S.begin()
    load_consts(S, C)
    maskA = S.sb([128, 2, 512], BF16, "maskA")
    S.dma("sp", maskA[:], C.maskA_d[:, :, :], W=[maskA])
    sinkb = S.sb([128, 8], F32, "sinkb")
    S.dma("sp", sinkb[:], C.sink_d[0, :].partition_broadcast(128), W=[sinkb])
    Q = [S.sb([128, Sq], BF16, "Q%d" % j) for j in range(4)]
    V = S.sb([128, N, 256], BF16, "V")
    for j in range(4):
        for h in range(0, Sq, 2048):
            w_ = min(2048, Sq - h)
            S.dma("sp" if j % 2 == 0 else "act", Q[j][:, h:h + w_], C.fm0_d[j, :, h:h + w_], W=[Q[j]])
    Kz = [[S.sb([128, Sq], BF16, "Kz%d%d" % (j, r)) for r in range(2)] for j in range(2)]
    for j in range(2):
        for r in range(2):
            S.memset("pool", Kz[j][r], Kz[j][r][(1 - r) * 64:(2 - r) * 64, :], 0.0)
            for h in range(0, Sq, 2048):
                w_ = min(2048, Sq - h)
                S.dma("pool", Kz[j][r][r * 64:(r + 1) * 64, h:h + w_], C.fm0_d[4 + j, r * 64:(r + 1) * 64, h:h + w_], W=[Kz[j][r]])
    K = [Kz[0][0], Kz[1][0]]
    S.dma("sp", V[:], C.va0_d.rearrange("(n p) c -> p n c", p=128), W=[V])
    sq = [S.sb([128, GT], F32, "sq") for _ in range(2)]
    accq = S.sb([1, GT], F32, "accq")
    acck = S.sb([1, GT], F32, "acck")
    S.memset("dve", accq, accq[:], 0.0)
    S.memset("dve", acck, acck[:], 0.0)
    pn = S.ps([128, GT], F32, "pn")
    i = 0
    for (lst, acc, kp) in ((Q, accq, 128), (K, acck, 64)):
        for b in lst:
            for g in range(NG):
                s_ = sq[i % 2]
                i += 1
                S.op("act", lambda e, s_=s_, b=b, g=g: e.activation(out=s_[:], in_=b[:, g * GT:(g + 1) * GT], func=AF.Square),
                     R=[b], W=[s_])
                S.mm(pn, pn[0:1, :], C.onesf, C.onesf[0:kp, 0:1], s_, s_[0:kp, :], True, True)
                S.tt("dve", acc, acc[:], pn, pn[0:1, :], acc, acc[:], ALU.max)
    mq = S.sb([1, 4], F32, "mq")
    S.op("dve", lambda e: e.tensor_reduce(out=mq[:, 0:1], in_=accq[:], axis=AX.X, op=ALU.max), R=[accq], W=[mq])
    S.op("dve", lambda e: e.tensor_reduce(out=mq[:, 1:2], in_=acck[:], axis=AX.X, op=ALU.max), R=[acck], W=[mq])
    S.tt("dve", mq, mq[:, 2:3], mq, mq[:, 0:1], mq, mq[:, 1:2], ALU.mult)
    S.op("act", lambda e: e.activation(out=mq[:, 3:4], in_=mq[:, 2:3], func=AF.Sqrt), R=[mq], W=[mq])
    S.ts("dve", mq, mq[:, 3:4], mq, mq[:, 3:4], -1.0, None, ALU.mult)
    negM = S.sb([128, 1], F32, "negM")
    bcast_row_to_parts(S, C, mq, mq[0:1, 3:4], negM, negM[:], pn, 1)
    sinkexp = S.sb([128, 8], F32, "sinkexp")
    S.op("act", lambda e: e.activation(out=sinkexp[:], in_=sinkb[:], func=AF.Exp, bias=negM[:, 0:1]),
         R=[sinkb, negM], W=[sinkexp])
    pss = [S.ps([128, 512], F32, "pss") for _ in range(4)]
    pso = [S.ps([128, 512], F32, "pso") for _ in range(2)]
    pts = [S.sb([128, 512], BF16, "pt") for _ in range(4)]
    den = [S.sb([1, 512], F32, "den") for _ in range(2)]
    rdb = [S.sb([128, 512], F32, "rdb") for _ in range(2)]
    osb = [S.sb([128, 512], BF16, "osb") for _ in range(2)]
    ks = 0
    it = 0
    for n in range(N):
        for g in range(2):
            po = pso[it % 2]
            ms = [m for m in (n - 1, n, n + 1) if 0 <= m < N]
            ptl = []
            for m in ms:
                ps_ = pss[ks % 4]
                pt = pts[ks % 4]
                ks += 1
                for hh in range(4):
                    h = 4 * g + hh
                    j, r = h // 2, h % 2
                    S.mm(ps_, ps_[:, hh * 128:(hh + 1) * 128], Kz[g][r], Kz[g][r][:, m * 128:(m + 1) * 128],
                         Q[j], Q[j][:, n * 128:(n + 1) * 128], True, True)
                S.op("act", lambda e, pt=pt, ps_=ps_: e.activation(out=pt[:], in_=ps_[:], func=AF.Exp, bias=negM[:, 0:1]),
                     R=[ps_, negM], W=[pt])
                if m != n:
                    mi = 0 if m < n else 1
                    S.tt("pool", pt, pt[:], pt, pt[:], maskA, maskA[:, mi, :], ALU.mult)
                ptl.append((m, pt))
            for idx, (m, pt) in enumerate(ptl):
                S.mm(po, po[:, :], V, V[:, m, g * 128:(g + 1) * 128], pt, pt[:], idx == 0, idx == len(ptl) - 1)
            dn, rb, ob = den[it % 2], rdb[it % 2], osb[it % 2]
            for hh in range(4):
                h = 4 * g + hh
                S.ts("dve", dn, dn[0:1, hh * 128:(hh + 1) * 128], po, po[0:1, hh * 128:(hh + 1) * 128],
                     sinkexp[0:1, h:h + 1], None, ALU.add, R=[sinkexp])
            S.op("dve", lambda e, dn=dn: e.reciprocal(out=dn[0:1, :], in_=dn[0:1, :]), R=[dn], W=[dn])
            pb = pss[ks % 4]
            ks += 1
            S.mm(pb, pb[:, :], C.onesf, C.onesf[0:1, :], dn, dn[0:1, :], True, True)
            S.copy("act", rb, rb[64:128, :], pb, pb[64:128, :])
            S.tt("dve", ob, ob[64:128, :], po, po[64:128, :], rb, rb[64:128, :], ALU.mult)
            S.dma("sp", C.mixT0_d[(4 * g) * 64:(4 * g + 4) * 64, n * 128:(n + 1) * 128].rearrange("(h d) q -> d h q", d=64),
                  ob[64:128, :].rearrange("d (h q) -> d h q", h=4), R=[ob])
            it += 1
    S.end()


def stage_B(S, C):
    Sq = C.S
    N = Sq // 128
    L = 128
    S.begin()
    load_consts(S, C)
    relB = S.sb([128, 2, 128], F32, "relB")
    S.dma("sp", relB[:], C.relB_d[:, :, :], W=[relB])
    maskB = S.sb([128, 2, 128], F32, "maskB")
    S.dma("sp", maskB[:], C.maskB_d[:, :, :], W=[maskB])
    cvec = S.sb([128, 4], F32, "cvec")
    S.dma("sp", cvec[:], C.cvecB_d[:, :], W=[cvec])
    lgb = S.sb([128, 16], F32, "lgb")
    S.dma("sp", lgb[:], C.decay_d.rearrange("a b h -> a (b h)")[0, :].partition_broadcast(128), W=[lgb])
    lgp = S.sb([128, 8], F32, "lgp")
    S.dma("sp", lgp[:], C.decayp_d[:, :], W=[lgp])
    for t in (lgb, lgp):
        S.op("act", lambda e, t=t: e.activation(out=t[:], in_=t[:], func=AF.Exp, scale=-1.0), R=[t], W=[t])
        S.ts("dve", t, t[:], t, t[:], 1.0, None, ALU.add)
        S.op("act", lambda e, t=t: e.activation(out=t[:], in_=t[:], func=AF.Ln), R=[t], W=[t])
        S.ts("dve", t, t[:], t, t[:], -1.0, None, ALU.mult)
    zx = S.sb([128, 4, 8], F32, "zx")
    for k_, (ci, d) in enumerate(((0, 0), (1, 1), (2, 0), (3, 1))):
        S.ts("dve", zx, zx[:, k_, :], lgb, lgb[:, d * 8:(d + 1) * 8], cvec[:, ci:ci + 1], None, ALU.mult, R=[cvec])
    S.op("act", lambda e: e.activation(out=zx[:], in_=zx[:], func=AF.Exp), R=[zx], W=[zx])
    cd = S.sb([128, 8], F32, "cd")
    S.op("act", lambda e: e.activation(out=cd[:], in_=lgp[:], func=AF.Exp, scale=float(L)), R=[lgp], W=[cd])
    dc = S.sb([128, 8, 128], F32, "dc")
    dtmp = S.sb([128, 128], F32, "dtmp")
    for h in range(8):
        S.op("act", lambda e, h=h: e.activation(out=dc[:, h, :], in_=relB[:, 0, :], func=AF.Exp, scale=lgb[:, h:h + 1]),
             R=[relB, lgb], W=[dc])
        S.tt("dve", dc, dc[:, h, :], dc, dc[:, h, :], maskB, maskB[:, 0, :], ALU.mult)
        S.op("act", lambda e, h=h: e.activation(out=dtmp[:], in_=relB[:, 1, :], func=AF.Exp, scale=lgb[:, 8 + h:9 + h]),
             R=[relB, lgb], W=[dtmp])
        S.tt("dve", dtmp, dtmp[:], dtmp, dtmp[:], maskB, maskB[:, 1, :], ALU.mult)
        S.tt("dve", dc, dc[:, h, :], dc, dc[:, h, :], dtmp, dtmp[:], ALU.add)
    Vt = S.sb([128, N, 128], BF16, "Vt")
    bdm = S.sb([128, 128], F32, "bdm")
    S.dma("sp", bdm[:], C.bdm_d[:, :], W=[bdm])
    Kpz = [S.sb([128, Sq], BF16, "Kpz%d" % r) for r in range(2)]
    Gt = S.sb([128, N, 128], F32, "Gt")
    Qp = S.sb([128, Sq], BF16, "Qp")
    Kp = S.sb([128, Sq], BF16, "Kp")
    Rf = S.sb([128, 128], F32, "Rf")
    Rb = S.sb([128, 128], F32, "Rb")
    Rfp = S.sb([128, N, 128], BF16, "Rfp")
    Rbp = S.sb([128, N, 128], BF16, "Rbp")
    pk = [S.ps([128, 128], BF16, "pk") for _ in range(2)]
    pu = [S.ps([128, 256], F32, "pu") for _ in range(2)]
    pss = [S.ps([128, 256], F32, "pss") for _ in range(2)]
    py = [S.ps([128, 384], F32, "py") for _ in range(2)]
    kz = [S.sb([128, 2, 128], BF16, "kz") for _ in range(2)]
    sd = [S.sb([128, 2, 128], BF16, "sd") for _ in range(2)]
    ysb = [S.sb([128, 128], F32, "ysb") for _ in range(2)]
    junk = S.sb([128, 64], F32, "junkB")
    st = [S.sb([128, 8], F32, "st") for _ in range(2)]
    ob = [S.sb([128, 128], BF16, "ob") for _ in range(2)]
    oT = [S.sb([128, 512], BF16, "oT") for _ in range(2)]
    gneps = S.sb([128, 1], F32, "gneps")
    S.memset("dve", gneps, gneps[:], 1e-5)
    for pr_ in range(4):
        for h0 in range(0, Sq, 2048):
            w_ = min(2048, Sq - h0)
            S.dma("sp", Qp[:, h0:h0 + w_], C.fm0_d[6 + pr_, :, h0:h0 + w_], W=[Qp])
            S.dma("act", Kp[:, h0:h0 + w_], C.fm0_d[10 + pr_, :, h0:h0 + w_], W=[Kp])
        S.dma("pool", Gt[:], C.gate0_d[:, pr_ * 128:(pr_ + 1) * 128].rearrange("(n p) c -> p n c", p=128), W=[Gt])
        S.dma("sp", Vt[:], C.bv0_d[:, pr_ * 128:(pr_ + 1) * 128].rearrange("(n p) c -> p n c", p=128), W=[Vt])
        for r in range(2):
            S.copy("pool", Kpz[r], Kpz[r][:], Kp, Kp[:])
            S.memset("pool", Kpz[r], Kpz[r][(1 - r) * 64:(2 - r) * 64, :], 0.0)
        S.memset("dve", Rf, Rf[:], 0.0)
        S.memset("dve", Rb, Rb[:], 0.0)
        for dirn in (0, 1):
            R_, Rp = (Rf, Rfp) if dirn == 0 else (Rb, Rbp)
            order = range(N) if dirn == 0 else range(N - 1, -1, -1)
            for n in order:
                p_, kz_, pu_ = pk[n % 2], kz[n % 2], pu[n % 2]
                S.tr(p_, p_[:], Kp, Kp[:, n * 128:(n + 1) * 128], C.ident, C.ident[:])
                for r in range(2):
                    h = 2 * pr_ + r
                    S.ts("dve", kz_, kz_[:, 0, r * 64:(r + 1) * 64], p_, p_[:, r * 64:(r + 1) * 64],
                         zx[:, dirn, h:h + 1], None, ALU.mult, R=[zx])
                S.mm(pu_, pu_[:, 0:128], kz_, kz_[:, 0, :], Vt, Vt[:, n, :], True, True)
                S.tt("pool", Rp, Rp[:, n, :], R_, R_[:], bdm, bdm[:], ALU.mult)
                S.stt("dve", R_, R_[:], R_, R_[:], cd[:, pr_ * 2 + dirn:pr_ * 2 + dirn + 1], pu_, pu_[:, 0:128],
                      ALU.mult, ALU.add, R=[cd])
        for n in range(N):
            ps_, sd_, py_, y_, st_, ob_ = pss[n % 2], sd[n % 2], py[n % 2], ysb[n % 2], st[n % 2], ob[n % 2]
            tok = slice(n * 128, (n + 1) * 128)
            for r in range(2):
                S.mm(ps_, ps_[:, r * 128:(r + 1) * 128], Kpz[r], Kpz[r][:, tok], Qp, Qp[:, tok], True, True)
            S.tt("dve", sd_, sd_[:].rearrange("p a b -> p (a b)"), ps_, ps_[:],
                 dc, dc[:, 2 * pr_:2 * pr_ + 2, :].rearrange("p a b -> p (a b)"), ALU.mult)
            for r in range(2):
                S.mm(py_, py_[:, r * 64:(r + 1) * 64], sd_, sd_[:, r, :], Vt, Vt[:, n, r * 64:(r + 1) * 64], True, True)
            S.mm(py_, py_[:, 128:256], Qp, Qp[:, tok], Rfp, Rfp[:, n, :], True, True)
            S.mm(py_, py_[:, 256:384], Qp, Qp[:, tok], Rbp, Rbp[:, n, :], True, True)
            S.copy("act", y_, y_[:], py_, py_[:, 0:128])
            for r in range(2):
                h = 2 * pr_ + r
                c_ = slice(r * 64, (r + 1) * 64)
                S.stt("dve", y_, y_[:, c_], py_, py_[:, 128 + r * 64:128 + (r + 1) * 64], zx[:, 2, h:h + 1], y_, y_[:, c_],
                      ALU.mult, ALU.add, R=[zx])
                S.stt("dve", y_, y_[:, c_], py_, py_[:, 256 + r * 64:256 + (r + 1) * 64], zx[:, 3, h:h + 1], y_, y_[:, c_],
                      ALU.mult, ALU.add, R=[zx])
            S.op("dve", lambda e, y_=y_, st_=st_: e.tensor_reduce(out=st_[:, 0:2], in_=y_[:].rearrange("p (a b) -> p a b", a=2),
                                                                 axis=AX.X, op=ALU.add), R=[y_], W=[st_])
            for r in range(2):
                S.op("act", lambda e, y_=y_, st_=st_, r=r: e.activation(out=junk[:], in_=y_[:, r * 64:(r + 1) * 64], func=AF.Square,
                                                                         accum_out=st_[:, 2 + r:3 + r]), R=[y_], W=[junk, st_])
            S.ts("dve", st_, st_[:, 4:6], st_, st_[:, 0:2], 1.0 / 64, None, ALU.mult)
            S.tt("dve", st_, st_[:, 0:2], st_, st_[:, 4:6], st_, st_[:, 4:6], ALU.mult)
            S.stt("dve", st_, st_[:, 6:8], st_, st_[:, 2:4], 1.0 / 64, st_, st_[:, 0:2], ALU.mult, ALU.subtract)
            S.op("act", lambda e, st_=st_: e.activation(out=st_[:, 6:8], in_=st_[:, 6:8], func=AF.Sqrt, bias=gneps[:, 0:1]),
                 R=[st_, gneps], W=[st_])
            S.op("dve", lambda e, st_=st_: e.reciprocal(out=st_[:, 6:8], in_=st_[:, 6:8]), R=[st_], W=[st_])
            for r in range(2):
                c_ = slice(r * 64, (r + 1) * 64)
                S.ts("dve", y_, y_[:, c_], y_, y_[:, c_], st_[:, 4 + r:5 + r], st_[:, 6 + r:7 + r], ALU.subtract, ALU.mult, R=[st_])
            S.tt("pool", ob_, ob_[:], y_, y_[:], Gt, Gt[:, n, :], ALU.mult)
            pt_ = pk[n % 2]
            S.tr(pt_, pt_[:], ob_, ob_[:], C.ident, C.ident[:])
            oT_ = oT[(n // 4) % 2]
            S.copy("act", oT_, oT_[:, (n % 4) * 128:(n % 4 + 1) * 128], pt_, pt_[:])
            if n % 4 == 3:
                S.dma("sp", C.mixT0_d[512 + pr_ * 128:512 + (pr_ + 1) * 128, (n - 3) * 128:(n + 1) * 128], oT_[:], R=[oT_])
    S.end()


def declare(nc, C, Sq, debug=False):
    ks = "ExternalOutput" if debug else "Internal"
    def inp(name, shape, dt):
        return nc.dram_tensor(name, list(shape), dt, kind="ExternalInput").ap()
    def scr(name, shape, dt):
        return nc.dram_tensor(name, list(shape), dt, kind=ks).ap()
    C.S = Sq
    C.x_d = inp("x", [Sq, D], F32)
    C.pos_d = inp("pos", [Sq], I32)
    C.norm_mix_d = inp("norm_mix", [2, D], F32)
    C.w0_d = inp("w0", [D, 14 * 256 + 1152], F32)
    C.ident_d = inp("ident", [128, 128], BF16)
    C.ropec0_d = inp("ropec0", [128, 8], F32)
    C.maskA_d = inp("maskA", [128, 2, 512], BF16)
    C.sink_d = inp("sink", [1, 8], F32)
    C.relB_d = inp("relB", [128, 2, 128], F32)
    C.maskB_d = inp("maskB", [128, 2, 128], F32)
    C.cvecB_d = inp("cvecB", [128, 4], F32)
    C.decay_d = inp("decay", [1, 2, 8], F32)
    C.decayp_d = inp("decayp", [128, 8], F32)
    C.identf_d = inp("identf", [128, 128], F32)
    C.norm_ffn_d = inp("norm_ffn", [2, D], F32)
    C.router_d = inp("router", [2, D, 16], F32)
    C.tokc_d = inp("tokc", [128, Sq // 128], I32)
    C.trib_d = inp("trib", [128, 128], BF16)
    C.ecap_d = inp("ecap", [128, 16], F32)
    C.wout0_d = inp("wout0", [D, D], F32)
    C.wg_d = inp("wg", [2, 16, D, 2048], F32)
    C.wu_d = inp("wu", [2, 16, D, 2048], F32)
    C.wd_d = inp("wd", [2, 16, 2048, D], F32)
    C.norm_final_d = inp("norm_final", [1, D], F32)
    C.y_d = nc.dram_tensor("y", [Sq, D], F32, kind="ExternalOutput").ap()
    C.x1_d = scr("x1", [Sq, D], F32)
    C.hnx_d = scr("hnx", [Sq, XW], I32)
    C.xin_d = scr("xin", [16 * (Sq // 8), XW], I32)
    C.bdm_d = inp("bdm", [128, 128], F32)
    C.w1_d = inp("w1", [D, W1C], F32)
    C.wuq_d = inp("wuq", [512, 1536], F32)
    C.wukv_d = inp("wukv", [256, 1024], F32)
    C.ropec1_d = inp("ropec1", [128, 2], F32)
    C.mla_nq_d = inp("mla_nq", [1, 512], F32)
    C.mla_nkv_d = inp("mla_nkv", [1, 256], F32)
    C.gbias_d = inp("gbias", [4, 4], F32)
    C.wout1_d = inp("wout1", [D, D], F32)
    C.fmq_d = scr("fmq", [8, 96, Sq], BF16)
    C.fmk_d = scr("fmk", [8, 96, Sq], BF16)
    C.vC_d = scr("vC", [Sq, 1024], BF16)
    C.qkraw_d = scr("qkraw", [1024, Sq], F32)
    C.gatesT_d = scr("gatesT", [4, 4, Sq], F32)
    C.vD_d = scr("vD", [Sq, 4 * 129], BF16)
    C.og_d = scr("og", [Sq, 512], F32)
    C.mixT1_d = scr("mixT1", [1024, Sq], BF16)
    C.conv_d = inp("conv", [128, 40], F32)
    C.maskD_d = inp("maskD", [128, 2, 128], F32)
    C.qkc_d = scr("qkc", [8, 128, Sq], BF16)
    C.vec_d = scr("vec", [10, 4, Sq], F32)
    C.vec2_d = scr("vec2", [4, 4, Sq // 128], F32)
    C.chunk_d = scr("chunkd", [4, 4, Sq // 128], F32)
    C.mp_d = scr("mpd", [2, 4, Sq // 128], F32)
    C.x3_d = scr("x3", [Sq, D], F32)
    C.fm0_d = scr("fm0", [14, 128, Sq], BF16)
    C.va0_d = scr("va0", [Sq, 256], BF16)
    C.bv0_d = scr("bv0", [Sq, 512], BF16)
    C.gate0_d = scr("gate0", [Sq, 512], F32)
    C.mixT0_d = scr("mixT0", [1024, Sq], BF16)


def host_consts():
    import ml_dtypes
    bf = ml_dtypes.bfloat16
    c = {}
    c["ident"] = np.eye(128, dtype=np.float32).astype(bf)
    rc = np.zeros((128, 8), np.float32)
    rc[:, 0], rc[:, 1] = rope_consts(64, 16, 500000.0)
    rc[:, 2], rc[:, 3] = rope_consts(64, 64, 10000.0)
    c["ropec0"] = rc
    j = np.arange(128)[:, None]
    i = np.arange(128)[None, :]
    mA = np.zeros((128, 2, 512), np.float32)
    mA[:, 0, :] = np.tile((j >= i).astype(np.float32), (1, 4))
    mA[:, 1, :] = np.tile((j <= i).astype(np.float32), (1, 4))
    c["maskA"] = mA.astype(bf)
    rel = (i - j).astype(np.float32)
    rB = np.zeros((128, 2, 128), np.float32)
    rB[:, 0] = np.maximum(rel, 0)
    rB[:, 1] = np.maximum(-rel, 0)
    c["relB"] = rB
    mB = np.zeros((128, 2, 128), np.float32)
    mB[:, 0] = (rel >= 0)
    mB[:, 1] = (rel < 0)
    c["maskB"] = mB
    c["identf"] = np.eye(128, dtype=np.float32)
    c["bdm"] = ((j // 64) == (i // 64)).astype(np.float32)
    c["trib"] = (j < i).astype(np.float32).astype(bf)
    l = np.arange(128, dtype=np.float32)
    c["cvecB"] = np.stack([127 - l, l, l + 1, 128 - l], axis=1).astype(np.float32)
    return c


def host_inputs(inp, b):
    d = dict(host_consts())
    d["x"] = np.ascontiguousarray(inp["x"][b])
    d["pos"] = np.ascontiguousarray(inp["positions"][b]).astype(np.int32)
    d["norm_mix"] = np.asarray(inp["norm_mix"], np.float32)
    d["w0"] = host_prep_P0(inp["ev_w_in"][0])
    Sq = d["x"].shape[0]
    d["tokc"] = (np.arange(Sq // 128, dtype=np.int32)[None, :] * 128 + np.arange(128, dtype=np.int32)[:, None]).astype(np.int32)
    d["ecap"] = np.tile((np.arange(16, dtype=np.float32) * (Sq // 8))[None, :], (128, 1)).astype(np.float32)
    d["norm_ffn"] = np.asarray(inp["norm_ffn"], np.float32)
    d["router"] = np.asarray(inp["moe_router"], np.float32)
    d["wout0"] = np.asarray(inp["ev_w_out"][0], np.float32)
    d["wg"] = np.asarray(inp["moe_w_gate"], np.float32)
    d["wu"] = np.asarray(inp["moe_w_up"], np.float32)
    d["wd"] = np.asarray(inp["moe_w_down"], np.float32)
    d["norm_final"] = np.asarray(inp["norm_final"], np.float32).reshape(1, D)
    d["w1"], d["wuq"], d["wukv"] = host_prep_P1(inp["od_w_in"][0], inp["mla_w_uq"][0], inp["mla_w_ukv"][0])
    d["ropec1"] = ropec1()
    d["mla_nq"] = np.asarray(inp["mla_norm_q"], np.float32).reshape(1, 512)
    d["mla_nkv"] = np.asarray(inp["mla_norm_kv"], np.float32).reshape(1, 256)
    d["gbias"] = np.ascontiguousarray(np.asarray(inp["mlstm_gate_bias"], np.float32)[0].T)
    d["wout1"] = np.asarray(inp["od_w_out"][0], np.float32)
    d["conv"] = np.ascontiguousarray(np.asarray(inp["mlstm_conv"][0], np.float32).reshape(5, 8, 128).transpose(2, 1, 0).reshape(128, 40))
    jj = np.arange(128)[:, None]; tt_ = np.arange(128)[None, :]
    d["maskD"] = np.stack([(jj <= tt_), (jj > tt_)], axis=1).astype(np.float32)
    d["sink"] = np.asarray(inp["attn_sink"], np.float32).reshape(1, 8)
    dl = np.asarray(inp["ret_decay_logit"], np.float32).reshape(1, 2, 8)
    d["decay"] = dl
    dp = np.zeros((128, 8), np.float32)
    for pr_ in range(4):
        for dr in range(2):
            dp[0:64, pr_ * 2 + dr] = dl[0, dr, 2 * pr_]
            dp[64:128, pr_ * 2 + dr] = dl[0, dr, 2 * pr_ + 1]
    d["decayp"] = dp
    return d

NE = 16
XW = 529


def stage_OUT(S, C, l, mixT_d, wout_d, xin_d, xout_d):
    Sq = C.S
    NG = Sq // GT
    S.begin()
    load_consts(S, C)
    identf = S.sb([128, 128], F32, "identf")
    S.dma("sp", identf[:], C.identf_d[:, :], W=[identf])
    w = S.sb([128, NCH, D], BF16, "wout")
    load_weights_bf16(S, wout_d, D, None, 0, w)
    gb = S.sb([128, D], F32, "gb")
    S.dma("sp", gb[:], C.norm_ffn_d[l, :].partition_broadcast(128), W=[gb])
    wr = S.sb([128, NCH, NE], F32, "wr")
    S.dma("sp", wr[:], C.router_d[l].rearrange("(c p) e -> p c e", p=128), W=[wr])
    tokc = S.sb([128, Sq // 128], I32, "tokc")
    S.dma("sp", tokc[:], C.tokc_d[:, :], W=[tokc])
    mT = [S.sb([128, NCH, GT], BF16, "mT") for _ in range(2)]
    xt = [S.sb([128, D], F32, "xt") for _ in range(2)]
    x1 = [S.sb([128, D], F32, "x1") for _ in range(2)]
    hn = [S.sb([128, D], F32, "hn") for _ in range(2)]
    hT = S.sb([128, NCH, 128], F32, "hT")
    row = [S.sb([128, XW], I32, "row") for _ in range(2)]
    junk = S.sb([128, D], BF16, "junk")
    st = [S.sb([128, 8], F32, "st") for _ in range(2)]
    lg = [S.sb([128, NE], F32, "lg") for _ in range(2)]
    po = [S.ps([128, 512], F32, "po") for _ in range(4)]
    pt = [S.ps([128, 512], F32, "pt") for _ in range(2)]
    pl = S.ps([128, NE], F32, "pl")
    for g in range(NG):
        M = mT[g % 2]
        S.dma("sp", M[:], mixT_d[:, g * GT:(g + 1) * GT].rearrange("(c p) t -> p c t", p=128), W=[M])
        for j in range(4):
            it = g * 4 + j
            tok0 = it * 128
            X, X1, H, R, st_, lg_ = xt[it % 2], x1[it % 2], hn[it % 2], row[it % 2], st[it % 2], lg[it % 2]
            S.dma("act", X[:], xin_d[tok0:tok0 + 128, :], W=[X])
            for hf in range(2):
                p_ = po[(it * 2 + hf) % 4]
                for c in range(NCH):
                    S.mm(p_, p_[:], M, M[:, c, j * 128:(j + 1) * 128], w, w[:, c, hf * 512:(hf + 1) * 512], c == 0, c == NCH - 1)
                S.tt("dve", X1, X1[:, hf * 512:(hf + 1) * 512], p_, p_[:], X, X[:, hf * 512:(hf + 1) * 512], ALU.add)
            S.dma("sp", xout_d[tok0:tok0 + 128, :], X1[:], R=[X1])
            S.op("act", lambda e, X1=X1, st_=st_: e.activation(out=junk[:], in_=X1[:], func=AF.Square, accum_out=st_[:, 0:1]),
                 R=[X1], W=[junk, st_])
            S.op("act", lambda e, st_=st_: e.activation(out=st_[:, 1:2], in_=st_[:, 0:1], func=AF.Sqrt, bias=C.epsb[:, 0:1], scale=1.0 / D),
                 R=[st_, C.epsb], W=[st_])
            S.op("dve", lambda e, st_=st_: e.reciprocal(out=st_[:, 1:2], in_=st_[:, 1:2]), R=[st_], W=[st_])
            S.stt("dve", H, H[:], X1, X1[:], st_[:, 1:2], gb, gb[:], ALU.mult, ALU.mult, R=[st_])
            S.copy("pool", R, R[:, 0:512].bitcast(BF16), H, H[:])
            for c in range(NCH):
                p_ = pt[c % 2]
                S.tr(p_, p_[:, 0:128], H, H[:, c * 128:(c + 1) * 128], identf, identf[:])
                S.copy("act" if c % 2 == 0 else "dve", hT, hT[:, c, :], p_, p_[:, 0:128])
            for c in range(NCH):
                S.mm(pl, pl[:], hT, hT[:, c, :], wr, wr[:, c, :], c == 0, c == NCH - 1)
            S.op("dve", lambda e, st_=st_: e.tensor_reduce(out=st_[:, 2:3], in_=pl[:], axis=AX.X, op=ALU.max), R=[pl], W=[st_])
            S.ts("dve", st_, st_[:, 2:3], st_, st_[:, 2:3], -1.0, None, ALU.mult)
            S.op("act", lambda e, st_=st_, lg_=lg_: e.activation(out=lg_[:], in_=pl[:], func=AF.Exp, bias=st_[:, 2:3], accum_out=st_[:, 3:4]),
                 R=[pl, st_], W=[lg_, st_])
            S.op("dve", lambda e, st_=st_: e.reciprocal(out=st_[:, 4:5], in_=st_[:, 3:4]), R=[st_], W=[st_])
            S.ts("dve", R, R[:, 512:528].bitcast(F32), lg_, lg_[:], st_[:, 4:5], None, ALU.mult, R=[st_])
            S.copy("pool", R, R[:, 528:529], tokc, tokc[:, it:it + 1])
            S.dma("sp", C.hnx_d[tok0:tok0 + 128, :], R[:], R=[R])
    S.end()


def stage_MOE(S, C, l, x_d):
    Sq = C.S
    N = Sq // 128
    cap = Sq // 8
    NT = cap // 128
    BIG = float(NE * cap + 4096)
    S.begin()
    load_consts(S, C)
    aff = S.sb([128, N, NE], F32, "aff")
    S.dma("sp", aff[:], C.hnx_d[:, 512:528].bitcast(F32).rearrange("(n p) e -> p n e", p=128), W=[aff],
          allow_slow_non_contiguous=True)
    lo = S.sb([128, NE], F32, "lo")
    mid = S.sb([128, NE], F32, "mid")
    ge = S.sb([128, NE], F32, "ge")
    cnt = S.sb([128, NE], F32, "cnt")
    cmp_ = S.sb([128, N, NE], F32, "cmp")
    pc = S.ps([128, NE], F32, "pc")
    S.memset("dve", lo, lo[:], 0.0)
    for k in range(1, 29):
        wk = 2.0 ** (-k)
        S.ts("dve", mid, mid[:], lo, lo[:], wk, None, ALU.add)
        S.tt("dve", cmp_, cmp_[:], aff, aff[:], mid, mid[:].unsqueeze(1).to_broadcast([128, N, NE]), ALU.is_ge)
        S.op("dve", lambda e: e.tensor_reduce(out=cnt[:], in_=cmp_[:].rearrange("p n e -> p e n"), axis=AX.X, op=ALU.add),
             R=[cmp_], W=[cnt])
        S.mm(pc, pc[:], C.onesf, C.onesf[:], cnt, cnt[:], True, True)
        S.ts("dve", ge, ge[:], pc, pc[:], float(cap) - 0.5, None, ALU.is_ge)
        S.stt("dve", lo, lo[:], ge, ge[:], wk, lo, lo[:], ALU.mult, ALU.add)
    S.tt("dve", cmp_, cmp_[:], aff, aff[:], lo, lo[:].unsqueeze(1).to_broadcast([128, N, NE]), ALU.is_ge)
    maskb = S.sb([128, N * NE], BF16, "maskb")
    S.copy("dve", maskb, maskb[:], cmp_, cmp_[:].rearrange("p n e -> p (n e)"))
    trib = S.sb([128, 128], BF16, "trib")
    S.dma("sp", trib[:], C.trib_d[:, :], W=[trib])
    onesb = S.sb([128, 128], BF16, "onesb")
    S.memset("pool", onesb, onesb[:], 1.0)
    slot = S.sb([128, N, NE], F32, "slot")
    tot = [S.sb([128, N, NE], F32, "tot") for _ in range(2)]
    pp = [S.ps([128, 512], F32, "pp") for _ in range(2)]
    W_ = N * NE
    for c0 in range(0, W_, 512):
        cw = min(512, W_ - c0)
        S.mm(pp[0], pp[0][:, 0:cw], trib, trib[:], maskb, maskb[:, c0:c0 + cw], True, True)
        S.copy("dve", slot, slot[:].rearrange("p n e -> p (n e)")[:, c0:c0 + cw], pp[0], pp[0][:, 0:cw])
        S.mm(pp[1], pp[1][:, 0:cw], onesb, onesb[:], maskb, maskb[:, c0:c0 + cw], True, True)
        S.copy("dve", tot[0], tot[0][:].rearrange("p n e -> p (n e)")[:, c0:c0 + cw], pp[1], pp[1][:, 0:cw])
    S.tt("dve", slot, slot[:], slot, slot[:], tot[0], tot[0][:], ALU.subtract)
    cur = 0
    s_ = 1
    while s_ < N:
        a, b = tot[cur], tot[1 - cur]
        S.copy("dve", b, b[:, 0:s_, :], a, a[:, 0:s_, :])
        S.tt("dve", b, b[:, s_:N, :], a, a[:, s_:N, :], a, a[:, 0:N - s_, :], ALU.add)
        cur = 1 - cur
        s_ *= 2
    S.tt("dve", slot, slot[:], slot, slot[:], tot[cur], tot[cur][:], ALU.add)
    ecap = S.sb([128, NE], F32, "ecap")
    S.dma("sp", ecap[:], C.ecap_d[:, :], W=[ecap])
    val = tot[1 - cur]
    S.ts("dve", val, val[:], slot, slot[:], float(cap) - 0.5, None, ALU.is_lt)
    S.tt("dve", val, val[:], val, val[:], cmp_, cmp_[:], ALU.mult)
    S.tt("dve", slot, slot[:], slot, slot[:], ecap, ecap[:].unsqueeze(1).to_broadcast([128, N, NE]), ALU.add)
    S.ts("dve", slot, slot[:], slot, slot[:], -BIG, None, ALU.add)
    S.tt("dve", slot, slot[:], slot, slot[:], val, val[:], ALU.mult)
    S.ts("dve", slot, slot[:], slot, slot[:], BIG, None, ALU.add)
    idx = S.sb([128, N, NE], I32, "idx")
    S.copy("dve", idx, idx[:], slot, slot[:])
    rows = [S.sb([128, XW], I32, "rows") for _ in range(3)]
    breg = S.reg("pool", NE * cap - 1)
    for n in range(N):
        R = rows[n % 3]
        S.dma("sp", R[:], C.hnx_d[n * 128:(n + 1) * 128, :], W=[R])
        for e_ in range(NE):
            def fn(eng, R=R, n=n, e_=e_):
                return eng.indirect_dma_start(
                    out=C.xin_d[:, :], out_offset=bass.IndirectOffsetOnAxis(ap=idx[:, n, e_:e_ + 1], axis=0),
                    in_=R[:], in_offset=None, bounds_check=breg["r"], oob_is_err=False)
            S.dma("pool", None, None, R=[R, idx], sem_buf=R, fn=fn)
    S.end()
    S.begin()
    load_consts(S, C)
    wg = S.sb([128, NCH, 2048], BF16, "wg")
    wu = S.sb([128, NCH, 2048], BF16, "wu")
    wd = S.sb([128, 16, D], BF16, "wd")
    stg = [S.sb([128, 1024], F32, "wstg") for _ in range(3)]
    xs = [S.sb([128, XW], I32, "xs") for _ in range(2)]
    xT = S.sb([128, NCH, cap], BF16, "xT")
    gates = S.sb([128, NT], F32, "gates")
    toks = S.sb([128, NT], I32, "toks")
    HS = min(512, cap)
    hid = S.sb([128, 16, HS], BF16, "hid")
    sg = [S.sb([128, HS], F32, "sg") for _ in range(2)]
    yo = [S.sb([128, D], F32, "yo") for _ in range(2)]
    ptp = [S.ps([128, 512], BF16, "ptp") for _ in range(2)]
    pg = [S.ps([128, 512], F32, "pg") for _ in range(2)]
    pu = [S.ps([128, 512], F32, "pu") for _ in range(2)]
    py = [S.ps([128, 512], F32, "py") for _ in range(2)]
    kk = 0
    breg2 = S.reg("pool", Sq - 1)
    prev_scatter = []
    for e_ in range(NE):
        for (wsb, wdr, kch, ncol) in ((wg, C.wg_d[l, e_], NCH, 2048), (wu, C.wu_d[l, e_], NCH, 2048), (wd, C.wd_d[l, e_], 16, D)):
            for c in range(kch):
                for c0 in range(0, ncol, 1024):
                    st_ = stg[kk % 3]
                    S.dma("sp" if kk % 2 == 0 else "act", st_[:], wdr[c * 128:(c + 1) * 128, c0:c0 + 1024], W=[st_])
                    S.copy(("dve", "pool", "act")[kk % 3], wsb, wsb[:, c, c0:c0 + 1024], st_, st_[:])
                    kk += 1
        for t in range(NT):
            X = xs[t % 2]
            S.dma("sp", X[:], C.xin_d[e_ * cap + t * 128:e_ * cap + (t + 1) * 128, :], W=[X])
            S.copy("dve", gates, gates[:, t:t + 1], X, X[:, 512 + e_:513 + e_].bitcast(F32))
            S.copy("dve", toks, toks[:, t:t + 1], X, X[:, 528:529])
            for c in range(NCH):
                p_ = ptp[c % 2]
                S.tr(p_, p_[:, 0:128], X, X[:, 0:512].bitcast(BF16)[:, c * 128:(c + 1) * 128], C.ident, C.ident[:])
                S.copy("act" if c % 2 == 0 else "dve", xT, xT[:, c, t * 128:(t + 1) * 128], p_, p_[:, 0:128])
        new_scatter = []
        for s0 in range(0, cap, HS):
            for fb in range(16):
                g_, u_, sg_ = pg[fb % 2], pu[fb % 2], sg[fb % 2]
                for c in range(NCH):
                    S.mm(g_, g_[:, 0:HS], wg, wg[:, c, fb * 128:(fb + 1) * 128], xT, xT[:, c, s0:s0 + HS], c == 0, c == NCH - 1)
                for c in range(NCH):
                    S.mm(u_, u_[:, 0:HS], wu, wu[:, c, fb * 128:(fb + 1) * 128], xT, xT[:, c, s0:s0 + HS], c == 0, c == NCH - 1)
                S.op("act", lambda e, sg_=sg_, g_=g_: e.activation(out=sg_[:], in_=g_[:, 0:HS], func=AF.Silu), R=[g_], W=[sg_])
                S.tt("dve", hid, hid[:, fb, :], u_, u_[:, 0:HS], sg_, sg_[:], ALU.mult)
            for t in range(HS // 128):
                tt_ = s0 // 128 + t
                Y = yo[tt_ % 2]
                for hf in range(2):
                    p_ = py[hf]
                    for fb in range(16):
                        S.mm(p_, p_[:], hid, hid[:, fb, t * 128:(t + 1) * 128], wd, wd[:, fb, hf * 512:(hf + 1) * 512], fb == 0, fb == 15)
                    S.ts("dve" if hf == 0 else "dve", Y, Y[:, hf * 512:(hf + 1) * 512], p_, p_[:], gates[:, tt_:tt_ + 1], None, ALU.mult, R=[gates])

                def fn(eng, Y=Y, tt_=tt_):
                    return eng.indirect_dma_start(
                        out=x_d[:, :], out_offset=bass.IndirectOffsetOnAxis(ap=toks[:, tt_:tt_ + 1], axis=0),
                        in_=Y[:], in_offset=None, bounds_check=breg2["r"], oob_is_err=False, compute_op=ALU.add)
                tk = S.dma("pool", None, None, R=[Y, toks], sem_buf=Y, fn=fn, extra=prev_scatter)
                new_scatter.append(tk)
        prev_scatter = new_scatter
    S.end()


def stage_FIN(S, C, x_d, out_d):
    Sq = C.S
    S.begin()
    load_consts(S, C)
    gb = S.sb([128, D], F32, "gbf")
    S.dma("sp", gb[:], C.norm_final_d[0, :].partition_broadcast(128), W=[gb])
    xt = [S.sb([128, D], F32, "xf") for _ in range(2)]
    yo = [S.sb([128, D], F32, "yf") for _ in range(2)]
    junk = S.sb([128, D], BF16, "junkf")
    st = [S.sb([128, 2], F32, "stf") for _ in range(2)]
    for t in range(Sq // 128):
        X, Y, st_ = xt[t % 2], yo[t % 2], st[t % 2]
        S.dma("sp", X[:], x_d[t * 128:(t + 1) * 128, :], W=[X])
        S.op("act", lambda e, X=X, st_=st_: e.activation(out=junk[:], in_=X[:], func=AF.Square, accum_out=st_[:, 0:1]),
             R=[X], W=[junk, st_])
        S.op("act", lambda e, st_=st_: e.activation(out=st_[:, 1:2], in_=st_[:, 0:1], func=AF.Sqrt, bias=C.epsb[:, 0:1], scale=1.0 / D),
             R=[st_, C.epsb], W=[st_])
        S.op("dve", lambda e, st_=st_: e.reciprocal(out=st_[:, 1:2], in_=st_[:, 1:2]), R=[st_], W=[st_])
        S.stt("dve", Y, Y[:], X, X[:], st_[:, 1:2], gb, gb[:], ALU.mult, ALU.mult, R=[st_])
        S.dma("act", out_d[t * 128:(t + 1) * 128, :], Y[:], R=[Y])
    S.end()


def run_stage(S, C, st):
    if st == "P0": stage_P0(S, C, C.x_d)
    elif st == "A": stage_A(S, C)
    elif st == "B": stage_B(S, C)
    elif st == "OUT0": stage_OUT(S, C, 0, C.mixT0_d, C.wout0_d, C.x_d, C.x1_d)
    elif st == "MOE0": stage_MOE(S, C, 0, C.x1_d)
    elif st == "P1": stage_P1(S, C, C.x1_d)
    elif st == "C": stage_C(S, C)
    elif st == "D": stage_D(S, C)
    elif st == "OUT1": stage_OUT(S, C, 1, C.mixT1_d, C.wout1_d, C.x1_d, C.x3_d)
    elif st == "MOE1": stage_MOE(S, C, 1, C.x3_d)
    elif st == "FIN": stage_FIN(S, C, C.x3_d, C.y_d)
    else: raise ValueError(st)


W1C = 3024
O_KR, O_KRR, O_DQ, O_DK, O_G, O_CQ, O_CKV, O_DV, O_DO = 0, 96, 192, 704, 1216, 1232, 1744, 2000, 2512


def host_prep_P1(od_w_in, w_uq, w_ukv):
    w = np.asarray(od_w_in, np.float32)
    cq, ckv, kr, dq, dk, dv, do, gt = np.split(w, np.cumsum([512, 256, 32, 512, 512, 512, 512])[:], axis=1)
    p32 = np.array([(d + 16 if d < 16 else d - 16) for d in range(32)])
    w1 = np.concatenate([cq[:, 0:64], kr, cq[:, 0:64], kr[:, p32], dq, dk, gt, cq, ckv, dv, do], axis=1)
    assert w1.shape[1] == W1C
    uq = np.asarray(w_uq, np.float32)
    perm = np.arange(768)
    for h in range(8):
        for dd in range(32):
            perm[h * 96 + 64 + dd] = h * 96 + 64 + (dd + 16 if dd < 16 else dd - 16)
    wuq = np.concatenate([uq, uq[:, perm]], axis=1)
    ukv = np.asarray(w_ukv, np.float32).reshape(256, 8, 128)
    wukv = np.concatenate([ukv[:, :, 0:64].reshape(256, 512), ukv[:, :, 64:128].reshape(256, 512)], axis=1)
    return np.ascontiguousarray(w1), np.ascontiguousarray(wuq), np.ascontiguousarray(wukv)


def ropec1():
    inv = np.zeros(128, np.float32)
    sgn = np.zeros(128, np.float32)
    fr = (10000.0 ** (-np.arange(16, dtype=np.float32) * 2.0 / 32)).astype(np.float32)
    for p in range(64, 96):
        d = p - 64
        inv[p] = fr[d % 16]
        sgn[p] = -1.0 if d < 16 else 1.0
    return np.stack([inv, sgn], axis=1).astype(np.float32)


def stage_P1(S, C, x_dram):
    Sq = C.S
    NG = Sq // GT
    S.begin()
    load_consts(S, C)
    ident = C.ident
    cst = S.sb([128, 2], F32, "cst1")
    S.dma("sp", cst[:], C.ropec1_d[:, :], W=[cst])
    gv = S.sb([128, NCH], F32, "gv1")
    S.dma("sp", gv[:], C.norm_mix_d[1, :].rearrange("(c p) -> p c", p=128), W=[gv], allow_slow_non_contiguous=True)
    gq = S.sb([128, 4], F32, "gq")
    S.dma("sp", gq[:], C.mla_nq_d[0, :].rearrange("(c p) -> p c", p=128), W=[gq], allow_slow_non_contiguous=True)
    gkv = S.sb([128, 2], F32, "gkv")
    S.dma("sp", gkv[:], C.mla_nkv_d[0, :].rearrange("(c p) -> p c", p=128), W=[gkv], allow_slow_non_contiguous=True)
    gb4 = S.sb([4, 4], F32, "gb4")
    S.dma("sp", gb4[:], C.gbias_d[:, :], W=[gb4])
    C.rt_ang = S.sb([128, GT], F32, "rt_ang")
    C.rt_ki = S.sb([128, GT], I32, "rt_ki")
    C.rt_kf = S.sb([128, GT], F32, "rt_kf")
    w = S.sb([128, NCH, W1C], BF16, "w1")
    stg = load_weights_bf16(S, C.w1_d, W1C, gv, 0, w)
    wuq = S.sb([128, 4, 1536], BF16, "wuq")
    load_weights_bf16(S, C.wuq_d, 1536, gq, 0, wuq, kchunks=4, stg=stg)
    wukv = S.sb([128, 2, 1024], BF16, "wukv")
    load_weights_bf16(S, C.wukv_d, 1024, gkv, 0, wukv, kchunks=2, stg=stg)

    xt = [S.sb([128, 4, D], F32, "xt")] * 2
    junk = S.sb([128, D], BF16, "junk")
    ssq = S.sb([128, 4], F32, "ssq")
    rstd = S.sb([128, 4], F32, "rstd")
    xn = S.sb([128, 4, D], BF16, "xn")
    xnT = [S.sb([128, NCH, GT], BF16, "xnT") for _ in range(2)]
    pT = [S.ps([128, GT], BF16, "pT") for _ in range(2)]
    pz = [S.ps([128, GT], F32, "pz") for _ in range(2)]
    pr = [S.ps([128, GT], F32, "pr") for _ in range(2)]
    pm = [S.ps([128, GT], F32, "pm") for _ in range(2)]
    posi = S.sb([128, GT], I32, "posi")
    posf = S.sb([128, GT], F32, "posf")
    tmp = S.sb([128, GT], F32, "tmp")
    cosC = S.sb([128, GT], F32, "cosC")
    sinC = S.sb([128, GT], F32, "sinC")
    t1 = [S.sb([128, GT], F32, "t1") for _ in range(2)]
    t2 = [S.sb([128, GT], F32, "t2") for _ in range(2)]
    ofm = [S.sb([128, GT], BF16, "ofm") for _ in range(3)]
    oraw = [S.sb([128, GT], F32, "oraw") for _ in range(2)]
    og4 = [S.sb([4, GT], F32, "og4") for _ in range(2)]
    ovd = [S.sb([128, 4, 129], BF16, "ovd") for _ in range(2)]
    for b in ovd:
        S.memset("pool", b, b[:], 1.0)
    ovc = [S.sb([128, 8, 128], BF16, "ovc") for _ in range(2)]
    for b in ovc:
        S.memset("pool", b, b[:], 0.0)
        S.memset("pool", b, b[:, :, 0:1], 1.0)
    ogo = [S.sb([128, 512], F32, "ogo") for _ in range(2)]
    cn4 = S.sb([128, 4, 768], BF16, "cn4")
    cnT = S.sb([128, 6, GT], BF16, "cnT")
    st2 = [S.sb([128, 4], F32, "st2") for _ in range(2)]
    eps2 = C.epsb
    kf = 0
    for g in range(NG):
        X = xnT[g % 2]
        gs = slice(g * GT, (g + 1) * GT)
        norm_transpose_group(S, C, x_dram, g, xt[0], junk, ssq, rstd, xn, pT, X, ident)
        rope_tables(S, C, C.pos_d, g, posi, posf, [(cst, cst[:, 0:1], cst[:, 1:2], [(cosC, sinC, 1.0)])], tmp, None)
        z, r = pz[kf % 2], pr[kf % 2]
        for c in range(NCH):
            S.mm(z, z[0:96, :], w, w[:, c, O_KR:O_KR + 96], X, X[:, c, :], c == 0, c == NCH - 1)
        for c in range(NCH):
            S.mm(r, r[0:96, :], w, w[:, c, O_KRR:O_KRR + 96], X, X[:, c, :], c == 0, c == NCH - 1)
        a, b2, o = t1[kf % 2], t2[kf % 2], ofm[kf % 3]
        kf += 1
        S.tt("dve", a, a[64:96, :], z, z[64:96, :], cosC, cosC[64:96, :], ALU.mult)
        S.tt("dve", b2, b2[64:96, :], r, r[64:96, :], sinC, sinC[64:96, :], ALU.mult)
        S.tt("pool", o, o[64:96, :], a, a[64:96, :], b2, b2[64:96, :], ALU.add)
        for h in range(8):
            S.dma("sp" if h % 2 == 0 else "act", C.fmk_d[h, 64:96, gs], o[64:96, :], R=[o])
        for blk in range(8):
            z = pz[kf % 2]
            orw = oraw[kf % 2]
            kf += 1
            c0 = O_DQ + blk * 128
            for c in range(NCH):
                S.mm(z, z[:], w, w[:, c, c0:c0 + 128], X, X[:, c, :], c == 0, c == NCH - 1)
            S.copy("act" if blk % 2 == 0 else "dve", orw, orw[:], z, z[:])
            S.dma("sp", C.qkraw_d[blk * 128:(blk + 1) * 128, gs], orw[:], R=[orw])
        for ty in range(4):
            z = pr[ty % 2]
            o4 = og4[ty % 2]
            c0 = O_G + ty * 4
            for c in range(NCH):
                S.mm(z, z[0:4, :], w, w[:, c, c0:c0 + 4], X, X[:, c, :], c == 0, c == NCH - 1)
            S.ts("dve", o4, o4[:], z, z[0:4, :], gb4[:, ty:ty + 1], None, ALU.add, R=[gb4])
            S.dma("sp", C.gatesT_d[ty, :, gs], o4[:], R=[o4])
        for j in range(4):
            tok0 = g * GT + j * 128
            pcq, pckv, pdv, pdo = pm[0], pm[1], pz[j % 2], pr[j % 2]
            for (pp, cc, nn) in ((pcq, O_CQ, 512), (pckv, O_CKV, 256), (pdv, O_DV, 512), (pdo, O_DO, 512)):
                for c in range(NCH):
                    S.mm(pp, pp[:, 0:nn], X, X[:, c, j * 128:(j + 1) * 128], w, w[:, c, cc:cc + nn], c == 0, c == NCH - 1)
            s2 = st2[j % 2]
            S.op("act", lambda e, s2=s2, pcq=pcq: e.activation(out=junk[:, 0:512], in_=pcq[:, 0:512], func=AF.Square, accum_out=s2[:, 0:1]),
                 R=[pcq], W=[junk, s2])
            S.op("act", lambda e, s2=s2, pckv=pckv: e.activation(out=junk[:, 0:256], in_=pckv[:, 0:256], func=AF.Square, accum_out=s2[:, 1:2]),
                 R=[pckv], W=[junk, s2])
            S.op("act", lambda e, s2=s2: e.activation(out=s2[:, 2:3], in_=s2[:, 0:1], func=AF.Sqrt, bias=eps2[:, 0:1], scale=1.0 / 512),
                 R=[s2, eps2], W=[s2])
            S.op("act", lambda e, s2=s2: e.activation(out=s2[:, 3:4], in_=s2[:, 1:2], func=AF.Sqrt, bias=eps2[:, 0:1], scale=1.0 / 256),
                 R=[s2, eps2], W=[s2])
            S.op("dve", lambda e, s2=s2: e.reciprocal(out=s2[:, 2:4], in_=s2[:, 2:4]), R=[s2], W=[s2])
            S.ts("dve", cn4, cn4[:, j, 0:512], pcq, pcq[:, 0:512], s2[:, 2:3], None, ALU.mult, R=[s2])
            S.ts("dve", cn4, cn4[:, j, 512:768], pckv, pckv[:, 0:256], s2[:, 3:4], None, ALU.mult, R=[s2])
            vd, go = ovd[j % 2], ogo[j % 2]
            S.copy("act", vd, vd[:, :, 0:128], pdv, pdv[:].rearrange("p (h d) -> p h d", h=4))
            S.op("act", lambda e, go=go, pdo=pdo: e.activation(out=go[:], in_=pdo[:], func=AF.Sigmoid), R=[pdo], W=[go])
            S.dma("pool", C.vD_d[tok0:tok0 + 128, :], vd[:].rearrange("p h d -> p (h d)"), R=[vd])
            S.dma("pool", C.og_d[tok0:tok0 + 128, :], go[:], R=[go])
        for c in range(6):
            p = pT[c % 2]
            for j in range(4):
                S.tr(p, p[:, j * 128:(j + 1) * 128], cn4, cn4[:, j, c * 128:(c + 1) * 128], ident, ident[:])
            S.copy("act" if c % 2 == 0 else "dve", cnT, cnT[:, c, :], p, p[:])
        for h in range(8):
            z, r = pz[kf % 2], pr[kf % 2]
            a, b2, o = t1[kf % 2], t2[kf % 2], ofm[kf % 3]
            kf += 1
            for c in range(4):
                S.mm(z, z[0:96, :], wuq, wuq[:, c, h * 96:(h + 1) * 96], cnT, cnT[:, c, :], c == 0, c == 3)
            for c in range(4):
                S.mm(r, r[0:96, :], wuq, wuq[:, c, 768 + h * 96:768 + (h + 1) * 96], cnT, cnT[:, c, :], c == 0, c == 3)
            S.copy("act", o, o[0:64, :], z, z[0:64, :])
            S.tt("dve", a, a[64:96, :], z, z[64:96, :], cosC, cosC[64:96, :], ALU.mult)
            S.tt("dve", b2, b2[64:96, :], r, r[64:96, :], sinC, sinC[64:96, :], ALU.mult)
            S.tt("pool", o, o[64:96, :], a, a[64:96, :], b2, b2[64:96, :], ALU.add)
            S.dma("sp", C.fmq_d[h, :, gs], o[0:96, :], R=[o])
        for h in range(8):
            z = pm[h % 2]
            o = ofm[kf % 3]
            kf += 1
            for c in range(2):
                S.mm(z, z[0:64, :], wukv, wukv[:, c, h * 64:(h + 1) * 64], cnT, cnT[:, 4 + c, :], c == 0, c == 1)
            S.copy("act" if h % 2 == 0 else "dve", o, o[0:64, :], z, z[0:64, :])
            S.dma("act", C.fmk_d[h, 0:64, gs], o[0:64, :], R=[o])
        for j in range(4):
            tok0 = g * GT + j * 128
            z = pz[j % 2]
            vc = ovc[j % 2]
            for c in range(2):
                S.mm(z, z[:], cnT, cnT[:, 4 + c, j * 128:(j + 1) * 128], wukv, wukv[:, c, 512:1024], c == 0, c == 1)
            S.copy("dve", vc, vc[:, :, 64:128], z, z[:].rearrange("p (h d) -> p h d", h=8))
            S.dma("pool", C.vC_d[tok0:tok0 + 128, :], vc[:].rearrange("p h d -> p (h d)"), R=[vc])
    S.end()


def stage_C(S, C):
    Sq = C.S
    N = Sq // 128
    NG = Sq // GT
    scale = 96 ** -0.5
    S.begin()
    load_consts(S, C)
    Qh = [S.sb([96, Sq], BF16, "Qh") for _ in range(2)]
    Kh = [S.sb([96, Sq], BF16, "Kh") for _ in range(2)]
    Vh = [S.sb([128, N, 128], BF16, "Vh") for _ in range(2)]
    sq = [S.sb([96, GT], F32, "sqc") for _ in range(2)]
    acc = S.sb([1, 2, GT], F32, "accc")
    mq = S.sb([1, 4], F32, "mqc")
    negM = [S.sb([128, 1], F32, "negMc") for _ in range(2)]
    pn = S.ps([128, GT], F32, "pnc")
    pss = [S.ps([128, 512], F32, "pssc") for _ in range(4)]
    pso = [S.ps([128, 512], F32, "psoc") for _ in range(2)]
    pts = [S.sb([128, 512], BF16, "ptc") for _ in range(4)]
    den = [S.sb([1, 512], F32, "denc") for _ in range(2)]
    rdb = [S.sb([128, 512], F32, "rdbc") for _ in range(2)]
    osb = [S.sb([128, 512], BF16, "osbc") for _ in range(2)]
    ks = 0
    it = 0
    for h in range(8):
        Q, K, V, nM = Qh[h % 2], Kh[h % 2], Vh[h % 2], negM[h % 2]
        for h0 in range(0, Sq, 2048):
            w_ = min(2048, Sq - h0)
            S.dma("sp", Q[:, h0:h0 + w_], C.fmq_d[h, :, h0:h0 + w_], W=[Q])
            S.dma("act", K[:, h0:h0 + w_], C.fmk_d[h, :, h0:h0 + w_], W=[K])
        S.dma("pool", V[:], C.vC_d[:, h * 128:(h + 1) * 128].rearrange("(n p) c -> p n c", p=128), W=[V])
        S.memset("dve", acc, acc[:], 0.0)
        i = 0
        for ai, T in enumerate((Q, K)):
            for g in range(NG):
                s_ = sq[i % 2]
                i += 1
                S.op("act", lambda e, s_=s_, T=T, g=g: e.activation(out=s_[:], in_=T[:, g * GT:(g + 1) * GT], func=AF.Square),
                     R=[T], W=[s_])
                S.mm(pn, pn[0:1, :], C.onesf, C.onesf[0:96, 0:1], s_, s_[:, :], True, True)
                S.tt("dve", acc, acc[:, ai, :], pn, pn[0:1, :], acc, acc[:, ai, :], ALU.max)
        S.op("dve", lambda e: e.tensor_reduce(out=mq[:, 0:2], in_=acc[:], axis=AX.X, op=ALU.max), R=[acc], W=[mq])
        S.tt("dve", mq, mq[:, 2:3], mq, mq[:, 0:1], mq, mq[:, 1:2], ALU.mult)
        S.op("act", lambda e: e.activation(out=mq[:, 3:4], in_=mq[:, 2:3], func=AF.Sqrt), R=[mq], W=[mq])
        S.ts("dve", mq, mq[:, 3:4], mq, mq[:, 3:4], -scale, None, ALU.mult)
        bcast_row_to_parts(S, C, mq, mq[0:1, 3:4], nM, nM[:], pn, 1)
        for g in range(NG):
            po = pso[it % 2]
            for m in range(N):
                ps_ = pss[ks % 4]
                pt = pts[ks % 4]
                ks += 1
                S.mm(ps_, ps_[:], K, K[:, m * 128:(m + 1) * 128], Q, Q[:, g * GT:(g + 1) * GT], True, True)
                S.op("act", lambda e, pt=pt, ps_=ps_, nM=nM: e.activation(out=pt[:], in_=ps_[:], func=AF.Exp, bias=nM[:, 0:1], scale=scale),
                     R=[ps_, nM], W=[pt])
                S.mm(po, po[:, :], V, V[:, m, :], pt, pt[:], m == 0, m == N - 1)
            dn, rb, ob = den[it % 2], rdb[it % 2], osb[it % 2]
            S.op("dve", lambda e, dn=dn, po=po: e.reciprocal(out=dn[0:1, :], in_=po[0:1, :]), R=[po], W=[dn])
            pb = pss[ks % 4]
            ks += 1
            S.mm(pb, pb[:, :], C.onesf, C.onesf[0:1, :], dn, dn[0:1, :], True, True)
            S.copy("act", rb, rb[64:128, :], pb, pb[64:128, :])
            S.tt("dve", ob, ob[64:128, :], po, po[64:128, :], rb, rb[64:128, :], ALU.mult)
            S.dma("sp", C.mixT1_d[h * 64:(h + 1) * 64, g * GT:(g + 1) * GT], ob[64:128, :], R=[ob])
            it += 1
    S.end()


def _v3(ap):
    return ap.rearrange("p (n l) -> p n l", l=128)


def logstep3(S, bufs, cur, op, suffix, L=128):
    s = 1
    while s < L:
        a, b = bufs[cur], bufs[1 - cur]
        a3, b3 = _v3(a[:]), _v3(b[:])
        if not suffix:
            S.copy("pool", b, b3[:, :, 0:s], a, a3[:, :, 0:s])
            S.tt("dve", b, b3[:, :, s:L], a, a3[:, :, s:L], a, a3[:, :, 0:L - s], op)
        else:
            S.copy("pool", b, b3[:, :, L - s:L], a, a3[:, :, L - s:L])
            S.tt("dve", b, b3[:, :, 0:L - s], a, a3[:, :, 0:L - s], a, a3[:, :, s:L], op)
        cur = 1 - cur
        s *= 2
    return cur


def logstep2(S, bufs, cur, op, suffix, N):
    s = 1
    while s < N:
        a, b = bufs[cur], bufs[1 - cur]
        if not suffix:
            S.copy("pool", b, b[:, 0:s], a, a[:, 0:s])
            S.tt("dve", b, b[:, s:N], a, a[:, s:N], a, a[:, 0:N - s], op)
        else:
            S.copy("pool", b, b[:, N - s:N], a, a[:, N - s:N])
            S.tt("dve", b, b[:, 0:N - s], a, a[:, 0:N - s], a, a[:, s:N], op)
        cur = 1 - cur
        s *= 2
    return cur


def stage_D(S, C):
    Sq = C.S
    N = Sq // 128
    L = 128
    NEGB = -1.0e30
    S.begin()
    load_consts(S, C)
    cw = S.sb([128, 8, 5], F32, "cw")
    S.dma("sp", cw[:].rearrange("p b w -> p (b w)"), C.conv_d[:, :], W=[cw])
    raw = [S.sb([128, Sq], F32, "raw") for _ in range(2)]
    acc = S.sb([128, Sq], F32, "cacc")
    sil = S.sb([128, Sq], F32, "sil")
    ocv = [S.sb([128, Sq], BF16, "ocv") for _ in range(2)]
    for blk in range(8):
        R, O = raw[blk % 2], ocv[blk % 2]
        for h0 in range(0, Sq, 2048):
            w_ = min(2048, Sq - h0)
            S.dma("sp" if (h0 // 2048) % 2 == 0 else "act", R[:, h0:h0 + w_], C.qkraw_d[blk * 128:(blk + 1) * 128, h0:h0 + w_], W=[R])
        S.ts("dve", acc, acc[:], R, R[:], cw[:, blk, 2:3], None, ALU.mult, R=[cw])
        for wi in (0, 1, 3, 4):
            sh = wi - 2
            if sh > 0:
                S.stt("dve", acc, acc[:, 0:Sq - sh], R, R[:, sh:Sq], cw[:, blk, wi:wi + 1], acc, acc[:, 0:Sq - sh], ALU.mult, ALU.add, R=[cw])
            else:
                S.stt("dve", acc, acc[:, -sh:Sq], R, R[:, 0:Sq + sh], cw[:, blk, wi:wi + 1], acc, acc[:, -sh:Sq], ALU.mult, ALU.add, R=[cw])
        if blk < 4:
            S.op("act", lambda e, O=O: e.activation(out=O[:], in_=acc[:], func=AF.Silu), R=[acc], W=[O])
        else:
            S.op("act", lambda e: e.activation(out=sil[:], in_=acc[:], func=AF.Silu), R=[acc], W=[sil])
            S.ts("pool", O, O[:], sil, sil[:], 128 ** -0.5, None, ALU.mult)
        for h0 in range(0, Sq, 2048):
            w_ = min(2048, Sq - h0)
            S.dma("sp", C.qkc_d[blk, :, h0:h0 + w_], O[:, h0:h0 + w_], R=[O])
    S.end()
    S.begin()
    load_consts(S, C)
    TPG = 256
    G = Sq // TPG
    P_ = 4 * G
    J = TPG // L

    def fold(ap2):
        return ap2.rearrange("h (g t) -> (h g) t", t=TPG)

    def foldn(ap2):
        return ap2.rearrange("h (g j) -> (h g) j", j=J)

    def bc(t):
        return t[:].unsqueeze(2).to_broadcast([P_, J, L])
    gi = S.sb([P_, TPG], F32, "gi")
    gf = S.sb([P_, TPG], F32, "gf")
    bb = [S.sb([P_, TPG], F32, "bb") for _ in range(2)]
    cc = S.sb([P_, TPG], F32, "cc")
    cm = [S.sb([P_, TPG], F32, "cm") for _ in range(2)]
    aa = S.sb([P_, TPG], F32, "aa")
    mmx = S.sb([P_, TPG], F32, "mmx")
    ov = [S.sb([P_, TPG], F32, "ov") for _ in range(3)]
    g_ = S.sb([P_, J], F32, "g_")
    amax = S.sb([P_, J], F32, "amax")
    mPf = S.sb([P_, J], F32, "mPf")
    g4 = S.sb([4, N], F32, "g4")
    am4 = S.sb([4, N], F32, "am4")
    GG = [S.sb([4, N], F32, "GG") for _ in range(2)]
    PP = [S.sb([4, N], F32, "PP") for _ in range(2)]
    mN = S.sb([4, N], F32, "mN")
    mP = S.sb([4, N], F32, "mP")
    spc = [S.sb([4, N], F32, "spc") for _ in range(2)]
    kv = 0
    for dirn in (0, 1):
        suffix = (dirn == 1)
        dch = Buf(None, "dch%d" % dirn)
        dmp = Buf(None, "dmp%d" % dirn)
        S.dma("sp", gi[:], fold(C.gatesT_d[2 * dirn, :, :]), W=[gi])
        S.dma("act", gf[:], fold(C.gatesT_d[2 * dirn + 1, :, :]), W=[gf])
        S.op("act", lambda e: e.activation(out=bb[0][:], in_=gf[:], func=AF.Exp, scale=-1.0), R=[gf], W=[bb[0]])
        S.ts("dve", bb[0], bb[0][:], bb[0], bb[0][:], 1.0, None, ALU.add)
        S.op("act", lambda e: e.activation(out=bb[0][:], in_=bb[0][:], func=AF.Ln), R=[bb[0]], W=[bb[0]])
        S.ts("dve", bb[0], bb[0][:], bb[0], bb[0][:], -1.0, None, ALU.mult)
        cb = logstep3(S, bb, 0, ALU.add, suffix)
        B_ = bb[cb]
        B3 = _v3(B_[:])
        gcol = (L - 1) if not suffix else 0
        S.copy("dve", g_, g_[:], B_, B3[:, :, gcol])
        S.tt("dve", cc, cc[:], gi, gi[:], B_, B_[:], ALU.subtract)
        S.tt("dve", aa, _v3(aa[:]), cc, _v3(cc[:]), g_, bc(g_), ALU.add)
        S.op("dve", lambda e: e.tensor_reduce(out=amax[:], in_=_v3(aa[:]), axis=AX.X, op=ALU.max), R=[aa], W=[amax])
        S.dma("sp", foldn(C.chunk_d[dirn * 2 + 0, :, :]), g_[:], R=[g_], W=[dch])
        S.dma("sp", foldn(C.chunk_d[dirn * 2 + 1, :, :]), amax[:], R=[amax], W=[dch])
        wv = ov[kv % 3]
        kv += 1
        S.tt("dve", aa, _v3(aa[:]), aa, _v3(aa[:]), amax, bc(amax), ALU.subtract)
        S.op("act", lambda e, wv=wv: e.activation(out=wv[:], in_=aa[:], func=AF.Exp), R=[aa], W=[wv])
        S.dma("sp", fold(C.vec_d[dirn * 5 + 0, :, :]), wv[:], R=[wv])
        e1 = ov[kv % 3]
        kv += 1
        S.op("act", lambda e, e1=e1: e.activation(out=e1[:], in_=cc[:], func=AF.Exp), R=[cc], W=[e1])
        S.dma("sp", fold(C.vec_d[dirn * 5 + 1, :, :]), e1[:], R=[e1])
        S.copy("pool", cm[0], cm[0][:], cc, cc[:])
        ci = logstep3(S, cm, 0, ALU.max, suffix)
        if suffix:
            a3, b3 = _v3(cm[ci][:]), _v3(cm[1 - ci][:])
            S.copy("pool", cm[1 - ci], b3[:, :, 0:L - 1], cm[ci], a3[:, :, 1:L])
            S.memset("pool", cm[1 - ci], b3[:, :, L - 1:L], NEGB)
            ci = 1 - ci
        CM = cm[ci]
        S.dma("sp", g4[:], C.chunk_d[dirn * 2 + 0, :, :], R=[dch], W=[g4])
        S.dma("sp", am4[:], C.chunk_d[dirn * 2 + 1, :, :], R=[dch], W=[am4])
        S.copy("pool", GG[0], GG[0][:], g4, g4[:])
        gi_ = logstep2(S, GG, 0, ALU.add, suffix, N)
        G_ = GG[gi_]
        S.tt("dve", PP[0], PP[0][:], am4, am4[:], G_, G_[:], ALU.subtract)
        pi_ = logstep2(S, PP, 0, ALU.max, suffix, N)
        S.ts("dve", mN, mN[:], PP[pi_], PP[pi_][:], 0.0, None, ALU.max)
        S.tt("dve", mN, mN[:], mN, mN[:], G_, G_[:], ALU.add)
        S.memset("pool", mP, mP[:], 0.0)
        if not suffix:
            S.copy("pool", mP, mP[:, 1:N], mN, mN[:, 0:N - 1])
        else:
            S.copy("pool", mP, mP[:, 0:N - 1], mN, mN[:, 1:N])
        S.tt("dve", spc[0], spc[0][:], g4, g4[:], mP, mP[:], ALU.add)
        S.tt("dve", spc[0], spc[0][:], spc[0], spc[0][:], mN, mN[:], ALU.subtract)
        S.op("act", lambda e: e.activation(out=spc[0][:], in_=spc[0][:], func=AF.Exp), R=[spc[0]], W=[spc[0]])
        S.tt("dve", spc[1], spc[1][:], am4, am4[:], mN, mN[:], ALU.subtract)
        S.op("act", lambda e: e.activation(out=spc[1][:], in_=spc[1][:], func=AF.Exp), R=[spc[1]], W=[spc[1]])
        S.dma("sp", C.vec2_d[dirn * 2 + 0, :, :], spc[0][:], R=[spc[0]])
        S.dma("sp", C.vec2_d[dirn * 2 + 1, :, :], spc[1][:], R=[spc[1]])
        S.dma("sp", C.mp_d[dirn, :, :], mP[:], R=[mP], W=[dmp])
        S.dma("sp", mPf[:], foldn(C.mp_d[dirn, :, :]), R=[dmp], W=[mPf])
        S.tt("dve", mmx, _v3(mmx[:]), CM, _v3(CM[:]), mPf, bc(mPf), ALU.max)
        e2 = ov[kv % 3]
        kv += 1
        S.op("act", lambda e, e2=e2: e.activation(out=e2[:], in_=mmx[:], func=AF.Exp, scale=-1.0), R=[mmx], W=[e2])
        S.dma("sp", fold(C.vec_d[dirn * 5 + 2, :, :]), e2[:], R=[e2])
        iw = ov[kv % 3]
        kv += 1
        S.tt("dve", iw, _v3(iw[:]), mmx, _v3(mmx[:]), mPf, bc(mPf), ALU.subtract)
        S.op("act", lambda e, iw=iw: e.activation(out=iw[:], in_=iw[:], func=AF.Exp, scale=-1.0), R=[iw], W=[iw])
        S.dma("sp", fold(C.vec_d[dirn * 5 + 3, :, :]), iw[:], R=[iw])
        em = ov[kv % 3]
        kv += 1
        S.tt("dve", em, em[:], mmx, mmx[:], B_, B_[:], ALU.add)
        S.op("act", lambda e, em=em: e.activation(out=em[:], in_=em[:], func=AF.Exp, scale=-1.0), R=[em], W=[em])
        S.dma("sp", fold(C.vec_d[dirn * 5 + 4, :, :]), em[:], R=[em])
    S.end()
    S.begin()
    load_consts(S, C)
    identf = S.sb([128, 128], F32, "identfD")
    S.dma("sp", identf[:], C.identf_d[:, :], W=[identf])
    mk2 = S.sb([128, 2, 128], F32, "mk2")
    S.dma("sp", mk2[:], C.maskD_d[:, :, :], W=[mk2])
    VW = min(2048, Sq)
    VT = [S.sb([40, VW], F32, "VT") for _ in range(2)]
    tv = S.sb([128, N, 40], F32, "tv")
    pu = [S.ps([128, 129], F32, "pud") for _ in range(2)]
    for h0 in range(0, Sq, VW):
        V_ = VT[(h0 // VW) % 2]
        S.dma("sp", V_[:], C.vec_d.rearrange("a h s -> (a h) s")[:, h0:h0 + VW], W=[V_])
        for nn in range(VW // 128):
            n = h0 // 128 + nn
            p_ = pu[n % 2]
            S.tr(p_, p_[:, 0:40], V_, V_[:, nn * 128:(nn + 1) * 128], identf, identf[0:40, 0:40])
            S.copy("act" if n % 2 == 0 else "dve", tv, tv[:, n, :], p_, p_[:, 0:40])
    spsc = S.sb([128, 16 * N], F32, "spsc")
    S.dma("sp", spsc[:], C.vec2_d.rearrange("a h n -> (a h n)").partition_broadcast(128), W=[spsc])

    def vcol(dirn, k, h):
        return (dirn * 5 + k) * 4 + h

    qT = S.sb([128, Sq], BF16, "qTd")
    kT = S.sb([128, Sq], BF16, "kTd")
    Kt = S.sb([128, N, 128], BF16, "Ktd")
    Va = S.sb([128, N, 129], BF16, "Vad")
    Og4 = [S.sb([128, 4, 128], F32, "Ogd") for _ in range(2)]
    Cst = S.sb([128, 129], F32, "Cst")
    Cp = [S.sb([128, N, 129], BF16, "Cp%d" % d) for d in range(2)]
    pk = [S.ps([128, 128], BF16, "pkd") for _ in range(2)]
    pS = S.ps([128, 128], F32, "pSd")
    pA = [S.ps([128, 2, 129], F32, "pAd") for _ in range(2)]
    vw = [S.sb([128, 129], BF16, "vw") for _ in range(2)]
    tU = [S.sb([128, 129], F32, "tU") for _ in range(2)]
    sF = [S.sb([128, 128], BF16, "sF") for _ in range(2)]
    sB = [S.sb([128, 128], BF16, "sB") for _ in range(2)]
    vF = [S.sb([128, 129], BF16, "vF") for _ in range(2)]
    vB = [S.sb([128, 129], BF16, "vB") for _ in range(2)]
    t1 = [S.sb([128, 129], F32, "t1d") for _ in range(2)]
    tot = [S.sb([128, 129], F32, "totd") for _ in range(2)]
    dn = [S.sb([128, 2], F32, "dnd") for _ in range(2)]
    hacc = [S.sb([128, 128], F32, "hacc") for _ in range(2)]
    ob = [S.sb([128, 128], BF16, "obd") for _ in range(2)]
    oT = [S.sb([128, 512], BF16, "oTd") for _ in range(2)]
    for h in range(4):
        for h0 in range(0, Sq, 2048):
            w_ = min(2048, Sq - h0)
            S.dma("sp", qT[:, h0:h0 + w_], C.qkc_d[h, :, h0:h0 + w_], W=[qT])
            S.dma("act", kT[:, h0:h0 + w_], C.qkc_d[4 + h, :, h0:h0 + w_], W=[kT])
        S.dma("pool", Va[:], C.vD_d[:, h * 129:(h + 1) * 129].rearrange("(n p) c -> p n c", p=128), W=[Va])
        for n in range(N):
            p_ = pk[n % 2]
            S.tr(p_, p_[:], kT, kT[:, n * 128:(n + 1) * 128], C.ident, C.ident[:])
            S.copy("act" if n % 2 == 0 else "dve", Kt, Kt[:, n, :], p_, p_[:])
        for dirn in (0, 1):
            S.memset("dve", Cst, Cst[:], 0.0)
            order = range(N) if dirn == 0 else range(N - 1, -1, -1)
            for n in order:
                vw_, pu_, tU_ = vw[n % 2], pu[n % 2], tU[n % 2]
                cw_ = vcol(dirn, 0, h)
                S.ts("pool", vw_, vw_[:], Va, Va[:, n, :], tv[:, n, cw_:cw_ + 1], None, ALU.mult, R=[tv])
                S.mm(pu_, pu_[:], Kt, Kt[:, n, :], vw_, vw_[:], True, True)
                S.copy("act", Cp[dirn], Cp[dirn][:, n, :], Cst, Cst[:])
                isp = ((dirn * 2 + 0) * 4 + h) * N + n
                isc = ((dirn * 2 + 1) * 4 + h) * N + n
                S.ts("dve", tU_, tU_[:], pu_, pu_[:], spsc[:, isc:isc + 1], None, ALU.mult, R=[spsc])
                S.stt("dve", Cst, Cst[:], Cst, Cst[:], spsc[:, isp:isp + 1], tU_, tU_[:], ALU.mult, ALU.add, R=[spsc])
        for n in range(N):
            tok = slice(n * 128, (n + 1) * 128)
            i2 = n % 2
            Og = Og4[(n // 4) % 2]
            if n % 4 == 0:
                S.dma("pool", Og[:], C.og_d[n * 128:(n + 4) * 128, h * 128:(h + 1) * 128].rearrange("(n p) c -> p n c", p=128), W=[Og])
            S.mm(pS, pS[:], kT, kT[:, tok], qT, qT[:, tok], True, True)
            S.tt("dve", sF[i2], sF[i2][:], pS, pS[:], mk2, mk2[:, 0, :], ALU.mult)
            S.tt("dve", sB[i2], sB[i2][:], pS, pS[:], mk2, mk2[:, 1, :], ALU.mult)
            ha = hacc[i2]
            for dirn in (0, 1):
                s_ = (sF if dirn == 0 else sB)[i2]
                v_ = (vF if dirn == 0 else vB)[i2]
                pa = pA[dirn]
                c1, c2, c3, c4 = vcol(dirn, 1, h), vcol(dirn, 2, h), vcol(dirn, 3, h), vcol(dirn, 4, h)
                S.ts("pool", v_, v_[:], Va, Va[:, n, :], tv[:, n, c1:c1 + 1], None, ALU.mult, R=[tv])
                S.mm(pa, pa[:, 0, :], s_, s_[:], v_, v_[:], True, True)
                S.mm(pa, pa[:, 1, :], qT, qT[:, tok], Cp[dirn], Cp[dirn][:, n, :], True, True)
                t_, T_, d_ = t1[dirn], tot[dirn], dn[dirn]
                S.ts("dve", t_, t_[:], pa, pa[:, 1, :], tv[:, n, c3:c3 + 1], None, ALU.mult, R=[tv])
                S.stt("dve", T_, T_[:], pa, pa[:, 0, :], tv[:, n, c2:c2 + 1], t_, t_[:], ALU.mult, ALU.add, R=[tv])
                S.stt("dve", d_, d_[:, 0:1], T_, T_[:, 128:129], -1.0, T_, T_[:, 128:129], ALU.mult, ALU.max)
                S.tt("dve", d_, d_[:, 0:1], d_, d_[:, 0:1], tv, tv[:, n, c4:c4 + 1], ALU.max)
                S.op("dve", lambda e, d_=d_: e.reciprocal(out=d_[:, 1:2], in_=d_[:, 0:1]), R=[d_], W=[d_])
                if dirn == 0:
                    S.ts("dve", ha, ha[:], T_, T_[:, 0:128], d_[:, 1:2], None, ALU.mult, R=[d_])
                else:
                    S.stt("dve", ha, ha[:], T_, T_[:, 0:128], d_[:, 1:2], ha, ha[:], ALU.mult, ALU.add, R=[d_])
            o_ = ob[i2]
            S.tt("pool", o_, o_[:], ha, ha[:], Og, Og[:, n % 4, :], ALU.mult)
            pt_ = pk[i2]
            S.tr(pt_, pt_[:], o_, o_[:], C.ident, C.ident[:])
            oT_ = oT[(n // 4) % 2]
            S.copy("act", oT_, oT_[:, (n % 4) * 128:(n % 4 + 1) * 128], pt_, pt_[:])
            if n % 4 == 3:
                S.dma("sp", C.mixT1_d[512 + h * 128:512 + (h + 1) * 128, (n - 3) * 128:(n + 1) * 128], oT_[:], R=[oT_])
    S.end()


STAGES = ("P0", "A", "B", "OUT0", "MOE0", "P1", "C", "D", "OUT1", "MOE1", "FIN")


def build_program(Sq):
    nc = bass.Bass("TRN2", target_bir_lowering=False)
    C = Ctx()
    declare(nc, C, Sq, debug=False)
    S = Sched(nc)
    for st in STAGES:
        run_stage(S, C, st)
    S.close()
    return nc


def kernel(**inputs):
    from concourse.bass_utils import run_bass_kernel_spmd
    inp = {k: np.asarray(v) for k, v in inputs.items()}
    B, Sq, _ = inp["x"].shape
    nc = build_program(Sq)
    shared = host_inputs(inp, 0)
    in_maps = []
    for b in range(B):
        d = dict(shared)
        d["x"] = np.ascontiguousarray(inp["x"][b], dtype=np.float32)
        d["pos"] = np.ascontiguousarray(inp["positions"][b]).astype(np.int32)
        in_maps.append(d)
    res = run_bass_kernel_spmd(nc, in_maps, core_ids=list(range(B)))
    return np.stack([np.asarray(r["y"], dtype=np.float32) for r in res.results], axis=0)
```

```python
import numpy as np
from contextlib import ExitStack
import concourse.bass as bass
import concourse.mybir as mybir

F32 = mybir.dt.float32
BF16 = mybir.dt.bfloat16
I32 = mybir.dt.int32
AF = mybir.ActivationFunctionType
ALU = mybir.AluOpType
AX = mybir.AxisListType


class Buf:
    __slots__ = ("t", "w", "r", "dsem", "name")

    def __init__(self, t, name=""):
        self.t = t
        self.w = None
        self.r = []
        self.dsem = None
        self.name = name

    def __getitem__(self, k):
        return self.t[k]


class Eng:
    def __init__(self, name, eng, sem, inorder=False):
        self.name, self.eng, self.sem = name, eng, sem
        self.count = 0
        self.seen = {}
        self.ops = []
        self.inorder = inorder


class Sched:
    def __init__(self, nc):
        self.nc = nc
        self.es = ExitStack()
        self.E = {}
        for name, eng, ino in (("pe", nc.tensor, True), ("act", nc.scalar, False),
                               ("dve", nc.vector, False), ("pool", nc.gpsimd, False),
                               ("sp", nc.sync, False)):
            sem = self.es.enter_context(nc.semaphore("e_" + name))
            self.E[name] = Eng(name, eng, sem, ino)
        self.dsems = []
        self.free_ds = {"hw": [], "sw": []}
        self.stage_es = None
        self.stage_ds = []
        self.nbuf = 0

    def _get_dsem(self, kind):
        if self.free_ds[kind]:
            i = self.free_ds[kind].pop()
        else:
            sem = self.es.enter_context(self.nc.semaphore("d%d" % len(self.dsems)))
            self.dsems.append([sem, 0])
            i = len(self.dsems) - 1
        self.stage_ds.append((kind, i))
        return i

    def begin(self):
        self.stage_es = ExitStack()
        self.stage_ds = []

    def sb(self, shape, dt, name=None):
        self.nbuf += 1
        name = (name or "b") + "_%d" % self.nbuf
        t = self.stage_es.enter_context(self.nc.sbuf_tensor(name, list(shape), dt))
        return Buf(t, name)

    def ps(self, shape, dt=F32, name=None):
        self.nbuf += 1
        name = (name or "p") + "_%d" % self.nbuf
        t = self.stage_es.enter_context(self.nc.psum_tensor(name, list(shape), dt))
        return Buf(t, name)

    def end(self):
        self.barrier()
        with self.nc.Block() as block:
            for name, sect in (("pe", block.tensor), ("act", block.scalar), ("dve", block.vector),
                               ("pool", block.gpsimd), ("sp", block.sync)):
                E = self.E[name]
                ops = E.ops
                E.ops = []

                def body(eng, ops=ops):
                    for waits, fn, inc in ops:
                        for (sem, val) in waits:
                            eng.wait_ge(sem, val)
                        if fn is not None:
                            ins = fn(eng)
                            if inc is not None:
                                ins.then_inc(inc[0], inc[1])
                sect(body)
        for kind, i in self.stage_ds:
            self.free_ds[kind].append(i)
        self.stage_ds = []
        self.stage_es.close()
        self.stage_es = None

    def barrier(self):
        toks = [(E.sem, E.count) for E in self.E.values() if E.count > 0]
        toks += [(s, v) for (s, v) in self.dsems if v > 0]
        for E in self.E.values():
            w = self._filter(E, toks, barrier=True)
            if w:
                E.ops.append((w, None, None))

    def _filter(self, E, toks, barrier=False):
        best = {}
        for (sem, val) in toks:
            if sem is E.sem and (E.inorder or barrier):
                continue
            k = id(sem)
            if E.seen.get(k, 0) >= val:
                continue
            if k not in best or best[k][1] < val:
                best[k] = (sem, val)
        out = []
        for k, (sem, val) in best.items():
            E.seen[k] = val
            out.append((sem, val))
        return out

    def _deps(self, R, W, skip_sem=None):
        toks = []
        for b in R:
            if b.w is not None:
                toks.append(b.w)
        for b in W:
            if b.w is not None and not (skip_sem is not None and b.w[0] is skip_sem):
                toks.append(b.w)
            toks.extend(b.r)
        return toks

    def op(self, ename, fn, R=(), W=()):
        E = self.E[ename]
        waits = self._filter(E, self._deps(R, W))
        E.count += 1
        tok = (E.sem, E.count)
        E.ops.append((waits, fn, (E.sem, 1)))
        for b in R:
            b.r.append(tok)
        for b in W:
            b.w = tok
            b.r = []
        return tok

    def dma(self, qname, out, in_, R=(), W=(), sem_buf=None, fn=None, extra=(), **kw):
        E = self.E[qname]
        sb = sem_buf if sem_buf is not None else (W[0] if W else R[0])
        kind = "sw" if qname == "pool" else "hw"
        if sb.dsem is None:
            sb.dsem = {}
        if kind not in sb.dsem:
            sb.dsem[kind] = self._get_dsem(kind)
        ds = self.dsems[sb.dsem[kind]]
        waits = self._filter(E, self._deps(R, W, skip_sem=ds[0]) + list(extra))
        ds[1] += 16
        tok = (ds[0], ds[1])
        if fn is None:
            def fn(eng, out=out, in_=in_, kw=kw):
                return eng.dma_start(out=out, in_=in_, **kw)
        E.ops.append((waits, fn, (ds[0], 16)))
        for b in R:
            b.r.append(tok)
        for b in W:
            b.w = tok
            b.r = []
        return tok

    def reg(self, ename, value):
        holder = {}

        def fn(eng):
            holder["r"] = eng.alloc_register("rg%d" % id(holder))
            return eng.reg_mov(holder["r"], value)
        self.E[ename].ops.append(([], fn, None))
        return holder

    def mm(self, out_b, out_ap, lhsT_b, lhsT, rhs_b, rhs, start, stop):
        self.op("pe", lambda e: e.matmul(out_ap, lhsT, rhs, start=start, stop=stop),
                R=[lhsT_b, rhs_b], W=[out_b])

    def tr(self, out_b, out_ap, in_b, in_ap, ident_b, ident_ap):
        self.op("pe", lambda e: e.transpose(out_ap, in_ap, ident_ap), R=[in_b, ident_b], W=[out_b])

    def act(self, out_b, out_ap, in_b, in_ap, func, R=(), eng="act", **kw):
        self.op(eng, lambda e: e.activation(out=out_ap, in_=in_ap, func=func, **kw),
                R=[in_b] + list(R), W=[out_b] + ([kw["accum_b"]] if "accum_b" in kw else []))

    def tt(self, eng, out_b, out_ap, a_b, a_ap, b_b, b_ap, op):
        self.op(eng, lambda e: e.tensor_tensor(out=out_ap, in0=a_ap, in1=b_ap, op=op),
                R=[a_b, b_b], W=[out_b])

    def ts(self, eng, out_b, out_ap, a_b, a_ap, s1, s2, op0, op1=None, R=()):
        if op1 is None:
            f = lambda e: e.tensor_scalar(out=out_ap, in0=a_ap, scalar1=s1, scalar2=None, op0=op0)
        else:
            f = lambda e: e.tensor_scalar(out=out_ap, in0=a_ap, scalar1=s1, scalar2=s2, op0=op0, op1=op1)
        self.op(eng, f, R=[a_b] + list(R), W=[out_b])

    def stt(self, eng, out_b, out_ap, a_b, a_ap, scalar, b_b, b_ap, op0, op1, R=()):
        self.op(eng, lambda e: e.scalar_tensor_tensor(out=out_ap, in0=a_ap, scalar=scalar, in1=b_ap,
                                                     op0=op0, op1=op1),
                R=[a_b, b_b] + list(R), W=[out_b])

    def copy(self, eng, out_b, out_ap, in_b, in_ap):
        if eng == "act":
            self.op(eng, lambda e: e.copy(out=out_ap, in_=in_ap), R=[in_b], W=[out_b])
        else:
            self.op(eng, lambda e: e.tensor_copy(out=out_ap, in_=in_ap), R=[in_b], W=[out_b])

    def memset(self, eng, out_b, out_ap, val):
        self.op(eng, lambda e: e.memset(out_ap, val), W=[out_b])

    def close(self):
        self.es.close()


import math
import numpy as np

D = 1024
NCH = 8
GT = 512
RMS_EPS = 1e-6
PI = math.pi


def rot_perm(n_heads, hd, rope_dim):
    half = rope_dim // 2
    perm = np.arange(n_heads * hd)
    for h in range(n_heads):
        for d in range(rope_dim):
            perm[h * hd + d] = h * hd + (d + half if d < half else d - half)
    return perm


def rope_consts(hd, rope_dim, theta, n=128):
    half = rope_dim // 2
    inv = np.zeros(n, np.float32)
    sgn = np.zeros(n, np.float32)
    fr = (theta ** (-np.arange(half, dtype=np.float32) * 2.0 / rope_dim)).astype(np.float32)
    for p in range(n):
        d = p % hd
        if d < rope_dim:
            inv[p] = fr[d % half]
            sgn[p] = -1.0 if d < half else 1.0
    return inv, sgn


class Ctx:
    pass


def load_weights_bf16(S, wdram, ncols, gvec_b, gcol, wsb, col0=0, kchunks=NCH, scale_cols=None, stg=None):
    CW = 1024
    if stg is None:
        stg = [S.sb([128, CW], F32, "wstg") for _ in range(2)]
    k = 0
    for c in range(kchunks):
        for c0 in range(0, ncols, CW):
            cw = min(CW, ncols - c0)
            st = stg[k % 2]
            S.dma("sp" if k % 2 == 0 else "act", st[:, 0:cw], wdram[c * 128:(c + 1) * 128, c0:c0 + cw], W=[st])
            eng = "dve" if k % 2 == 0 else "pool"
            if gvec_b is not None:
                S.ts(eng, wsb, wsb[:, c, col0 + c0:col0 + c0 + cw], st, st[:, 0:cw],
                     gvec_b[:, gcol + c:gcol + c + 1], None, ALU.mult, R=[gvec_b])
            else:
                S.copy(eng, wsb, wsb[:, c, col0 + c0:col0 + c0 + cw], st, st[:, 0:cw])
            k += 1
    return stg


def norm_transpose_group(S, C, x_dram, g, xt, junk, ssq, rstd, xn, pT, xnT, ident):
    S.dma("sp", xt[:], x_dram[g * GT:(g + 1) * GT, :].rearrange("(j p) d -> p j d", p=128), W=[xt])
    for j in range(4):
        S.op("act", lambda e, j=j: e.activation(out=junk[:], in_=xt[:, j, :], func=AF.Square,
                                                 accum_out=ssq[:, j:j + 1]),
             R=[xt], W=[junk, ssq])
    S.op("act", lambda e: e.activation(out=rstd[:], in_=ssq[:], func=AF.Sqrt, bias=C.epsb[:, 0:1], scale=1.0 / D),
         R=[ssq, C.epsb], W=[rstd])
    S.op("dve", lambda e: e.reciprocal(out=rstd[:], in_=rstd[:]), R=[rstd], W=[rstd])
    for j in range(4):
        if j % 2 == 0:
            S.ts("dve", xn, xn[:, j, :], xt, xt[:, j, :], rstd[:, j:j + 1], None, ALU.mult, R=[rstd])
        else:
            S.op("act", lambda e, j=j: e.activation(out=xn[:, j, :], in_=xt[:, j, :], func=AF.Copy, scale=rstd[:, j:j + 1]),
                 R=[xt, rstd], W=[xn])
    for c in range(NCH):
        p = pT[c % 2]
        for j in range(4):
            S.tr(p, p[:, j * 128:(j + 1) * 128], xn, xn[:, j, c * 128:(c + 1) * 128], ident, ident[:])
        S.copy("act" if c % 2 == 0 else "dve", xnT, xnT[:, c, :], p, p[:])


def rope_tables(S, C, pos_dram, g, posi, posf, specs, tmp, bias_negpi):
    ang, ki, kf = C.rt_ang, C.rt_ki, C.rt_kf
    S.dma("act", posi[:], pos_dram[g * GT:(g + 1) * GT].partition_broadcast(128), W=[posi])
    S.copy("dve", posf, posf[:], posi, posi[:])
    for (cb, invf, sgn, outs) in specs:
        S.ts("dve", ang, ang[:], posf, posf[:], invf, None, ALU.mult, R=[cb])
        for which in (0, 1):
            if which == 1:
                S.ts("dve", ang, ang[:], ang, ang[:], PI / 2, None, ALU.add)
            S.ts("dve", tmp, tmp[:], ang, ang[:], 1.0 / (2 * PI), None, ALU.mult)
            S.copy("dve", ki, ki[:], tmp, tmp[:])
            S.copy("dve", kf, kf[:], ki, ki[:])
            S.stt("dve", tmp, tmp[:], kf, kf[:], -2 * PI, ang, ang[:], ALU.mult, ALU.add)
            S.ts("dve", tmp, tmp[:], tmp, tmp[:], -PI, PI, ALU.max, ALU.min)
            S.op("act", lambda e: e.activation(out=kf[:], in_=tmp[:], func=AF.Sin), R=[tmp], W=[kf])
            for (cosb, sinb, scale) in outs:
                if which == 0:
                    S.ts("dve", sinb, sinb[:], kf, kf[:], sgn, scale, ALU.mult, ALU.mult, R=[cb])
                else:
                    S.ts("dve", cosb, cosb[:], kf, kf[:], scale, None, ALU.mult)


def stage_P0(S, C, x_dram):
    nc = S.nc
    Sq = C.S
    NG = Sq // GT
    S.begin()
    ident = S.sb([128, 128], BF16, "ident")
    S.dma("sp", ident[:], C.ident_d[:, :], W=[ident])
    cst = S.sb([128, 8], F32, "cst")
    S.dma("sp", cst[:], C.ropec0_d[:, :], W=[cst])
    gv = S.sb([128, NCH], F32, "gv")
    S.dma("sp", gv[:], C.norm_mix_d[0, :].rearrange("(c p) -> p c", p=128), W=[gv], allow_slow_non_contiguous=True)
    negpi = None
    C.epsb = S.sb([128, 1], F32, "epsb")
    S.memset("dve", C.epsb, C.epsb[:], RMS_EPS)
    C.rt_ang = S.sb([128, GT], F32, "rt_ang")
    C.rt_ki = S.sb([128, GT], I32, "rt_ki")
    C.rt_kf = S.sb([128, GT], F32, "rt_kf")
    NFM = 14
    WC = NFM * 256 + 1152
    w = S.sb([128, NCH, WC], BF16, "w0")
    load_weights_bf16(S, C.w0_d, WC, gv, 0, w)

    xt = [S.sb([128, 4, D], F32, "xt")] * 2
    junk = S.sb([128, D], BF16, "junk")
    ssq = S.sb([128, 4], F32, "ssq")
    rstd = S.sb([128, 4], F32, "rstd")
    xn = S.sb([128, 4, D], BF16, "xn")
    xnT = [S.sb([128, NCH, GT], BF16, "xnT") for _ in range(2)]
    pT = [S.ps([128, GT], BF16, "pT") for _ in range(2)]
    pz = [S.ps([128, GT], F32, "pz") for _ in range(2)]
    pr = [S.ps([128, GT], F32, "pr") for _ in range(2)]
    pm = [S.ps([128, GT], F32, "pm") for _ in range(2)]
    posi = S.sb([128, GT], I32, "posi")
    posf = S.sb([128, GT], F32, "posf")
    tmp = S.sb([128, GT], F32, "tmp")
    tabs = {}
    for nm in ("A", "Ak", "B", "Bk"):
        tabs[nm] = (S.sb([128, GT], F32, "cos" + nm), S.sb([128, GT], F32, "sin" + nm))
    t1 = [S.sb([128, GT], F32, "t1") for _ in range(2)]
    t2 = [S.sb([128, GT], F32, "t2") for _ in range(2)]
    ofm = [S.sb([128, GT], BF16, "ofm") for _ in range(3)]
    ova = [S.sb([128, 2, 128], BF16, "ova") for _ in range(2)]
    for b in ova:
        S.memset("pool", b, b[:], 0.0)
        S.memset("pool", b, b[:, :, 0:1], 1.0)
    obv = [S.sb([128, 512], BF16, "obv") for _ in range(2)]
    obg = [S.sb([128, 512], F32, "obg") for _ in range(2)]
    tab_of = ["A"] * 4 + ["Ak"] * 2 + ["B"] * 4 + ["Bk"] * 4
    k = 0
    for g in range(NG):
        X = xnT[g % 2]
        norm_transpose_group(S, C, x_dram, g, xt[g % 2], junk, ssq, rstd, xn, pT, X, ident)
        specs = [(cst, cst[:, 0:1], cst[:, 1:2], [(tabs["A"][0], tabs["A"][1], 1.0), (tabs["Ak"][0], tabs["Ak"][1], 0.125)]),
                 (cst, cst[:, 2:3], cst[:, 3:4], [(tabs["B"][0], tabs["B"][1], 1.0), (tabs["Bk"][0], tabs["Bk"][1], 0.125)])]
        rope_tables(S, C, C.pos_d, g, posi, posf, specs, tmp, negpi)
        for blk in range(NFM):
            z, r = pz[blk % 2], pr[blk % 2]
            c0 = blk * 256
            for c in range(NCH):
                S.mm(z, z[:], w, w[:, c, c0:c0 + 128], X, X[:, c, :], c == 0, c == NCH - 1)
            for c in range(NCH):
                S.mm(r, r[:], w, w[:, c, c0 + 128:c0 + 256], X, X[:, c, :], c == 0, c == NCH - 1)
            cosb, sinb = tabs[tab_of[blk]]
            a, b2, o = t1[blk % 2], t2[blk % 2], ofm[blk % 3]
            S.tt("dve", a, a[:], z, z[:], cosb, cosb[:], ALU.mult)
            S.tt("dve", b2, b2[:], r, r[:], sinb, sinb[:], ALU.mult)
            S.tt("dve" if blk % 4 != 3 else "pool", o, o[:], a, a[:], b2, b2[:], ALU.add)
            S.dma("sp", C.fm0_d[blk, :, g * GT:(g + 1) * GT], o[:], R=[o])
        c0 = NFM * 256
        for j in range(4):
            tok0 = g * GT + j * 128
            p1, p2, p3 = pm[0], pm[1], pz[j % 2]
            for (pp, cc, nn) in ((p1, c0, 128), (p2, c0 + 128, 512), (p3, c0 + 640, 512)):
                for c in range(NCH):
                    S.mm(pp, pp[:, 0:nn], X, X[:, c, j * 128:(j + 1) * 128], w, w[:, c, cc:cc + nn], c == 0, c == NCH - 1)
            va, bv, bg = ova[j % 2], obv[j % 2], obg[j % 2]
            S.copy("act", va, va[:, :, 64:128], p1, p1[:, 0:128].rearrange("p (g d) -> p g d", g=2))
            S.copy("dve", bv, bv[:], p2, p2[:])
            S.op("act", lambda e, bg=bg, p3=p3: e.activation(out=bg[:], in_=p3[:], func=AF.Silu), R=[p3], W=[bg])
            S.dma("pool", C.va0_d[tok0:tok0 + 128, :], va[:].rearrange("p g d -> p (g d)"), R=[va])
            S.dma("pool", C.bv0_d[tok0:tok0 + 128, :], bv[:], R=[bv])
            S.dma("pool", C.gate0_d[tok0:tok0 + 128, :], bg[:], R=[bg])
    S.end()


def host_prep_P0(ev_w_in):
    w = np.asarray(ev_w_in, np.float32)
    aq, ak, av, bq, bk, bv, bg = np.split(w, np.cumsum([512, 128, 128, 512, 512, 512])[:], axis=1)
    pa8 = rot_perm(8, 64, 16)
    pa2 = rot_perm(2, 64, 16)
    pb8 = rot_perm(8, 64, 64)
    cols = []
    aqr = aq[:, pa8]
    for j in range(4):
        cols += [aq[:, j * 128:(j + 1) * 128], aqr[:, j * 128:(j + 1) * 128]]
    akr = ak[:, pa2]
    for gq in range(2):
        kk = ak[:, gq * 64:(gq + 1) * 64]
        kr = akr[:, gq * 64:(gq + 1) * 64]
        cols += [kk, kk, kr, kr]
    bqr = bq[:, pb8]
    for j in range(4):
        cols += [bq[:, j * 128:(j + 1) * 128], bqr[:, j * 128:(j + 1) * 128]]
    bkr = bk[:, pb8]
    for j in range(4):
        cols += [bk[:, j * 128:(j + 1) * 128], bkr[:, j * 128:(j + 1) * 128]]
    cols += [av, bv, bg]
    return np.ascontiguousarray(np.concatenate(cols, axis=1))


def bcast_row_to_parts(S, C, src_b, src_ap, dst_b, dst_ap, ps_b, ncol):
    S.mm(ps_b, ps_b[:, 0:ncol], C.onesf, C.onesf[0:1, :], src_b, src_ap, True, True)
    S.copy("dve", dst_b, dst_ap, ps_b, ps_b[:, 0:ncol])


def load_consts(S, C):
    C.ident = S.sb([128, 128], BF16, "ident")
    S.dma("sp", C.ident[:], C.ident_d[:, :], W=[C.ident])
    C.onesf = S.sb([128, 128], F32, "onesf")
    S.memset("pool", C.onesf, C.onesf[:], 1.0)
    C.epsb = S.sb([128, 1], F32, "epsb")
    S.memset("dve", C.epsb, C.epsb[:], RMS_EPS)


def stage_A(S, C):
    Sq = C.S
    N = Sq // 128
    NG = Sq // GT
    S.begin()
    load_consts(S, C)
    maskA = S.sb([128, 2, 512], BF16, "maskA")
    S.dma("sp", maskA[:], C.maskA_d[:, :, :], W=[maskA])
    sinkb = S.sb([128, 8], F32, "sinkb")
    S.dma("sp", sinkb[:], C.sink_d[0, :].partition_broadcast(128), W=[sinkb])
    Q = [S.sb([128, Sq], BF16, "Q%d" % j) for j in range(4)]
    V = S.sb([128, N, 256], BF16, "V")
    for j in range(4):
        for h in range(0, Sq, 2048):
            w_ = min(2048, Sq - h)
            S.dma("sp" if j % 2 == 0 else "act", Q[j][:, h:h + w_], C.fm0_d[j, :, h:h + w_], W=[Q[j]])
    Kz = [[S.sb([128, Sq], BF16, "Kz%d%d" % (j, r)) for r in range(2)] for j in range(2)]
    for j in range(2):
        for r in range(2):
            S.memset("pool", Kz[j][r], Kz[j][r][(1 - r) * 64:(2 - r) * 64, :], 0.0)
            for h in range(0, Sq, 2048):
                w_ = min(2048, Sq - h)
                S.dma("pool", Kz[j][r][r * 64:(r + 1) * 64, h:h + w_], C.fm0_d[4 + j, r * 64:(r + 1) * 64, h:h + w_], W=[Kz[j][r]])
    K = [Kz[0][0], Kz[1][0]]
    S.dma("sp", V[:], C.va0_d.rearrange("(n p) c -> p n c", p=128), W=[V])
    sq = [S.sb([128, GT], F32, "sq") for _ in range(2)]
    accq = S.sb([1, GT], F32, "accq")
    acck = S.sb([1, GT], F32, "acck")
    S.memset("dve", accq, accq[:], 0.0)
    S.memset("dve", acck, acck[:], 0.0)
    pn = S.ps([128, GT], F32, "pn")
    i = 0
    for (lst, acc, kp) in ((Q, accq, 128), (K, acck, 64)):
        for b in lst:
            for g in range(NG):
                s_ = sq[i % 2]
                i += 1
                S.op("act", lambda e, s_=s_, b=b, g=g: e.activation(out=s_[:], in_=b[:, g * GT:(g + 1) * GT], func=AF.Square),
                     R=[b], W=[s_])
                S.mm(pn, pn[0:1, :], C.onesf, C.onesf[0:kp, 0:1], s_, s_[0:kp, :], True, True)
                S.tt("dve", acc, acc[:], pn, pn[0:1, :], acc, acc[:], ALU.max)
    mq = S.sb([1, 4], F32, "mq")
    S.op("dve", lambda e: e.tensor_reduce(out=mq[:, 0:1], in_=accq[:], axis=AX.X, op=ALU.max), R=[accq], W=[mq])
    S.op("dve", lambda e: e.tensor_reduce(out=mq[:, 1:2], in_=acck[:], axis=AX.X, op=ALU.max), R=[acck], W=[mq])
    S.tt("dve", mq, mq[:, 2:3], mq, mq[:, 0:1], mq, mq[:, 1:2], ALU.mult)
    S.op("act", lambda e: e.activation(out=mq[:, 3:4], in_=mq[:, 2:3], func=AF.Sqrt), R=[mq], W=[mq])
    S.ts("dve", mq, mq[:, 3:4], mq, mq[:, 3:4], -1.0, None, ALU.mult)
    negM = S.sb([128, 1], F32, "negM")
    bcast_row_to_parts(S, C, mq, mq[0:1, 3:4], negM, negM[:], pn, 1)
    sinkexp = S.sb([128, 8], F32, "sinkexp")
    S.op("act", lambda e: e.activation(out=sinkexp[:], in_=sinkb[:], func=AF.Exp, bias=negM[:, 0:1]),
         R=[sinkb, negM], W=[sinkexp])
    pss = [S.ps([128, 512], F32, "pss") for _ in range(4)]
    pso = [S.ps([128, 512], F32, "pso") for _ in range(2)]
    pts = [S.sb([128, 512], BF16, "pt") for _ in range(4)]
    den = [S.sb([1, 512], F32, "den") for _ in range(2)]
    rdb = [S.sb([128, 512], F32, "rdb") for _ in range(2)]
    osb = [S.sb([128, 512], BF16, "osb") for _ in range(2)]
    ks = 0
    it = 0
    for n in range(N):
        for g in range(2):
            po = pso[it % 2]
            ms = [m for m in (n - 1, n, n + 1) if 0 <= m < N]
            ptl = []
            for m in ms:
                ps_ = pss[ks % 4]
                pt = pts[ks % 4]
                ks += 1
                for hh in range(4):
                    h = 4 * g + hh
                    j, r = h // 2, h % 2
                    S.mm(ps_, ps_[:, hh * 128:(hh + 1) * 128], Kz[g][r], Kz[g][r][:, m * 128:(m + 1) * 128],
                         Q[j], Q[j][:, n * 128:(n + 1) * 128], True, True)
                S.op("act", lambda e, pt=pt, ps_=ps_: e.activation(out=pt[:], in_=ps_[:], func=AF.Exp, bias=negM[:, 0:1]),
                     R=[ps_, negM], W=[pt])
                if m != n:
                    mi = 0 if m < n else 1
                    S.tt("pool", pt, pt[:], pt, pt[:], maskA, maskA[:, mi, :], ALU.mult)
                ptl.append((m, pt))
            for idx, (m, pt) in enumerate(ptl):
                S.mm(po, po[:, :], V, V[:, m, g * 128:(g + 1) * 128], pt, pt[:], idx == 0, idx == len(ptl) - 1)
            dn, rb, ob = den[it % 2], rdb[it % 2], osb[it % 2]
            for hh in range(4):
                h = 4 * g + hh
                S.ts("dve", dn, dn[0:1, hh * 128:(hh + 1) * 128], po, po[0:1, hh * 128:(hh + 1) * 128],
                     sinkexp[0:1, h:h + 1], None, ALU.add, R=[sinkexp])
            S.op("dve", lambda e, dn=dn: e.reciprocal(out=dn[0:1, :], in_=dn[0:1, :]), R=[dn], W=[dn])
            pb = pss[ks % 4]
            ks += 1
            S.mm(pb, pb[:, :], C.onesf, C.onesf[0:1, :], dn, dn[0:1, :], True, True)
            S.copy("act", rb, rb[64:128, :], pb, pb[64:128, :])
            S.tt("dve", ob, ob[64:128, :], po, po[64:128, :], rb, rb[64:128, :], ALU.mult)
            S.dma("sp", C.mixT0_d[(4 * g) * 64:(4 * g + 4) * 64, n * 128:(n + 1) * 128].rearrange("(h d) q -> d h q", d=64),
                  ob[64:128, :].rearrange("d (h q) -> d h q", h=4), R=[ob])
            it += 1
    S.end()


def stage_B(S, C):
    Sq = C.S
    N = Sq // 128
    L = 128
    S.begin()
    load_consts(S, C)
    relB = S.sb([128, 2, 128], F32, "relB")
    S.dma("sp", relB[:], C.relB_d[:, :, :], W=[relB])
    maskB = S.sb([128, 2, 128], F32, "maskB")
    S.dma("sp", maskB[:], C.maskB_d[:, :, :], W=[maskB])
    cvec = S.sb([128, 4], F32, "cvec")
    S.dma("sp", cvec[:], C.cvecB_d[:, :], W=[cvec])
    lgb = S.sb([128, 16], F32, "lgb")
    S.dma("sp", lgb[:], C.decay_d.rearrange("a b h -> a (b h)")[0, :].partition_broadcast(128), W=[lgb])
    lgp = S.sb([128, 8], F32, "lgp")
    S.dma("sp", lgp[:], C.decayp_d[:, :], W=[lgp])
    for t in (lgb, lgp):
        S.op("act", lambda e, t=t: e.activation(out=t[:], in_=t[:], func=AF.Exp, scale=-1.0), R=[t], W=[t])
        S.ts("dve", t, t[:], t, t[:], 1.0, None, ALU.add)
        S.op("act", lambda e, t=t: e.activation(out=t[:], in_=t[:], func=AF.Ln), R=[t], W=[t])
        S.ts("dve", t, t[:], t, t[:], -1.0, None, ALU.mult)
    zx = S.sb([128, 4, 8], F32, "zx")
    for k_, (ci, d) in enumerate(((0, 0), (1, 1), (2, 0), (3, 1))):
        S.ts("dve", zx, zx[:, k_, :], lgb, lgb[:, d * 8:(d + 1) * 8], cvec[:, ci:ci + 1], None, ALU.mult, R=[cvec])
    S.op("act", lambda e: e.activation(out=zx[:], in_=zx[:], func=AF.Exp), R=[zx], W=[zx])
    cd = S.sb([128, 8], F32, "cd")
    S.op("act", lambda e: e.activation(out=cd[:], in_=lgp[:], func=AF.Exp, scale=float(L)), R=[lgp], W=[cd])
    dc = S.sb([128, 8, 128], F32, "dc")
    dtmp = S.sb([128, 128], F32, "dtmp")
    for h in range(8):
        S.op("act", lambda e, h=h: e.activation(out=dc[:, h, :], in_=relB[:, 0, :], func=AF.Exp, scale=lgb[:, h:h + 1]),
             R=[relB, lgb], W=[dc])
        S.tt("dve", dc, dc[:, h, :], dc, dc[:, h, :], maskB, maskB[:, 0, :], ALU.mult)
        S.op("act", lambda e, h=h: e.activation(out=dtmp[:], in_=relB[:, 1, :], func=AF.Exp, scale=lgb[:, 8 + h:9 + h]),
             R=[relB, lgb], W=[dtmp])
        S.tt("dve", dtmp, dtmp[:], dtmp, dtmp[:], maskB, maskB[:, 1, :], ALU.mult)
        S.tt("dve", dc, dc[:, h, :], dc, dc[:, h, :], dtmp, dtmp[:], ALU.add)
    Vt = S.sb([128, N, 128], BF16, "Vt")
    bdm = S.sb([128, 128], F32, "bdm")
    S.dma("sp", bdm[:], C.bdm_d[:, :], W=[bdm])
    Kpz = [S.sb([128, Sq], BF16, "Kpz%d" % r) for r in range(2)]
    Gt = S.sb([128, N, 128], F32, "Gt")
    Qp = S.sb([128, Sq], BF16, "Qp")
    Kp = S.sb([128, Sq], BF16, "Kp")
    Rf = S.sb([128, 128], F32, "Rf")
    Rb = S.sb([128, 128], F32, "Rb")
    Rfp = S.sb([128, N, 128], BF16, "Rfp")
    Rbp = S.sb([128, N, 128], BF16, "Rbp")
    pk = [S.ps([128, 128], BF16, "pk") for _ in range(2)]
    pu = [S.ps([128, 256], F32, "pu") for _ in range(2)]
    pss = [S.ps([128, 256], F32, "pss") for _ in range(2)]
    py = [S.ps([128, 384], F32, "py") for _ in range(2)]
    kz = [S.sb([128, 2, 128], BF16, "kz") for _ in range(2)]
    sd = [S.sb([128, 2, 128], BF16, "sd") for _ in range(2)]
    ysb = [S.sb([128, 128], F32, "ysb") for _ in range(2)]
    junk = S.sb([128, 64], F32, "junkB")
    st = [S.sb([128, 8], F32, "st") for _ in range(2)]
    ob = [S.sb([128, 128], BF16, "ob") for _ in range(2)]
    oT = [S.sb([128, 512], BF16, "oT") for _ in range(2)]
    gneps = S.sb([128, 1], F32, "gneps")
    S.memset("dve", gneps, gneps[:], 1e-5)
    for pr_ in range(4):
        for h0 in range(0, Sq, 2048):
            w_ = min(2048, Sq - h0)
            S.dma("sp", Qp[:, h0:h0 + w_], C.fm0_d[6 + pr_, :, h0:h0 + w_], W=[Qp])
            S.dma("act", Kp[:, h0:h0 + w_], C.fm0_d[10 + pr_, :, h0:h0 + w_], W=[Kp])
        S.dma("pool", Gt[:], C.gate0_d[:, pr_ * 128:(pr_ + 1) * 128].rearrange("(n p) c -> p n c", p=128), W=[Gt])
        S.dma("sp", Vt[:], C.bv0_d[:, pr_ * 128:(pr_ + 1) * 128].rearrange("(n p) c -> p n c", p=128), W=[Vt])
        for r in range(2):
            S.copy("pool", Kpz[r], Kpz[r][:], Kp, Kp[:])
            S.memset("pool", Kpz[r], Kpz[r][(1 - r) * 64:(2 - r) * 64, :], 0.0)
        S.memset("dve", Rf, Rf[:], 0.0)
        S.memset("dve", Rb, Rb[:], 0.0)
        for dirn in (0, 1):
            R_, Rp = (Rf, Rfp) if dirn == 0 else (Rb, Rbp)
            order = range(N) if dirn == 0 else range(N - 1, -1, -1)
            for n in order:
                p_, kz_, pu_ = pk[n % 2], kz[n % 2], pu[n % 2]
                S.tr(p_, p_[:], Kp, Kp[:, n * 128:(n + 1) * 128], C.ident, C.ident[:])
                for r in range(2):
                    h = 2 * pr_ + r
                    S.ts("dve", kz_, kz_[:, 0, r * 64:(r + 1) * 64], p_, p_[:, r * 64:(r + 1) * 64],
                         zx[:, dirn, h:h + 1], None, ALU.mult, R=[zx])
                S.mm(pu_, pu_[:, 0:128], kz_, kz_[:, 0, :], Vt, Vt[:, n, :], True, True)
                S.tt("pool", Rp, Rp[:, n, :], R_, R_[:], bdm, bdm[:], ALU.mult)
                S.stt("dve", R_, R_[:], R_, R_[:], cd[:, pr_ * 2 + dirn:pr_ * 2 + dirn + 1], pu_, pu_[:, 0:128],
                      ALU.mult, ALU.add, R=[cd])
        for n in range(N):
            ps_, sd_, py_, y_, st_, ob_ = pss[n % 2], sd[n % 2], py[n % 2], ysb[n % 2], st[n % 2], ob[n % 2]
            tok = slice(n * 128, (n + 1) * 128)
            for r in range(2):
                S.mm(ps_, ps_[:, r * 128:(r + 1) * 128], Kpz[r], Kpz[r][:, tok], Qp, Qp[:, tok], True, True)
            S.tt("dve", sd_, sd_[:].rearrange("p a b -> p (a b)"), ps_, ps_[:],
                 dc, dc[:, 2 * pr_:2 * pr_ + 2, :].rearrange("p a b -> p (a b)"), ALU.mult)
            for r in range(2):
                S.mm(py_, py_[:, r * 64:(r + 1) * 64], sd_, sd_[:, r, :], Vt, Vt[:, n, r * 64:(r + 1) * 64], True, True)
            S.mm(py_, py_[:, 128:256], Qp, Qp[:, tok], Rfp, Rfp[:, n, :], True, True)
            S.mm(py_, py_[:, 256:384], Qp, Qp[:, tok], Rbp, Rbp[:, n, :], True, True)
            S.copy("act", y_, y_[:], py_, py_[:, 0:128])
            for r in range(2):
                h = 2 * pr_ + r
                c_ = slice(r * 64, (r + 1) * 64)
                S.stt("dve", y_, y_[:, c_], py_, py_[:, 128 + r * 64:128 + (r + 1) * 64], zx[:, 2, h:h + 1], y_, y_[:, c_],
                      ALU.mult, ALU.add, R=[zx])
                S.stt("dve", y_, y_[:, c_], py_, py_[:, 256 + r * 64:256 + (r + 1) * 64], zx[:, 3, h:h + 1], y_, y_[:, c_],
                      ALU.mult, ALU.add, R=[zx])
            S.op("dve", lambda e, y_=y_, st_=st_: e.tensor_reduce(out=st_[:, 0:2], in_=y_[:].rearrange("p (a b) -> p a b", a=2),
                                                                 axis=AX.X, op=ALU.add), R=[y_], W=[st_])
            for r in range(2):
                S.op("act", lambda e, y_=y_, st_=st_, r=r: e.activation(out=junk[:], in_=y_[:, r * 64:(r + 1) * 64], func=AF.Square,
                                                                         accum_out=st_[:, 2 + r:3 + r]), R=[y_], W=[junk, st_])
            S.ts("dve", st_, st_[:, 4:6], st_, st_[:, 0:2], 1.0 / 64, None, ALU.mult)
            S.tt("dve", st_, st_[:, 0:2], st_, st_[:, 4:6], st_, st_[:, 4:6], ALU.mult)
            S.stt("dve", st_, st_[:, 6:8], st_, st_[:, 2:4], 1.0 / 64, st_, st_[:, 0:2], ALU.mult, ALU.subtract)
            S.op("act", lambda e, st_=st_: e.activation(out=st_[:, 6:8], in_=st_[:, 6:8], func=AF.Sqrt, bias=gneps[:, 0:1]),
                 R=[st_, gneps], W=[st_])
            S.op("dve", lambda e, st_=st_: e.reciprocal(out=st_[:, 6:8], in_=st_[:, 6:8]), R=[st_], W=[st_])
            for r in range(2):
                c_ = slice(r * 64, (r + 1) * 64)
                S.ts("dve", y_, y_[:, c_], y_, y_[:, c_], st_[:, 4 + r:5 + r], st_[:, 6 + r:7 + r], ALU.subtract, ALU.mult, R=[st_])
            S.tt("pool", ob_, ob_[:], y_, y_[:], Gt, Gt[:, n, :], ALU.mult)
            pt_ = pk[n % 2]
            S.tr(pt_, pt_[:], ob_, ob_[:], C.ident, C.ident[:])
            oT_ = oT[(n // 4) % 2]
            S.copy("act", oT_, oT_[:, (n % 4) * 128:(n % 4 + 1) * 128], pt_, pt_[:])
            if n % 4 == 3:
                S.dma("sp", C.mixT0_d[512 + pr_ * 128:512 + (pr_ + 1) * 128, (n - 3) * 128:(n + 1) * 128], oT_[:], R=[oT_])
    S.end()


def declare(nc, C, Sq, debug=False):
    ks = "ExternalOutput" if debug else "Internal"
    def inp(name, shape, dt):
        return nc.dram_tensor(name, list(shape), dt, kind="ExternalInput").ap()
    def scr(name, shape, dt):
        return nc.dram_tensor(name, list(shape), dt, kind=ks).ap()
    C.S = Sq
    C.x_d = inp("x", [Sq, D], F32)
    C.pos_d = inp("pos", [Sq], I32)
    C.norm_mix_d = inp("norm_mix", [2, D], F32)
    C.w0_d = inp("w0", [D, 14 * 256 + 1152], F32)
    C.ident_d = inp("ident", [128, 128], BF16)
    C.ropec0_d = inp("ropec0", [128, 8], F32)
    C.maskA_d = inp("maskA", [128, 2, 512], BF16)
    C.sink_d = inp("sink", [1, 8], F32)
    C.relB_d = inp("relB", [128, 2, 128], F32)
    C.maskB_d = inp("maskB", [128, 2, 128], F32)
    C.cvecB_d = inp("cvecB", [128, 4], F32)
    C.decay_d = inp("decay", [1, 2, 8], F32)
    C.decayp_d = inp("decayp", [128, 8], F32)
    C.identf_d = inp("identf", [128, 128], F32)
    C.norm_ffn_d = inp("norm_ffn", [2, D], F32)
    C.router_d = inp("router", [2, D, 16], F32)
    C.tokc_d = inp("tokc", [128, Sq // 128], I32)
    C.trib_d = inp("trib", [128, 128], BF16)
    C.ecap_d = inp("ecap", [128, 16], F32)
    C.wout0_d = inp("wout0", [D, D], F32)
    C.wg_d = inp("wg", [2, 16, D, 2048], F32)
    C.wu_d = inp("wu", [2, 16, D, 2048], F32)
    C.wd_d = inp("wd", [2, 16, 2048, D], F32)
    C.norm_final_d = inp("norm_final", [1, D], F32)
    C.y_d = nc.dram_tensor("y", [Sq, D], F32, kind="ExternalOutput").ap()
    C.x1_d = scr("x1", [Sq, D], F32)
    C.hnx_d = scr("hnx", [Sq, XW], I32)
    C.xin_d = scr("xin", [16 * (Sq // 8), XW], I32)
    C.bdm_d = inp("bdm", [128, 128], F32)
    C.w1_d = inp("w1", [D, W1C], F32)
    C.wuq_d = inp("wuq", [512, 1536], F32)
    C.wukv_d = inp("wukv", [256, 1024], F32)
    C.ropec1_d = inp("ropec1", [128, 2], F32)
    C.mla_nq_d = inp("mla_nq", [1, 512], F32)
    C.mla_nkv_d = inp("mla_nkv", [1, 256], F32)
    C.gbias_d = inp("gbias", [4, 4], F32)
    C.wout1_d = inp("wout1", [D, D], F32)
    C.fmq_d = scr("fmq", [8, 96, Sq], BF16)
    C.fmk_d = scr("fmk", [8, 96, Sq], BF16)
    C.vC_d = scr("vC", [Sq, 1024], BF16)
    C.qkraw_d = scr("qkraw", [1024, Sq], F32)
    C.gatesT_d = scr("gatesT", [4, 4, Sq], F32)
    C.vD_d = scr("vD", [Sq, 4 * 129], BF16)
    C.og_d = scr("og", [Sq, 512], F32)
    C.mixT1_d = scr("mixT1", [1024, Sq], BF16)
    C.conv_d = inp("conv", [128, 40], F32)
    C.maskD_d = inp("maskD", [128, 2, 128], F32)
    C.qkc_d = scr("qkc", [8, 128, Sq], BF16)
    C.vec_d = scr("vec", [10, 4, Sq], F32)
    C.vec2_d = scr("vec2", [4, 4, Sq // 128], F32)
    C.chunk_d = scr("chunkd", [4, 4, Sq // 128], F32)
    C.mp_d = scr("mpd", [2, 4, Sq // 128], F32)
    C.x3_d = scr("x3", [Sq, D], F32)
    C.fm0_d = scr("fm0", [14, 128, Sq], BF16)
    C.va0_d = scr("va0", [Sq, 256], BF16)
    C.bv0_d = scr("bv0", [Sq, 512], BF16)
    C.gate0_d = scr("gate0", [Sq, 512], F32)
    C.mixT0_d = scr("mixT0", [1024, Sq], BF16)


def host_consts():
    import ml_dtypes
    bf = ml_dtypes.bfloat16
    c = {}
    c["ident"] = np.eye(128, dtype=np.float32).astype(bf)
    rc = np.zeros((128, 8), np.float32)
    rc[:, 0], rc[:, 1] = rope_consts(64, 16, 500000.0)
    rc[:, 2], rc[:, 3] = rope_consts(64, 64, 10000.0)
    c["ropec0"] = rc
    j = np.arange(128)[:, None]
    i = np.arange(128)[None, :]
    mA = np.zeros((128, 2, 512), np.float32)
    mA[:, 0, :] = np.tile((j >= i).astype(np.float32), (1, 4))
    mA[:, 1, :] = np.tile((j <= i).astype(np.float32), (1, 4))
    c["maskA"] = mA.astype(bf)
    rel = (i - j).astype(np.float32)
    rB = np.zeros((128, 2, 128), np.float32)
    rB[:, 0] = np.maximum(rel, 0)
    rB[:, 1] = np.maximum(-rel, 0)
    c["relB"] = rB
    mB = np.zeros((128, 2, 128), np.float32)
    mB[:, 0] = (rel >= 0)
    mB[:, 1] = (rel < 0)
    c["maskB"] = mB
    c["identf"] = np.eye(128, dtype=np.float32)
    c["bdm"] = ((j // 64) == (i // 64)).astype(np.float32)
    c["trib"] = (j < i).astype(np.float32).astype(bf)
    l = np.arange(128, dtype=np.float32)
    c["cvecB"] = np.stack([127 - l, l, l + 1, 128 - l], axis=1).astype(np.float32)
    return c


def host_inputs(inp, b):
    d = dict(host_consts())
    d["x"] = np.ascontiguousarray(inp["x"][b])
    d["pos"] = np.ascontiguousarray(inp["positions"][b]).astype(np.int32)
    d["norm_mix"] = np.asarray(inp["norm_mix"], np.float32)
    d["w0"] = host_prep_P0(inp["ev_w_in"][0])
    Sq = d["x"].shape[0]
    d["tokc"] = (np.arange(Sq // 128, dtype=np.int32)[None, :] * 128 + np.arange(128, dtype=np.int32)[:, None]).astype(np.int32)
    d["ecap"] = np.tile((np.arange(16, dtype=np.float32) * (Sq // 8))[None, :], (128, 1)).astype(np.float32)
    d["norm_ffn"] = np.asarray(inp["norm_ffn"], np.float32)
    d["router"] = np.asarray(inp["moe_router"], np.float32)
    d["wout0"] = np.asarray(inp["ev_w_out"][0], np.float32)
    d["wg"] = np.asarray(inp["moe_w_gate"], np.float32)
    d["wu"] = np.asarray(inp["moe_w_up"], np.float32)
    d["wd"] = np.asarray(inp["moe_w_down"], np.float32)
    d["norm_final"] = np.asarray(inp["norm_final"], np.float32).reshape(1, D)
    d["w1"], d["wuq"], d["wukv"] = host_prep_P1(inp["od_w_in"][0], inp["mla_w_uq"][0], inp["mla_w_ukv"][0])
    d["ropec1"] = ropec1()
    d["mla_nq"] = np.asarray(inp["mla_norm_q"], np.float32).reshape(1, 512)
    d["mla_nkv"] = np.asarray(inp["mla_norm_kv"], np.float32).reshape(1, 256)
    d["gbias"] = np.ascontiguousarray(np.asarray(inp["mlstm_gate_bias"], np.float32)[0].T)
    d["wout1"] = np.asarray(inp["od_w_out"][0], np.float32)
    d["conv"] = np.ascontiguousarray(np.asarray(inp["mlstm_conv"][0], np.float32).reshape(5, 8, 128).transpose(2, 1, 0).reshape(128, 40))
    jj = np.arange(128)[:, None]; tt_ = np.arange(128)[None, :]
    d["maskD"] = np.stack([(jj <= tt_), (jj > tt_)], axis=1).astype(np.float32)
    d["sink"] = np.asarray(inp["attn_sink"], np.float32).reshape(1, 8)
    dl = np.asarray(inp["ret_decay_logit"], np.float32).reshape(1, 2, 8)
    d["decay"] = dl
    dp = np.zeros((128, 8), np.float32)
    for pr_ in range(4):
        for dr in range(2):
            dp[0:64, pr_ * 2 + dr] = dl[0, dr, 2 * pr_]
            dp[64:128, pr_ * 2 + dr] = dl[0, dr, 2 * pr_ + 1]
    d["decayp"] = dp
    return d

NE = 16
XW = 529


def stage_OUT(S, C, l, mixT_d, wout_d, xin_d, xout_d):
    Sq = C.S
    NG = Sq // GT
    S.begin()
    load_consts(S, C)
    identf = S.sb([128, 128], F32, "identf")
    S.dma("sp", identf[:], C.identf_d[:, :], W=[identf])
    w = S.sb([128, NCH, D], BF16, "wout")
    load_weights_bf16(S, wout_d, D, None, 0, w)
    gb = S.sb([128, D], F32, "gb")
    S.dma("sp", gb[:], C.norm_ffn_d[l, :].partition_broadcast(128), W=[gb])
    wr = S.sb([128, NCH, NE], F32, "wr")
    S.dma("sp", wr[:], C.router_d[l].rearrange("(c p) e -> p c e", p=128), W=[wr])
    tokc = S.sb([128, Sq // 128], I32, "tokc")
    S.dma("sp", tokc[:], C.tokc_d[:, :], W=[tokc])
    mT = [S.sb([128, NCH, GT], BF16, "mT") for _ in range(2)]
    xt = [S.sb([128, D], F32, "xt") for _ in range(2)]
    x1 = [S.sb([128, D], F32, "x1") for _ in range(2)]
    hn = [S.sb([128, D], F32, "hn") for _ in range(2)]
    hT = S.sb([128, NCH, 128], F32, "hT")
    row = [S.sb([128, XW], I32, "row") for _ in range(2)]
    junk = S.sb([128, D], BF16, "junk")
    st = [S.sb([128, 8], F32, "st") for _ in range(2)]
    lg = [S.sb([128, NE], F32, "lg") for _ in range(2)]
    po = [S.ps([128, 512], F32, "po") for _ in range(4)]
    pt = [S.ps([128, 512], F32, "pt") for _ in range(2)]
    pl = S.ps([128, NE], F32, "pl")
    for g in range(NG):
        M = mT[g % 2]
        S.dma("sp", M[:], mixT_d[:, g * GT:(g + 1) * GT].rearrange("(c p) t -> p c t", p=128), W=[M])
        for j in range(4):
            it = g * 4 + j
            tok0 = it * 128
            X, X1, H, R, st_, lg_ = xt[it % 2], x1[it % 2], hn[it % 2], row[it % 2], st[it % 2], lg[it % 2]
            S.dma("act", X[:], xin_d[tok0:tok0 + 128, :], W=[X])
            for hf in range(2):
                p_ = po[(it * 2 + hf) % 4]
                for c in range(NCH):
                    S.mm(p_, p_[:], M, M[:, c, j * 128:(j + 1) * 128], w, w[:, c, hf * 512:(hf + 1) * 512], c == 0, c == NCH - 1)
                S.tt("dve", X1, X1[:, hf * 512:(hf + 1) * 512], p_, p_[:], X, X[:, hf * 512:(hf + 1) * 512], ALU.add)
            S.dma("sp", xout_d[tok0:tok0 + 128, :], X1[:], R=[X1])
            S.op("act", lambda e, X1=X1, st_=st_: e.activation(out=junk[:], in_=X1[:], func=AF.Square, accum_out=st_[:, 0:1]),
                 R=[X1], W=[junk, st_])
            S.op("act", lambda e, st_=st_: e.activation(out=st_[:, 1:2], in_=st_[:, 0:1], func=AF.Sqrt, bias=C.epsb[:, 0:1], scale=1.0 / D),
                 R=[st_, C.epsb], W=[st_])
            S.op("dve", lambda e, st_=st_: e.reciprocal(out=st_[:, 1:2], in_=st_[:, 1:2]), R=[st_], W=[st_])
            S.stt("dve", H, H[:], X1, X1[:], st_[:, 1:2], gb, gb[:], ALU.mult, ALU.mult, R=[st_])
            S.copy("pool", R, R[:, 0:512].bitcast(BF16), H, H[:])
            for c in range(NCH):
                p_ = pt[c % 2]
                S.tr(p_, p_[:, 0:128], H, H[:, c * 128:(c + 1) * 128], identf, identf[:])
                S.copy("act" if c % 2 == 0 else "dve", hT, hT[:, c, :], p_, p_[:, 0:128])
            for c in range(NCH):
                S.mm(pl, pl[:], hT, hT[:, c, :], wr, wr[:, c, :], c == 0, c == NCH - 1)
            S.op("dve", lambda e, st_=st_: e.tensor_reduce(out=st_[:, 2:3], in_=pl[:], axis=AX.X, op=ALU.max), R=[pl], W=[st_])
            S.ts("dve", st_, st_[:, 2:3], st_, st_[:, 2:3], -1.0, None, ALU.mult)
            S.op("act", lambda e, st_=st_, lg_=lg_: e.activation(out=lg_[:], in_=pl[:], func=AF.Exp, bias=st_[:, 2:3], accum_out=st_[:, 3:4]),
                 R=[pl, st_], W=[lg_, st_])
            S.op("dve", lambda e, st_=st_: e.reciprocal(out=st_[:, 4:5], in_=st_[:, 3:4]), R=[st_], W=[st_])
            S.ts("dve", R, R[:, 512:528].bitcast(F32), lg_, lg_[:], st_[:, 4:5], None, ALU.mult, R=[st_])
            S.copy("pool", R, R[:, 528:529], tokc, tokc[:, it:it + 1])
            S.dma("sp", C.hnx_d[tok0:tok0 + 128, :], R[:], R=[R])
    S.end()


def stage_MOE(S, C, l, x_d):
    Sq = C.S
    N = Sq // 128
    cap = Sq // 8
    NT = cap // 128
    BIG = float(NE * cap + 4096)
    S.begin()
    load_consts(S, C)
    aff = S.sb([128, N, NE], F32, "aff")
    S.dma("sp", aff[:], C.hnx_d[:, 512:528].bitcast(F32).rearrange("(n p) e -> p n e", p=128), W=[aff],
          allow_slow_non_contiguous=True)
    lo = S.sb([128, NE], F32, "lo")
    mid = S.sb([128, NE], F32, "mid")
    ge = S.sb([128, NE], F32, "ge")
    cnt = S.sb([128, NE], F32, "cnt")
    cmp_ = S.sb([128, N, NE], F32, "cmp")
    pc = S.ps([128, NE], F32, "pc")
    S.memset("dve", lo, lo[:], 0.0)
    for k in range(1, 29):
        wk = 2.0 ** (-k)
        S.ts("dve", mid, mid[:], lo, lo[:], wk, None, ALU.add)
        S.tt("dve", cmp_, cmp_[:], aff, aff[:], mid, mid[:].unsqueeze(1).to_broadcast([128, N, NE]), ALU.is_ge)
        S.op("dve", lambda e: e.tensor_reduce(out=cnt[:], in_=cmp_[:].rearrange("p n e -> p e n"), axis=AX.X, op=ALU.add),
             R=[cmp_], W=[cnt])
        S.mm(pc, pc[:], C.onesf, C.onesf[:], cnt, cnt[:], True, True)
        S.ts("dve", ge, ge[:], pc, pc[:], float(cap) - 0.5, None, ALU.is_ge)
        S.stt("dve", lo, lo[:], ge, ge[:], wk, lo, lo[:], ALU.mult, ALU.add)
    S.tt("dve", cmp_, cmp_[:], aff, aff[:], lo, lo[:].unsqueeze(1).to_broadcast([128, N, NE]), ALU.is_ge)
    maskb = S.sb([128, N * NE], BF16, "maskb")
    S.copy("dve", maskb, maskb[:], cmp_, cmp_[:].rearrange("p n e -> p (n e)"))
    trib = S.sb([128, 128], BF16, "trib")
    S.dma("sp", trib[:], C.trib_d[:, :], W=[trib])
    onesb = S.sb([128, 128], BF16, "onesb")
    S.memset("pool", onesb, onesb[:], 1.0)
    slot = S.sb([128, N, NE], F32, "slot")
    tot = [S.sb([128, N, NE], F32, "tot") for _ in range(2)]
    pp = [S.ps([128, 512], F32, "pp") for _ in range(2)]
    W_ = N * NE
    for c0 in range(0, W_, 512):
        cw = min(512, W_ - c0)
        S.mm(pp[0], pp[0][:, 0:cw], trib, trib[:], maskb, maskb[:, c0:c0 + cw], True, True)
        S.copy("dve", slot, slot[:].rearrange("p n e -> p (n e)")[:, c0:c0 + cw], pp[0], pp[0][:, 0:cw])
        S.mm(pp[1], pp[1][:, 0:cw], onesb, onesb[:], maskb, maskb[:, c0:c0 + cw], True, True)
        S.copy("dve", tot[0], tot[0][:].rearrange("p n e -> p (n e)")[:, c0:c0 + cw], pp[1], pp[1][:, 0:cw])
    S.tt("dve", slot, slot[:], slot, slot[:], tot[0], tot[0][:], ALU.subtract)
    cur = 0
    s_ = 1
    while s_ < N:
        a, b = tot[cur], tot[1 - cur]
        S.copy("dve", b, b[:, 0:s_, :], a, a[:, 0:s_, :])
        S.tt("dve", b, b[:, s_:N, :], a, a[:, s_:N, :], a, a[:, 0:N - s_, :], ALU.add)
        cur = 1 - cur
        s_ *= 2
    S.tt("dve", slot, slot[:], slot, slot[:], tot[cur], tot[cur][:], ALU.add)
    ecap = S.sb([128, NE], F32, "ecap")
    S.dma("sp", ecap[:], C.ecap_d[:, :], W=[ecap])
    val = tot[1 - cur]
    S.ts("dve", val, val[:], slot, slot[:], float(cap) - 0.5, None, ALU.is_lt)
    S.tt("dve", val, val[:], val, val[:], cmp_, cmp_[:], ALU.mult)
    S.tt("dve", slot, slot[:], slot, slot[:], ecap, ecap[:].unsqueeze(1).to_broadcast([128, N, NE]), ALU.add)
    S.ts("dve", slot, slot[:], slot, slot[:], -BIG, None, ALU.add)
    S.tt("dve", slot, slot[:], slot, slot[:], val, val[:], ALU.mult)
    S.ts("dve", slot, slot[:], slot, slot[:], BIG, None, ALU.add)
    idx = S.sb([128, N, NE], I32, "idx")
    S.copy("dve", idx, idx[:], slot, slot[:])
    rows = [S.sb([128, XW], I32, "rows") for _ in range(3)]
    breg = S.reg("pool", NE * cap - 1)
    for n in range(N):
        R = rows[n % 3]
        S.dma("sp", R[:], C.hnx_d[n * 128:(n + 1) * 128, :], W=[R])
        for e_ in range(NE):
            def fn(eng, R=R, n=n, e_=e_):
                return eng.indirect_dma_start(
                    out=C.xin_d[:, :], out_offset=bass.IndirectOffsetOnAxis(ap=idx[:, n, e_:e_ + 1], axis=0),
                    in_=R[:], in_offset=None, bounds_check=breg["r"], oob_is_err=False)
            S.dma("pool", None, None, R=[R, idx], sem_buf=R, fn=fn)
    S.end()
    S.begin()
    load_consts(S, C)
    wgq = [S.sb([128, NCH, 512], BF16, "wg%d" % q) for q in range(4)]
    wuq_ = [S.sb([128, NCH, 512], BF16, "wu%d" % q) for q in range(4)]
    wd = S.sb([128, 16, D], BF16, "wd")
    stg = [S.sb([128, 512], F32, "wstg") for _ in range(8)]
    xs = [S.sb([128, XW], I32, "xs") for _ in range(2)]
    xT = S.sb([128, NCH, cap], BF16, "xT")
    gates = S.sb([128, NT], F32, "gates")
    toks = S.sb([128, NT], I32, "toks")
    HS = min(512, cap)
    hid = S.sb([128, 16, cap], BF16, "hid")
    sg = [S.sb([128, HS], F32, "sg") for _ in range(2)]
    yo = [S.sb([128, D], F32, "yo") for _ in range(2)]
    ptp = [S.ps([128, 512], BF16, "ptp") for _ in range(2)]
    pg = [S.ps([128, 512], F32, "pg") for _ in range(2)]
    pu = [S.ps([128, 512], F32, "pu") for _ in range(2)]
    py = [S.ps([128, 512], F32, "py") for _ in range(2)]
    kkc = [0]
    breg2 = S.reg("pool", Sq - 1)
    prev_scatter = []

    def emit_wq(e2, q):
        for (wsb, wdr) in ((wgq[q], C.wg_d[l, e2]), (wuq_[q], C.wu_d[l, e2])):
            for c in range(NCH):
                st_ = stg[kkc[0] % 8]
                kkc[0] += 1
                S.dma("sp", st_[:], wdr[c * 128:(c + 1) * 128, q * 512:(q + 1) * 512], W=[st_])
                S.copy("pool", wsb, wsb[:, c, :], st_, st_[:])

    def emit_wdn(e2):
        for c in range(16):
            for hc in range(2):
                st_ = stg[kkc[0] % 8]
                kkc[0] += 1
                S.dma("sp", st_[:], C.wd_d[l, e2][c * 128:(c + 1) * 128, hc * 512:(hc + 1) * 512], W=[st_])
                S.copy("pool", wd, wd[:, c, hc * 512:(hc + 1) * 512], st_, st_[:])

    for e_ in range(NE):
        if e_ == 0:
            for q in range(4):
                emit_wq(0, q)
            emit_wdn(0)
        for t in range(NT):
            X = xs[t % 2]
            S.dma("sp", X[:], C.xin_d[e_ * cap + t * 128:e_ * cap + (t + 1) * 128, :], W=[X])
            S.copy("dve", gates, gates[:, t:t + 1], X, X[:, 512 + e_:513 + e_].bitcast(F32))
            S.copy("dve", toks, toks[:, t:t + 1], X, X[:, 528:529])
            for c in range(NCH):
                p_ = ptp[c % 2]
                S.tr(p_, p_[:, 0:128], X, X[:, 0:512].bitcast(BF16)[:, c * 128:(c + 1) * 128], C.ident, C.ident[:])
                S.copy("act" if c % 2 == 0 else "dve", xT, xT[:, c, t * 128:(t + 1) * 128], p_, p_[:, 0:128])
        new_scatter = []
        for fb in range(16):
            q, fo = fb // 4, (fb % 4) * 128
            for s0 in range(0, cap, HS):
                g_, u_, sg_ = pg[(fb * 2 + s0 // HS) % 2], pu[(fb * 2 + s0 // HS) % 2], sg[(fb * 2 + s0 // HS) % 2]
                for c in range(NCH):
                    S.mm(g_, g_[:, 0:HS], wgq[q], wgq[q][:, c, fo:fo + 128], xT, xT[:, c, s0:s0 + HS], c == 0, c == NCH - 1)
                for c in range(NCH):
                    S.mm(u_, u_[:, 0:HS], wuq_[q], wuq_[q][:, c, fo:fo + 128], xT, xT[:, c, s0:s0 + HS], c == 0, c == NCH - 1)
                S.op("act", lambda e, sg_=sg_, g_=g_: e.activation(out=sg_[:], in_=g_[:, 0:HS], func=AF.Silu), R=[g_], W=[sg_])
                S.tt("dve", hid, hid[:, fb, s0:s0 + HS], u_, u_[:, 0:HS], sg_, sg_[:], ALU.mult)
            if fb % 4 == 3 and e_ + 1 < NE:
                emit_wq(e_ + 1, fb // 4)
        if True:
            for tt_ in range(NT):
                Y = yo[tt_ % 2]
                for hf in range(2):
                    p_ = py[hf]
                    for fb in range(16):
                        S.mm(p_, p_[:], hid, hid[:, fb, tt_ * 128:(tt_ + 1) * 128], wd, wd[:, fb, hf * 512:(hf + 1) * 512], fb == 0, fb == 15)
                    S.ts("dve", Y, Y[:, hf * 512:(hf + 1) * 512], p_, p_[:], gates[:, tt_:tt_ + 1], None, ALU.mult, R=[gates])

                def fn(eng, Y=Y, tt_=tt_):
                    return eng.indirect_dma_start(
                        out=x_d[:, :], out_offset=bass.IndirectOffsetOnAxis(ap=toks[:, tt_:tt_ + 1], axis=0),
                        in_=Y[:], in_offset=None, bounds_check=breg2["r"], oob_is_err=False, compute_op=ALU.add)
                tk = S.dma("pool", None, None, R=[Y, toks], sem_buf=Y, fn=fn, extra=prev_scatter)
                new_scatter.append(tk)
        prev_scatter = new_scatter
        if e_ + 1 < NE:
            emit_wdn(e_ + 1)
    S.end()


def stage_FIN(S, C, x_d, out_d):
    Sq = C.S
    S.begin()
    load_consts(S, C)
    gb = S.sb([128, D], F32, "gbf")
    S.dma("sp", gb[:], C.norm_final_d[0, :].partition_broadcast(128), W=[gb])
    xt = [S.sb([128, D], F32, "xf") for _ in range(2)]
    yo = [S.sb([128, D], F32, "yf") for _ in range(2)]
    junk = S.sb([128, D], BF16, "junkf")
    st = [S.sb([128, 2], F32, "stf") for _ in range(2)]
    for t in range(Sq // 128):
        X, Y, st_ = xt[t % 2], yo[t % 2], st[t % 2]
        S.dma("sp", X[:], x_d[t * 128:(t + 1) * 128, :], W=[X])
        S.op("act", lambda e, X=X, st_=st_: e.activation(out=junk[:], in_=X[:], func=AF.Square, accum_out=st_[:, 0:1]),
             R=[X], W=[junk, st_])
        S.op("act", lambda e, st_=st_: e.activation(out=st_[:, 1:2], in_=st_[:, 0:1], func=AF.Sqrt, bias=C.epsb[:, 0:1], scale=1.0 / D),
             R=[st_, C.epsb], W=[st_])
        S.op("dve", lambda e, st_=st_: e.reciprocal(out=st_[:, 1:2], in_=st_[:, 1:2]), R=[st_], W=[st_])
        S.stt("dve", Y, Y[:], X, X[:], st_[:, 1:2], gb, gb[:], ALU.mult, ALU.mult, R=[st_])
        S.dma("act", out_d[t * 128:(t + 1) * 128, :], Y[:], R=[Y])
    S.end()


def run_stage(S, C, st):
    if st == "P0": stage_P0(S, C, C.x_d)
    elif st == "A": stage_A(S, C)
    elif st == "B": stage_B(S, C)
    elif st == "OUT0": stage_OUT(S, C, 0, C.mixT0_d, C.wout0_d, C.x_d, C.x1_d)
    elif st == "MOE0": stage_MOE(S, C, 0, C.x1_d)
    elif st == "P1": stage_P1(S, C, C.x1_d)
    elif st == "C": stage_C(S, C)
    elif st == "D": stage_D(S, C)
    elif st == "OUT1": stage_OUT(S, C, 1, C.mixT1_d, C.wout1_d, C.x1_d, C.x3_d)
    elif st == "MOE1": stage_MOE(S, C, 1, C.x3_d)
    elif st == "FIN": stage_FIN(S, C, C.x3_d, C.y_d)
    else: raise ValueError(st)


W1C = 3024
O_KR, O_KRR, O_DQ, O_DK, O_G, O_CQ, O_CKV, O_DV, O_DO = 0, 96, 192, 704, 1216, 1232, 1744, 2000, 2512


def host_prep_P1(od_w_in, w_uq, w_ukv):
    w = np.asarray(od_w_in, np.float32)
    cq, ckv, kr, dq, dk, dv, do, gt = np.split(w, np.cumsum([512, 256, 32, 512, 512, 512, 512])[:], axis=1)
    p32 = np.array([(d + 16 if d < 16 else d - 16) for d in range(32)])
    w1 = np.concatenate([cq[:, 0:64], kr, cq[:, 0:64], kr[:, p32], dq, dk, gt, cq, ckv, dv, do], axis=1)
    assert w1.shape[1] == W1C
    uq = np.asarray(w_uq, np.float32)
    perm = np.arange(768)
    for h in range(8):
        for dd in range(32):
            perm[h * 96 + 64 + dd] = h * 96 + 64 + (dd + 16 if dd < 16 else dd - 16)
    wuq = np.concatenate([uq, uq[:, perm]], axis=1)
    ukv = np.asarray(w_ukv, np.float32).reshape(256, 8, 128)
    wukv = np.concatenate([ukv[:, :, 0:64].reshape(256, 512), ukv[:, :, 64:128].reshape(256, 512)], axis=1)
    return np.ascontiguousarray(w1), np.ascontiguousarray(wuq), np.ascontiguousarray(wukv)


def ropec1():
    inv = np.zeros(128, np.float32)
    sgn = np.zeros(128, np.float32)
    fr = (10000.0 ** (-np.arange(16, dtype=np.float32) * 2.0 / 32)).astype(np.float32)
    for p in range(64, 96):
        d = p - 64
        inv[p] = fr[d % 16]
        sgn[p] = -1.0 if d < 16 else 1.0
    return np.stack([inv, sgn], axis=1).astype(np.float32)


def stage_P1(S, C, x_dram):
    Sq = C.S
    NG = Sq // GT
    S.begin()
    load_consts(S, C)
    ident = C.ident
    cst = S.sb([128, 2], F32, "cst1")
    S.dma("sp", cst[:], C.ropec1_d[:, :], W=[cst])
    gv = S.sb([128, NCH], F32, "gv1")
    S.dma("sp", gv[:], C.norm_mix_d[1, :].rearrange("(c p) -> p c", p=128), W=[gv], allow_slow_non_contiguous=True)
    gq = S.sb([128, 4], F32, "gq")
    S.dma("sp", gq[:], C.mla_nq_d[0, :].rearrange("(c p) -> p c", p=128), W=[gq], allow_slow_non_contiguous=True)
    gkv = S.sb([128, 2], F32, "gkv")
    S.dma("sp", gkv[:], C.mla_nkv_d[0, :].rearrange("(c p) -> p c", p=128), W=[gkv], allow_slow_non_contiguous=True)
    gb4 = S.sb([4, 4], F32, "gb4")
    S.dma("sp", gb4[:], C.gbias_d[:, :], W=[gb4])
    C.rt_ang = S.sb([128, GT], F32, "rt_ang")
    C.rt_ki = S.sb([128, GT], I32, "rt_ki")
    C.rt_kf = S.sb([128, GT], F32, "rt_kf")
    w = S.sb([128, NCH, W1C], BF16, "w1")
    stg = load_weights_bf16(S, C.w1_d, W1C, gv, 0, w)
    wuq = S.sb([128, 4, 1536], BF16, "wuq")
    load_weights_bf16(S, C.wuq_d, 1536, gq, 0, wuq, kchunks=4, stg=stg)
    wukv = S.sb([128, 2, 1024], BF16, "wukv")
    load_weights_bf16(S, C.wukv_d, 1024, gkv, 0, wukv, kchunks=2, stg=stg)

    xt = [S.sb([128, 4, D], F32, "xt")] * 2
    junk = S.sb([128, D], BF16, "junk")
    ssq = S.sb([128, 4], F32, "ssq")
    rstd = S.sb([128, 4], F32, "rstd")
    xn = S.sb([128, 4, D], BF16, "xn")
    xnT = [S.sb([128, NCH, GT], BF16, "xnT") for _ in range(2)]
    pT = [S.ps([128, GT], BF16, "pT") for _ in range(2)]
    pz = [S.ps([128, GT], F32, "pz") for _ in range(2)]
    pr = [S.ps([128, GT], F32, "pr") for _ in range(2)]
    pm = [S.ps([128, GT], F32, "pm") for _ in range(2)]
    posi = S.sb([128, GT], I32, "posi")
    posf = S.sb([128, GT], F32, "posf")
    tmp = S.sb([128, GT], F32, "tmp")
    cosC = S.sb([128, GT], F32, "cosC")
    sinC = S.sb([128, GT], F32, "sinC")
    t1 = [S.sb([128, GT], F32, "t1") for _ in range(2)]
    t2 = [S.sb([128, GT], F32, "t2") for _ in range(2)]
    ofm = [S.sb([128, GT], BF16, "ofm") for _ in range(3)]
    oraw = [S.sb([128, GT], F32, "oraw") for _ in range(2)]
    og4 = [S.sb([4, GT], F32, "og4") for _ in range(2)]
    ovd = [S.sb([128, 4, 129], BF16, "ovd") for _ in range(2)]
    for b in ovd:
        S.memset("pool", b, b[:], 1.0)
    ovc = [S.sb([128, 8, 128], BF16, "ovc") for _ in range(2)]
    for b in ovc:
        S.memset("pool", b, b[:], 0.0)
        S.memset("pool", b, b[:, :, 0:1], 1.0)
    ogo = [S.sb([128, 512], F32, "ogo") for _ in range(2)]
    cn4 = S.sb([128, 4, 768], BF16, "cn4")
    cnT = S.sb([128, 6, GT], BF16, "cnT")
    st2 = [S.sb([128, 4], F32, "st2") for _ in range(2)]
    eps2 = C.epsb
    kf = 0
    for g in range(NG):
        X = xnT[g % 2]
        gs = slice(g * GT, (g + 1) * GT)
        norm_transpose_group(S, C, x_dram, g, xt[0], junk, ssq, rstd, xn, pT, X, ident)
        rope_tables(S, C, C.pos_d, g, posi, posf, [(cst, cst[:, 0:1], cst[:, 1:2], [(cosC, sinC, 1.0)])], tmp, None)
        z, r = pz[kf % 2], pr[kf % 2]
        for c in range(NCH):
            S.mm(z, z[0:96, :], w, w[:, c, O_KR:O_KR + 96], X, X[:, c, :], c == 0, c == NCH - 1)
        for c in range(NCH):
            S.mm(r, r[0:96, :], w, w[:, c, O_KRR:O_KRR + 96], X, X[:, c, :], c == 0, c == NCH - 1)
        a, b2, o = t1[kf % 2], t2[kf % 2], ofm[kf % 3]
        kf += 1
        S.tt("dve", a, a[64:96, :], z, z[64:96, :], cosC, cosC[64:96, :], ALU.mult)
        S.tt("dve", b2, b2[64:96, :], r, r[64:96, :], sinC, sinC[64:96, :], ALU.mult)
        S.tt("pool", o, o[64:96, :], a, a[64:96, :], b2, b2[64:96, :], ALU.add)
        for h in range(8):
            S.dma("sp" if h % 2 == 0 else "act", C.fmk_d[h, 64:96, gs], o[64:96, :], R=[o])
        for blk in range(8):
            z = pz[kf % 2]
            orw = oraw[kf % 2]
            kf += 1
            c0 = O_DQ + blk * 128
            for c in range(NCH):
                S.mm(z, z[:], w, w[:, c, c0:c0 + 128], X, X[:, c, :], c == 0, c == NCH - 1)
            S.copy("act" if blk % 2 == 0 else "dve", orw, orw[:], z, z[:])
            S.dma("sp", C.qkraw_d[blk * 128:(blk + 1) * 128, gs], orw[:], R=[orw])
        for ty in range(4):
            z = pr[ty % 2]
            o4 = og4[ty % 2]
            c0 = O_G + ty * 4
            for c in range(NCH):
                S.mm(z, z[0:4, :], w, w[:, c, c0:c0 + 4], X, X[:, c, :], c == 0, c == NCH - 1)
            S.ts("dve", o4, o4[:], z, z[0:4, :], gb4[:, ty:ty + 1], None, ALU.add, R=[gb4])
            S.dma("sp", C.gatesT_d[ty, :, gs], o4[:], R=[o4])
        for j in range(4):
            tok0 = g * GT + j * 128
            pcq, pckv, pdv, pdo = pm[0], pm[1], pz[j % 2], pr[j % 2]
            for (pp, cc, nn) in ((pcq, O_CQ, 512), (pckv, O_CKV, 256), (pdv, O_DV, 512), (pdo, O_DO, 512)):
                for c in range(NCH):
                    S.mm(pp, pp[:, 0:nn], X, X[:, c, j * 128:(j + 1) * 128], w, w[:, c, cc:cc + nn], c == 0, c == NCH - 1)
            s2 = st2[j % 2]
            S.op("act", lambda e, s2=s2, pcq=pcq: e.activation(out=junk[:, 0:512], in_=pcq[:, 0:512], func=AF.Square, accum_out=s2[:, 0:1]),
                 R=[pcq], W=[junk, s2])
            S.op("act", lambda e, s2=s2, pckv=pckv: e.activation(out=junk[:, 0:256], in_=pckv[:, 0:256], func=AF.Square, accum_out=s2[:, 1:2]),
                 R=[pckv], W=[junk, s2])
            S.op("act", lambda e, s2=s2: e.activation(out=s2[:, 2:3], in_=s2[:, 0:1], func=AF.Sqrt, bias=eps2[:, 0:1], scale=1.0 / 512),
                 R=[s2, eps2], W=[s2])
            S.op("act", lambda e, s2=s2: e.activation(out=s2[:, 3:4], in_=s2[:, 1:2], func=AF.Sqrt, bias=eps2[:, 0:1], scale=1.0 / 256),
                 R=[s2, eps2], W=[s2])
            S.op("dve", lambda e, s2=s2: e.reciprocal(out=s2[:, 2:4], in_=s2[:, 2:4]), R=[s2], W=[s2])
            S.ts("dve", cn4, cn4[:, j, 0:512], pcq, pcq[:, 0:512], s2[:, 2:3], None, ALU.mult, R=[s2])
            S.ts("dve", cn4, cn4[:, j, 512:768], pckv, pckv[:, 0:256], s2[:, 3:4], None, ALU.mult, R=[s2])
            vd, go = ovd[j % 2], ogo[j % 2]
            S.copy("act", vd, vd[:, :, 0:128], pdv, pdv[:].rearrange("p (h d) -> p h d", h=4))
            S.op("act", lambda e, go=go, pdo=pdo: e.activation(out=go[:], in_=pdo[:], func=AF.Sigmoid), R=[pdo], W=[go])
            S.dma("pool", C.vD_d[tok0:tok0 + 128, :], vd[:].rearrange("p h d -> p (h d)"), R=[vd])
            S.dma("pool", C.og_d[tok0:tok0 + 128, :], go[:], R=[go])
        for c in range(6):
            p = pT[c % 2]
            for j in range(4):
                S.tr(p, p[:, j * 128:(j + 1) * 128], cn4, cn4[:, j, c * 128:(c + 1) * 128], ident, ident[:])
            S.copy("act" if c % 2 == 0 else "dve", cnT, cnT[:, c, :], p, p[:])
        for h in range(8):
            z, r = pz[kf % 2], pr[kf % 2]
            a, b2, o = t1[kf % 2], t2[kf % 2], ofm[kf % 3]
            kf += 1
            for c in range(4):
                S.mm(z, z[0:96, :], wuq, wuq[:, c, h * 96:(h + 1) * 96], cnT, cnT[:, c, :], c == 0, c == 3)
            for c in range(4):
                S.mm(r, r[0:96, :], wuq, wuq[:, c, 768 + h * 96:768 + (h + 1) * 96], cnT, cnT[:, c, :], c == 0, c == 3)
            S.copy("act", o, o[0:64, :], z, z[0:64, :])
            S.tt("dve", a, a[64:96, :], z, z[64:96, :], cosC, cosC[64:96, :], ALU.mult)
            S.tt("dve", b2, b2[64:96, :], r, r[64:96, :], sinC, sinC[64:96, :], ALU.mult)
            S.tt("dve", o, o[64:96, :], a, a[64:96, :], b2, b2[64:96, :], ALU.add)
            S.dma("sp", C.fmq_d[h, :, gs], o[0:96, :], R=[o])
        for h in range(8):
            z = pm[h % 2]
            o = ofm[kf % 3]
            kf += 1
            for c in range(2):
                S.mm(z, z[0:64, :], wukv, wukv[:, c, h * 64:(h + 1) * 64], cnT, cnT[:, 4 + c, :], c == 0, c == 1)
            S.copy("act" if h % 2 == 0 else "dve", o, o[0:64, :], z, z[0:64, :])
            S.dma("act", C.fmk_d[h, 0:64, gs], o[0:64, :], R=[o])
        for j in range(4):
            tok0 = g * GT + j * 128
            z = pz[j % 2]
            vc = ovc[j % 2]
            for c in range(2):
                S.mm(z, z[:], cnT, cnT[:, 4 + c, j * 128:(j + 1) * 128], wukv, wukv[:, c, 512:1024], c == 0, c == 1)
            S.copy("dve", vc, vc[:, :, 64:128], z, z[:].rearrange("p (h d) -> p h d", h=8))
            S.dma("pool", C.vC_d[tok0:tok0 + 128, :], vc[:].rearrange("p h d -> p (h d)"), R=[vc])
    S.end()


def stage_C(S, C):
    Sq = C.S
    N = Sq // 128
    NG = Sq // GT
    scale = 96 ** -0.5
    S.begin()
    load_consts(S, C)
    Qh = [S.sb([96, Sq], BF16, "Qh") for _ in range(2)]
    Kh = [S.sb([96, Sq], BF16, "Kh") for _ in range(2)]
    Vh = [S.sb([128, N, 128], BF16, "Vh") for _ in range(2)]
    sq = [S.sb([96, GT], F32, "sqc") for _ in range(2)]
    acc = S.sb([1, 2, GT], F32, "accc")
    mq = S.sb([1, 4], F32, "mqc")
    negM = [S.sb([128, 1], F32, "negMc") for _ in range(2)]
    pn = S.ps([128, GT], F32, "pnc")
    pss = [S.ps([128, 512], F32, "pssc") for _ in range(4)]
    pso = [S.ps([128, 512], F32, "psoc") for _ in range(2)]
    pts = [S.sb([128, 512], BF16, "ptc") for _ in range(4)]
    den = [S.sb([1, 512], F32, "denc") for _ in range(2)]
    rdb = [S.sb([128, 512], F32, "rdbc") for _ in range(2)]
    osb = [S.sb([128, 512], BF16, "osbc") for _ in range(2)]
    ks = 0
    it = 0
    for h in range(8):
        Q, K, V, nM = Qh[h % 2], Kh[h % 2], Vh[h % 2], negM[h % 2]
        for h0 in range(0, Sq, 2048):
            w_ = min(2048, Sq - h0)
            S.dma("sp", Q[:, h0:h0 + w_], C.fmq_d[h, :, h0:h0 + w_], W=[Q])
            S.dma("act", K[:, h0:h0 + w_], C.fmk_d[h, :, h0:h0 + w_], W=[K])
        S.dma("pool", V[:], C.vC_d[:, h * 128:(h + 1) * 128].rearrange("(n p) c -> p n c", p=128), W=[V])
        S.memset("dve", acc, acc[:], 0.0)
        i = 0
        for ai, T in enumerate((Q, K)):
            for g in range(NG):
                s_ = sq[i % 2]
                i += 1
                S.op("act", lambda e, s_=s_, T=T, g=g: e.activation(out=s_[:], in_=T[:, g * GT:(g + 1) * GT], func=AF.Square),
                     R=[T], W=[s_])
                S.mm(pn, pn[0:1, :], C.onesf, C.onesf[0:96, 0:1], s_, s_[:, :], True, True)
                S.tt("dve", acc, acc[:, ai, :], pn, pn[0:1, :], acc, acc[:, ai, :], ALU.max)
        S.op("dve", lambda e: e.tensor_reduce(out=mq[:, 0:2], in_=acc[:], axis=AX.X, op=ALU.max), R=[acc], W=[mq])
        S.tt("dve", mq, mq[:, 2:3], mq, mq[:, 0:1], mq, mq[:, 1:2], ALU.mult)
        S.op("act", lambda e: e.activation(out=mq[:, 3:4], in_=mq[:, 2:3], func=AF.Sqrt), R=[mq], W=[mq])
        S.ts("dve", mq, mq[:, 3:4], mq, mq[:, 3:4], -scale, None, ALU.mult)
        bcast_row_to_parts(S, C, mq, mq[0:1, 3:4], nM, nM[:], pn, 1)
        for g in range(NG):
            po = pso[it % 2]
            pend = []
            LAG = 2
            for m in range(N):
                ps_ = pss[ks % 4]
                pt = pts[ks % 4]
                ks += 1
                S.mm(ps_, ps_[:], K, K[:, m * 128:(m + 1) * 128], Q, Q[:, g * GT:(g + 1) * GT], True, True)
                S.op("act", lambda e, pt=pt, ps_=ps_, nM=nM: e.activation(out=pt[:], in_=ps_[:], func=AF.Exp, bias=nM[:, 0:1], scale=scale),
                     R=[ps_, nM], W=[pt])
                pend.append((m, pt))
                if len(pend) > LAG:
                    m2, pt2 = pend.pop(0)
                    S.mm(po, po[:, :], V, V[:, m2, :], pt2, pt2[:], m2 == 0, m2 == N - 1)
            for (m2, pt2) in pend:
                S.mm(po, po[:, :], V, V[:, m2, :], pt2, pt2[:], m2 == 0, m2 == N - 1)
            dn, rb, ob = den[it % 2], rdb[it % 2], osb[it % 2]
            S.op("dve", lambda e, dn=dn, po=po: e.reciprocal(out=dn[0:1, :], in_=po[0:1, :]), R=[po], W=[dn])
            pb = pss[ks % 4]
            ks += 1
            S.mm(pb, pb[:, :], C.onesf, C.onesf[0:1, :], dn, dn[0:1, :], True, True)
            S.copy("act", rb, rb[64:128, :], pb, pb[64:128, :])
            S.tt("dve", ob, ob[64:128, :], po, po[64:128, :], rb, rb[64:128, :], ALU.mult)
            S.dma("sp", C.mixT1_d[h * 64:(h + 1) * 64, g * GT:(g + 1) * GT], ob[64:128, :], R=[ob])
            it += 1
    S.end()


def _v3(ap):
    return ap.rearrange("p (n l) -> p n l", l=128)


def logstep3(S, bufs, cur, op, suffix, L=128):
    s = 1
    while s < L:
        a, b = bufs[cur], bufs[1 - cur]
        a3, b3 = _v3(a[:]), _v3(b[:])
        if not suffix:
            S.copy("pool", b, b3[:, :, 0:s], a, a3[:, :, 0:s])
            S.tt("dve", b, b3[:, :, s:L], a, a3[:, :, s:L], a, a3[:, :, 0:L - s], op)
        else:
            S.copy("pool", b, b3[:, :, L - s:L], a, a3[:, :, L - s:L])
            S.tt("dve", b, b3[:, :, 0:L - s], a, a3[:, :, 0:L - s], a, a3[:, :, s:L], op)
        cur = 1 - cur
        s *= 2
    return cur


def logstep2(S, bufs, cur, op, suffix, N):
    s = 1
    while s < N:
        a, b = bufs[cur], bufs[1 - cur]
        if not suffix:
            S.copy("pool", b, b[:, 0:s], a, a[:, 0:s])
            S.tt("dve", b, b[:, s:N], a, a[:, s:N], a, a[:, 0:N - s], op)
        else:
            S.copy("pool", b, b[:, N - s:N], a, a[:, N - s:N])
            S.tt("dve", b, b[:, 0:N - s], a, a[:, 0:N - s], a, a[:, s:N], op)
        cur = 1 - cur
        s *= 2
    return cur


def stage_D(S, C):
    Sq = C.S
    N = Sq // 128
    L = 128
    NEGB = -1.0e30
    S.begin()
    load_consts(S, C)
    cw = S.sb([128, 8, 5], F32, "cw")
    S.dma("sp", cw[:].rearrange("p b w -> p (b w)"), C.conv_d[:, :], W=[cw])
    raw = [S.sb([128, Sq], F32, "raw") for _ in range(2)]
    acc = S.sb([128, Sq], F32, "cacc")
    sil = S.sb([128, Sq], F32, "sil")
    ocv = [S.sb([128, Sq], BF16, "ocv") for _ in range(2)]
    for blk in range(8):
        R, O = raw[blk % 2], ocv[blk % 2]
        for h0 in range(0, Sq, 2048):
            w_ = min(2048, Sq - h0)
            S.dma("sp" if (h0 // 2048) % 2 == 0 else "act", R[:, h0:h0 + w_], C.qkraw_d[blk * 128:(blk + 1) * 128, h0:h0 + w_], W=[R])
        S.ts("dve", acc, acc[:], R, R[:], cw[:, blk, 2:3], None, ALU.mult, R=[cw])
        for wi in (0, 1, 3, 4):
            sh = wi - 2
            if sh > 0:
                S.stt("dve", acc, acc[:, 0:Sq - sh], R, R[:, sh:Sq], cw[:, blk, wi:wi + 1], acc, acc[:, 0:Sq - sh], ALU.mult, ALU.add, R=[cw])
            else:
                S.stt("dve", acc, acc[:, -sh:Sq], R, R[:, 0:Sq + sh], cw[:, blk, wi:wi + 1], acc, acc[:, -sh:Sq], ALU.mult, ALU.add, R=[cw])
        if blk < 4:
            S.op("act", lambda e, O=O: e.activation(out=O[:], in_=acc[:], func=AF.Silu), R=[acc], W=[O])
        else:
            S.op("act", lambda e: e.activation(out=sil[:], in_=acc[:], func=AF.Silu), R=[acc], W=[sil])
            S.ts("pool", O, O[:], sil, sil[:], 128 ** -0.5, None, ALU.mult)
        for h0 in range(0, Sq, 2048):
            w_ = min(2048, Sq - h0)
            S.dma("sp", C.qkc_d[blk, :, h0:h0 + w_], O[:, h0:h0 + w_], R=[O])
    S.end()
    S.begin()
    load_consts(S, C)
    TPG = 256
    G = Sq // TPG
    P_ = 4 * G
    J = TPG // L

    def fold(ap2):
        return ap2.rearrange("h (g t) -> (h g) t", t=TPG)

    def foldn(ap2):
        return ap2.rearrange("h (g j) -> (h g) j", j=J)

    def bc(t):
        return t[:].unsqueeze(2).to_broadcast([P_, J, L])
    gi = S.sb([P_, TPG], F32, "gi")
    gf = S.sb([P_, TPG], F32, "gf")
    bb = [S.sb([P_, TPG], F32, "bb") for _ in range(2)]
    cc = S.sb([P_, TPG], F32, "cc")
    cm = [S.sb([P_, TPG], F32, "cm") for _ in range(2)]
    aa = S.sb([P_, TPG], F32, "aa")
    mmx = S.sb([P_, TPG], F32, "mmx")
    ov = [S.sb([P_, TPG], F32, "ov") for _ in range(3)]
    g_ = S.sb([P_, J], F32, "g_")
    amax = S.sb([P_, J], F32, "amax")
    mPf = S.sb([P_, J], F32, "mPf")
    g4 = S.sb([4, N], F32, "g4")
    am4 = S.sb([4, N], F32, "am4")
    GG = [S.sb([4, N], F32, "GG") for _ in range(2)]
    PP = [S.sb([4, N], F32, "PP") for _ in range(2)]
    mN = S.sb([4, N], F32, "mN")
    mP = S.sb([4, N], F32, "mP")
    spc = [S.sb([4, N], F32, "spc") for _ in range(2)]
    kv = 0
    for dirn in (0, 1):
        suffix = (dirn == 1)
        dch = Buf(None, "dch%d" % dirn)
        dmp = Buf(None, "dmp%d" % dirn)
        S.dma("sp", gi[:], fold(C.gatesT_d[2 * dirn, :, :]), W=[gi])
        S.dma("act", gf[:], fold(C.gatesT_d[2 * dirn + 1, :, :]), W=[gf])
        S.op("act", lambda e: e.activation(out=bb[0][:], in_=gf[:], func=AF.Exp, scale=-1.0), R=[gf], W=[bb[0]])
        S.ts("dve", bb[0], bb[0][:], bb[0], bb[0][:], 1.0, None, ALU.add)
        S.op("act", lambda e: e.activation(out=bb[0][:], in_=bb[0][:], func=AF.Ln), R=[bb[0]], W=[bb[0]])
        S.ts("dve", bb[0], bb[0][:], bb[0], bb[0][:], -1.0, None, ALU.mult)
        cb = logstep3(S, bb, 0, ALU.add, suffix)
        B_ = bb[cb]
        B3 = _v3(B_[:])
        gcol = (L - 1) if not suffix else 0
        S.copy("dve", g_, g_[:], B_, B3[:, :, gcol])
        S.tt("dve", cc, cc[:], gi, gi[:], B_, B_[:], ALU.subtract)
        S.tt("dve", aa, _v3(aa[:]), cc, _v3(cc[:]), g_, bc(g_), ALU.add)
        S.op("dve", lambda e: e.tensor_reduce(out=amax[:], in_=_v3(aa[:]), axis=AX.X, op=ALU.max), R=[aa], W=[amax])
        S.dma("sp", foldn(C.chunk_d[dirn * 2 + 0, :, :]), g_[:], R=[g_], W=[dch])
        S.dma("sp", foldn(C.chunk_d[dirn * 2 + 1, :, :]), amax[:], R=[amax], W=[dch])
        wv = ov[kv % 3]
        kv += 1
        S.tt("dve", aa, _v3(aa[:]), aa, _v3(aa[:]), amax, bc(amax), ALU.subtract)
        S.op("act", lambda e, wv=wv: e.activation(out=wv[:], in_=aa[:], func=AF.Exp), R=[aa], W=[wv])
        S.dma("sp", fold(C.vec_d[dirn * 5 + 0, :, :]), wv[:], R=[wv])
        e1 = ov[kv % 3]
        kv += 1
        S.op("act", lambda e, e1=e1: e.activation(out=e1[:], in_=cc[:], func=AF.Exp), R=[cc], W=[e1])
        S.dma("sp", fold(C.vec_d[dirn * 5 + 1, :, :]), e1[:], R=[e1])
        S.copy("pool", cm[0], cm[0][:], cc, cc[:])
        ci = logstep3(S, cm, 0, ALU.max, suffix)
        if suffix:
            a3, b3 = _v3(cm[ci][:]), _v3(cm[1 - ci][:])
            S.copy("pool", cm[1 - ci], b3[:, :, 0:L - 1], cm[ci], a3[:, :, 1:L])
            S.memset("pool", cm[1 - ci], b3[:, :, L - 1:L], NEGB)
            ci = 1 - ci
        CM = cm[ci]
        S.dma("sp", g4[:], C.chunk_d[dirn * 2 + 0, :, :], R=[dch], W=[g4])
        S.dma("sp", am4[:], C.chunk_d[dirn * 2 + 1, :, :], R=[dch], W=[am4])
        S.copy("pool", GG[0], GG[0][:], g4, g4[:])
        gi_ = logstep2(S, GG, 0, ALU.add, suffix, N)
        G_ = GG[gi_]
        S.tt("dve", PP[0], PP[0][:], am4, am4[:], G_, G_[:], ALU.subtract)
        pi_ = logstep2(S, PP, 0, ALU.max, suffix, N)
        S.ts("dve", mN, mN[:], PP[pi_], PP[pi_][:], 0.0, None, ALU.max)
        S.tt("dve", mN, mN[:], mN, mN[:], G_, G_[:], ALU.add)
        S.memset("pool", mP, mP[:], 0.0)
        if not suffix:
            S.copy("pool", mP, mP[:, 1:N], mN, mN[:, 0:N - 1])
        else:
            S.copy("pool", mP, mP[:, 0:N - 1], mN, mN[:, 1:N])
        S.tt("dve", spc[0], spc[0][:], g4, g4[:], mP, mP[:], ALU.add)
        S.tt("dve", spc[0], spc[0][:], spc[0], spc[0][:], mN, mN[:], ALU.subtract)
        S.op("act", lambda e: e.activation(out=spc[0][:], in_=spc[0][:], func=AF.Exp), R=[spc[0]], W=[spc[0]])
        S.tt("dve", spc[1], spc[1][:], am4, am4[:], mN, mN[:], ALU.subtract)
        S.op("act", lambda e: e.activation(out=spc[1][:], in_=spc[1][:], func=AF.Exp), R=[spc[1]], W=[spc[1]])
        S.dma("sp", C.vec2_d[dirn * 2 + 0, :, :], spc[0][:], R=[spc[0]])
        S.dma("sp", C.vec2_d[dirn * 2 + 1, :, :], spc[1][:], R=[spc[1]])
        S.dma("sp", C.mp_d[dirn, :, :], mP[:], R=[mP], W=[dmp])
        S.dma("sp", mPf[:], foldn(C.mp_d[dirn, :, :]), R=[dmp], W=[mPf])
        S.tt("dve", mmx, _v3(mmx[:]), CM, _v3(CM[:]), mPf, bc(mPf), ALU.max)
        e2 = ov[kv % 3]
        kv += 1
        S.op("act", lambda e, e2=e2: e.activation(out=e2[:], in_=mmx[:], func=AF.Exp, scale=-1.0), R=[mmx], W=[e2])
        S.dma("sp", fold(C.vec_d[dirn * 5 + 2, :, :]), e2[:], R=[e2])
        iw = ov[kv % 3]
        kv += 1
        S.tt("dve", iw, _v3(iw[:]), mmx, _v3(mmx[:]), mPf, bc(mPf), ALU.subtract)
        S.op("act", lambda e, iw=iw: e.activation(out=iw[:], in_=iw[:], func=AF.Exp, scale=-1.0), R=[iw], W=[iw])
        S.dma("sp", fold(C.vec_d[dirn * 5 + 3, :, :]), iw[:], R=[iw])
        em = ov[kv % 3]
        kv += 1
        S.tt("dve", em, em[:], mmx, mmx[:], B_, B_[:], ALU.add)
        S.op("act", lambda e, em=em: e.activation(out=em[:], in_=em[:], func=AF.Exp, scale=-1.0), R=[em], W=[em])
        S.dma("sp", fold(C.vec_d[dirn * 5 + 4, :, :]), em[:], R=[em])
    S.end()
    S.begin()
    load_consts(S, C)
    identf = S.sb([128, 128], F32, "identfD")
    S.dma("sp", identf[:], C.identf_d[:, :], W=[identf])
    mk2 = S.sb([128, 2, 128], F32, "mk2")
    S.dma("sp", mk2[:], C.maskD_d[:, :, :], W=[mk2])
    VW = min(2048, Sq)
    VT = [S.sb([40, VW], F32, "VT") for _ in range(2)]
    tv = S.sb([128, N, 40], F32, "tv")
    pu = [S.ps([128, 129], F32, "pud") for _ in range(2)]
    for h0 in range(0, Sq, VW):
        V_ = VT[(h0 // VW) % 2]
        S.dma("sp", V_[:], C.vec_d.rearrange("a h s -> (a h) s")[:, h0:h0 + VW], W=[V_])
        for nn in range(VW // 128):
            n = h0 // 128 + nn
            p_ = pu[n % 2]
            S.tr(p_, p_[:, 0:40], V_, V_[:, nn * 128:(nn + 1) * 128], identf, identf[0:40, 0:40])
            S.copy("act" if n % 2 == 0 else "dve", tv, tv[:, n, :], p_, p_[:, 0:40])
    spsc = S.sb([128, 16 * N], F32, "spsc")
    S.dma("sp", spsc[:], C.vec2_d.rearrange("a h n -> (a h n)").partition_broadcast(128), W=[spsc])

    def vcol(dirn, k, h):
        return (dirn * 5 + k) * 4 + h

    qT = S.sb([128, Sq], BF16, "qTd")
    kT = S.sb([128, Sq], BF16, "kTd")
    Kt = S.sb([128, N, 128], BF16, "Ktd")
    Va = S.sb([128, N, 129], BF16, "Vad")
    Og4 = [S.sb([128, 4, 128], F32, "Ogd") for _ in range(2)]
    Cst = S.sb([128, 129], F32, "Cst")
    Cp = [S.sb([128, N, 129], BF16, "Cp%d" % d) for d in range(2)]
    pk = [S.ps([128, 128], BF16, "pkd") for _ in range(2)]
    pS = S.ps([128, 128], F32, "pSd")
    pA = [S.ps([128, 2, 129], F32, "pAd") for _ in range(2)]
    vw = [S.sb([128, 129], BF16, "vw") for _ in range(2)]
    tU = [S.sb([128, 129], F32, "tU") for _ in range(2)]
    sF = [S.sb([128, 128], BF16, "sF") for _ in range(2)]
    sB = [S.sb([128, 128], BF16, "sB") for _ in range(2)]
    vF = [S.sb([128, 129], BF16, "vF") for _ in range(2)]
    vB = [S.sb([128, 129], BF16, "vB") for _ in range(2)]
    t1 = [S.sb([128, 129], F32, "t1d") for _ in range(2)]
    tot = [S.sb([128, 129], F32, "totd") for _ in range(2)]
    dn = [S.sb([128, 2], F32, "dnd") for _ in range(2)]
    hacc = [S.sb([128, 128], F32, "hacc") for _ in range(2)]
    ob = [S.sb([128, 128], BF16, "obd") for _ in range(2)]
    oT = [S.sb([128, 512], BF16, "oTd") for _ in range(2)]
    for h in range(4):
        for h0 in range(0, Sq, 2048):
            w_ = min(2048, Sq - h0)
            S.dma("sp", qT[:, h0:h0 + w_], C.qkc_d[h, :, h0:h0 + w_], W=[qT])
            S.dma("act", kT[:, h0:h0 + w_], C.qkc_d[4 + h, :, h0:h0 + w_], W=[kT])
        S.dma("pool", Va[:], C.vD_d[:, h * 129:(h + 1) * 129].rearrange("(n p) c -> p n c", p=128), W=[Va])
        for n in range(N):
            p_ = pk[n % 2]
            S.tr(p_, p_[:], kT, kT[:, n * 128:(n + 1) * 128], C.ident, C.ident[:])
            S.copy("act" if n % 2 == 0 else "dve", Kt, Kt[:, n, :], p_, p_[:])
        for dirn in (0, 1):
            S.memset("dve", Cst, Cst[:], 0.0)
            order = range(N) if dirn == 0 else range(N - 1, -1, -1)
            for n in order:
                vw_, pu_, tU_ = vw[n % 2], pu[n % 2], tU[n % 2]
                cw_ = vcol(dirn, 0, h)
                S.op("act", lambda e, vw_=vw_, n=n, cw_=cw_: e.activation(out=vw_[:], in_=Va[:, n, :], func=AF.Copy, scale=tv[:, n, cw_:cw_ + 1]),
                     R=[Va, tv], W=[vw_])
                S.mm(pu_, pu_[:], Kt, Kt[:, n, :], vw_, vw_[:], True, True)
                S.copy("pool", Cp[dirn], Cp[dirn][:, n, :], Cst, Cst[:])
                isp = ((dirn * 2 + 0) * 4 + h) * N + n
                isc = ((dirn * 2 + 1) * 4 + h) * N + n
                S.ts("dve", tU_, tU_[:], pu_, pu_[:], spsc[:, isc:isc + 1], None, ALU.mult, R=[spsc])
                S.stt("dve", Cst, Cst[:], Cst, Cst[:], spsc[:, isp:isp + 1], tU_, tU_[:], ALU.mult, ALU.add, R=[spsc])
        for n in range(N):
            tok = slice(n * 128, (n + 1) * 128)
            i2 = n % 2
            Og = Og4[(n // 4) % 2]
            if n % 4 == 0:
                S.dma("pool", Og[:], C.og_d[n * 128:(n + 4) * 128, h * 128:(h + 1) * 128].rearrange("(n p) c -> p n c", p=128), W=[Og])
            S.mm(pS, pS[:], kT, kT[:, tok], qT, qT[:, tok], True, True)
            S.tt("dve", sF[i2], sF[i2][:], pS, pS[:], mk2, mk2[:, 0, :], ALU.mult)
            S.tt("dve", sB[i2], sB[i2][:], pS, pS[:], mk2, mk2[:, 1, :], ALU.mult)
            ha = hacc[i2]
            for dirn in (0, 1):
                s_ = (sF if dirn == 0 else sB)[i2]
                v_ = (vF if dirn == 0 else vB)[i2]
                pa = pA[dirn]
                c1, c2, c3, c4 = vcol(dirn, 1, h), vcol(dirn, 2, h), vcol(dirn, 3, h), vcol(dirn, 4, h)
                S.op("act", lambda e, v_=v_, n=n, c1=c1: e.activation(out=v_[:], in_=Va[:, n, :], func=AF.Copy, scale=tv[:, n, c1:c1 + 1]),
                     R=[Va, tv], W=[v_])
                S.mm(pa, pa[:, 0, :], s_, s_[:], v_, v_[:], True, True)
                S.mm(pa, pa[:, 1, :], qT, qT[:, tok], Cp[dirn], Cp[dirn][:, n, :], True, True)
                t_, T_, d_ = t1[dirn], tot[dirn], dn[dirn]
                S.ts("dve", t_, t_[:], pa, pa[:, 1, :], tv[:, n, c3:c3 + 1], None, ALU.mult, R=[tv])
                S.stt("dve", T_, T_[:], pa, pa[:, 0, :], tv[:, n, c2:c2 + 1], t_, t_[:], ALU.mult, ALU.add, R=[tv])
                S.stt("dve", d_, d_[:, 0:1], T_, T_[:, 128:129], -1.0, T_, T_[:, 128:129], ALU.mult, ALU.max)
                S.tt("dve", d_, d_[:, 0:1], d_, d_[:, 0:1], tv, tv[:, n, c4:c4 + 1], ALU.max)
                S.op("dve", lambda e, d_=d_: e.reciprocal(out=d_[:, 1:2], in_=d_[:, 0:1]), R=[d_], W=[d_])
                if dirn == 0:
                    S.ts("dve", ha, ha[:], T_, T_[:, 0:128], d_[:, 1:2], None, ALU.mult, R=[d_])
                else:
                    S.stt("dve", ha, ha[:], T_, T_[:, 0:128], d_[:, 1:2], ha, ha[:], ALU.mult, ALU.add, R=[d_])
            o_ = ob[i2]
            S.tt("pool", o_, o_[:], ha, ha[:], Og, Og[:, n % 4, :], ALU.mult)
            pt_ = pk[i2]
            S.tr(pt_, pt_[:], o_, o_[:], C.ident, C.ident[:])
            oT_ = oT[(n // 4) % 2]
            S.copy("act", oT_, oT_[:, (n % 4) * 128:(n % 4 + 1) * 128], pt_, pt_[:])
            if n % 4 == 3:
                S.dma("sp", C.mixT1_d[512 + h * 128:512 + (h + 1) * 128, (n - 3) * 128:(n + 1) * 128], oT_[:], R=[oT_])
    S.end()


STAGES = ("P0", "A", "B", "OUT0", "MOE0", "P1", "C", "D", "OUT1", "MOE1", "FIN")


def build_program(Sq):
    nc = bass.Bass("TRN2", target_bir_lowering=False)
    C = Ctx()
    declare(nc, C, Sq, debug=False)
    S = Sched(nc)
    for st in STAGES:
        run_stage(S, C, st)
    S.close()
    return nc


def kernel(**inputs):
    from concourse.bass_utils import run_bass_kernel_spmd
    inp = {k: np.asarray(v) for k, v in inputs.items()}
    B, Sq, _ = inp["x"].shape
    nc = build_program(Sq)
    shared = host_inputs(inp, 0)
    in_maps = []
    for b in range(B):
        d = dict(shared)
        d["x"] = np.ascontiguousarray(inp["x"][b], dtype=np.float32)
        d["pos"] = np.ascontiguousarray(inp["positions"][b]).astype(np.int32)
        in_maps.append(d)
    res = run_bass_kernel_spmd(nc, in_maps, core_ids=list(range(B)))
    return np.stack([np.asarray(r["y"], dtype=np.float32) for r in res.results], axis=0)
```

```python
import numpy as np
from contextlib import ExitStack
import concourse.bass as bass
import concourse.mybir as mybir

F32 = mybir.dt.float32
BF16 = mybir.dt.bfloat16
I32 = mybir.dt.int32
AF = mybir.ActivationFunctionType
ALU = mybir.AluOpType
AX = mybir.AxisListType


class Buf:
    __slots__ = ("t", "w", "r", "dsem", "name")

    def __init__(self, t, name=""):
        self.t = t
        self.w = None
        self.r = []
        self.dsem = None
        self.name = name

    def __getitem__(self, k):
        return self.t[k]


class Eng:
    def __init__(self, name, eng, sem, inorder=False):
        self.name, self.eng, self.sem = name, eng, sem
        self.count = 0
        self.seen = {}
        self.ops = []
        self.inorder = inorder


class Sched:
    def __init__(self, nc):
        self.nc = nc
        self.es = ExitStack()
        self.E = {}
        for name, eng, ino in (("pe", nc.tensor, True), ("act", nc.scalar, False),
                               ("dve", nc.vector, False), ("pool", nc.gpsimd, False),
                               ("sp", nc.sync, False)):
            sem = self.es.enter_context(nc.semaphore("e_" + name))
            self.E[name] = Eng(name, eng, sem, ino)
        self.dsems = []
        self.free_ds = {"hw": [], "sw": []}
        self.stage_es = None
        self.stage_ds = []
        self.nbuf = 0

    def _get_dsem(self, kind):
        if self.free_ds[kind]:
            i = self.free_ds[kind].pop()
        else:
            sem = self.es.enter_context(self.nc.semaphore("d%d" % len(self.dsems)))
            self.dsems.append([sem, 0])
            i = len(self.dsems) - 1
        self.stage_ds.append((kind, i))
        return i

    def begin(self):
        self.stage_es = ExitStack()
        self.stage_ds = []

    def sb(self, shape, dt, name=None):
        self.nbuf += 1
        name = (name or "b") + "_%d" % self.nbuf
        t = self.stage_es.enter_context(self.nc.sbuf_tensor(name, list(shape), dt))
        return Buf(t, name)

    def ps(self, shape, dt=F32, name=None):
        self.nbuf += 1
        name = (name or "p") + "_%d" % self.nbuf
        t = self.stage_es.enter_context(self.nc.psum_tensor(name, list(shape), dt))
        return Buf(t, name)

    def end(self):
        self.barrier()
        with self.nc.Block() as block:
            for name, sect in (("pe", block.tensor), ("act", block.scalar), ("dve", block.vector),
                               ("pool", block.gpsimd), ("sp", block.sync)):
                E = self.E[name]
                ops = E.ops
                E.ops = []

                def body(eng, ops=ops):
                    for waits, fn, inc in ops:
                        for (sem, val) in waits:
                            eng.wait_ge(sem, val)
                        if fn is not None:
                            ins = fn(eng)
                            if inc is not None:
                                ins.then_inc(inc[0], inc[1])
                sect(body)
        for kind, i in self.stage_ds:
            self.free_ds[kind].append(i)
        self.stage_ds = []
        self.stage_es.close()
        self.stage_es = None

    def barrier(self):
        toks = [(E.sem, E.count) for E in self.E.values() if E.count > 0]
        toks += [(s, v) for (s, v) in self.dsems if v > 0]
        for E in self.E.values():
            w = self._filter(E, toks, barrier=True)
            if w:
                E.ops.append((w, None, None))

    def _filter(self, E, toks, barrier=False):
        best = {}
        for (sem, val) in toks:
            if sem is E.sem and (E.inorder or barrier):
                continue
            k = id(sem)
            if E.seen.get(k, 0) >= val:
                continue
            if k not in best or best[k][1] < val:
                best[k] = (sem, val)
        out = []
        for k, (sem, val) in best.items():
            E.seen[k] = val
            out.append((sem, val))
        return out

    def _deps(self, R, W, skip_sem=None):
        toks = []
        for b in R:
            if b.w is not None:
                toks.append(b.w)
        for b in W:
            if b.w is not None and not (skip_sem is not None and b.w[0] is skip_sem):
                toks.append(b.w)
            toks.extend(b.r)
        return toks

    def op(self, ename, fn, R=(), W=()):
        E = self.E[ename]
        waits = self._filter(E, self._deps(R, W))
        E.count += 1
        tok = (E.sem, E.count)
        E.ops.append((waits, fn, (E.sem, 1)))
        for b in R:
            b.r.append(tok)
        for b in W:
            b.w = tok
            b.r = []
        return tok

    def dma(self, qname, out, in_, R=(), W=(), sem_buf=None, fn=None, extra=(), **kw):
        E = self.E[qname]
        sb = sem_buf if sem_buf is not None else (W[0] if W else R[0])
        kind = "sw" if qname == "pool" else "hw"
        if sb.dsem is None:
            sb.dsem = {}
        if kind not in sb.dsem:
            sb.dsem[kind] = self._get_dsem(kind)
        ds = self.dsems[sb.dsem[kind]]
        waits = self._filter(E, self._deps(R, W, skip_sem=ds[0]) + list(extra))
        ds[1] += 16
        tok = (ds[0], ds[1])
        if fn is None:
            def fn(eng, out=out, in_=in_, kw=kw):
                return eng.dma_start(out=out, in_=in_, **kw)
        E.ops.append((waits, fn, (ds[0], 16)))
        for b in R:
            b.r.append(tok)
        for b in W:
            b.w = tok
            b.r = []
        return tok

    def reg(self, ename, value):
        holder = {}

        def fn(eng):
            holder["r"] = eng.alloc_register("rg%d" % id(holder))
            return eng.reg_mov(holder["r"], value)
        self.E[ename].ops.append(([], fn, None))
        return holder

    def mm(self, out_b, out_ap, lhsT_b, lhsT, rhs_b, rhs, start, stop):
        self.op("pe", lambda e: e.matmul(out_ap, lhsT, rhs, start=start, stop=stop),
                R=[lhsT_b, rhs_b], W=[out_b])

    def tr(self, out_b, out_ap, in_b, in_ap, ident_b, ident_ap):
        self.op("pe", lambda e: e.transpose(out_ap, in_ap, ident_ap), R=[in_b, ident_b], W=[out_b])

    def act(self, out_b, out_ap, in_b, in_ap, func, R=(), eng="act", **kw):
        self.op(eng, lambda e: e.activation(out=out_ap, in_=in_ap, func=func, **kw),
                R=[in_b] + list(R), W=[out_b] + ([kw["accum_b"]] if "accum_b" in kw else []))

    def tt(self, eng, out_b, out_ap, a_b, a_ap, b_b, b_ap, op):
        self.op(eng, lambda e: e.tensor_tensor(out=out_ap, in0=a_ap, in1=b_ap, op=op),
                R=[a_b, b_b], W=[out_b])

    def ts(self, eng, out_b, out_ap, a_b, a_ap, s1, s2, op0, op1=None, R=()):
        if op1 is None:
            f = lambda e: e.tensor_scalar(out=out_ap, in0=a_ap, scalar1=s1, scalar2=None, op0=op0)
        else:
            f = lambda e: e.tensor_scalar(out=out_ap, in0=a_ap, scalar1=s1, scalar2=s2, op0=op0, op1=op1)
        self.op(eng, f, R=[a_b] + list(R), W=[out_b])

    def stt(self, eng, out_b, out_ap, a_b, a_ap, scalar, b_b, b_ap, op0, op1, R=()):
        self.op(eng, lambda e: e.scalar_tensor_tensor(out=out_ap, in0=a_ap, scalar=scalar, in1=b_ap,
                                                     op0=op0, op1=op1),
                R=[a_b, b_b] + list(R), W=[out_b])

    def copy(self, eng, out_b, out_ap, in_b, in_ap):
        if eng == "act":
            self.op(eng, lambda e: e.copy(out=out_ap, in_=in_ap), R=[in_b], W=[out_b])
        else:
            self.op(eng, lambda e: e.tensor_copy(out=out_ap, in_=in_ap), R=[in_b], W=[out_b])

    def memset(self, eng, out_b, out_ap, val):
        self.op(eng, lambda e: e.memset(out_ap, val), W=[out_b])

    def close(self):
        self.es.close()


import math
import numpy as np

D = 1024
NCH = 8
GT = 512
RMS_EPS = 1e-6
PI = math.pi


def rot_perm(n_heads, hd, rope_dim):
    half = rope_dim // 2
    perm = np.arange(n_heads * hd)
    for h in range(n_heads):
        for d in range(rope_dim):
            perm[h * hd + d] = h * hd + (d + half if d < half else d - half)
    return perm


def rope_consts(hd, rope_dim, theta, n=128):
    half = rope_dim // 2
    inv = np.zeros(n, np.float32)
    sgn = np.zeros(n, np.float32)
    fr = (theta ** (-np.arange(half, dtype=np.float32) * 2.0 / rope_dim)).astype(np.float32)
    for p in range(n):
        d = p % hd
        if d < rope_dim:
            inv[p] = fr[d % half]
            sgn[p] = -1.0 if d < half else 1.0
    return inv, sgn


class Ctx:
    pass


def load_weights_bf16(S, wdram, ncols, gvec_b, gcol, wsb, col0=0, kchunks=NCH, scale_cols=None, stg=None):
    CW = 1024
    if stg is None:
        stg = [S.sb([128, CW], F32, "wstg") for _ in range(2)]
    k = 0
    for c in range(kchunks):
        for c0 in range(0, ncols, CW):
            cw = min(CW, ncols - c0)
            st = stg[k % 2]
            S.dma("sp" if k % 2 == 0 else "act", st[:, 0:cw], wdram[c * 128:(c + 1) * 128, c0:c0 + cw], W=[st])
            eng = "dve" if k % 2 == 0 else "pool"
            if gvec_b is not None:
                S.ts(eng, wsb, wsb[:, c, col0 + c0:col0 + c0 + cw], st, st[:, 0:cw],
                     gvec_b[:, gcol + c:gcol + c + 1], None, ALU.mult, R=[gvec_b])
            else:
                S.copy(eng, wsb, wsb[:, c, col0 + c0:col0 + c0 + cw], st, st[:, 0:cw])
            k += 1
    return stg


def norm_transpose_group(S, C, x_dram, g, xt, junk, ssq, rstd, xn, pT, xnT, ident):
    S.dma("sp", xt[:], x_dram[g * GT:(g + 1) * GT, :].rearrange("(j p) d -> p j d", p=128), W=[xt])
    for j in range(4):
        S.op("act", lambda e, j=j: e.activation(out=junk[:], in_=xt[:, j, :], func=AF.Square,
                                                 accum_out=ssq[:, j:j + 1]),
             R=[xt], W=[junk, ssq])
    S.op("act", lambda e: e.activation(out=rstd[:], in_=ssq[:], func=AF.Sqrt, bias=C.epsb[:, 0:1], scale=1.0 / D),
         R=[ssq, C.epsb], W=[rstd])
    S.op("dve", lambda e: e.reciprocal(out=rstd[:], in_=rstd[:]), R=[rstd], W=[rstd])
    for j in range(4):
        if j % 2 == 0:
            S.ts("dve", xn, xn[:, j, :], xt, xt[:, j, :], rstd[:, j:j + 1], None, ALU.mult, R=[rstd])
        else:
            S.op("act", lambda e, j=j: e.activation(out=xn[:, j, :], in_=xt[:, j, :], func=AF.Copy, scale=rstd[:, j:j + 1]),
                 R=[xt, rstd], W=[xn])
    for c in range(NCH):
        p = pT[c % 2]
        for j in range(4):
            S.tr(p, p[:, j * 128:(j + 1) * 128], xn, xn[:, j, c * 128:(c + 1) * 128], ident, ident[:])
        S.copy("act" if c % 2 == 0 else "dve", xnT, xnT[:, c, :], p, p[:])


def rope_tables(S, C, pos_dram, g, posi, posf, specs, tmp, bias_negpi):
    ang, ki, kf = C.rt_ang, C.rt_ki, C.rt_kf
    S.dma("act", posi[:], pos_dram[g * GT:(g + 1) * GT].partition_broadcast(128), W=[posi])
    S.copy("dve", posf, posf[:], posi, posi[:])
    for (cb, invf, sgn, outs) in specs:
        S.ts("dve", ang, ang[:], posf, posf[:], invf, None, ALU.mult, R=[cb])
        for which in (0, 1):
            if which == 1:
                S.ts("dve", ang, ang[:], ang, ang[:], PI / 2, None, ALU.add)
            S.ts("dve", tmp, tmp[:], ang, ang[:], 1.0 / (2 * PI), None, ALU.mult)
            S.copy("dve", ki, ki[:], tmp, tmp[:])
            S.copy("dve", kf, kf[:], ki, ki[:])
            S.stt("dve", tmp, tmp[:], kf, kf[:], -2 * PI, ang, ang[:], ALU.mult, ALU.add)
            S.ts("dve", tmp, tmp[:], tmp, tmp[:], -PI, PI, ALU.max, ALU.min)
            S.op("act", lambda e: e.activation(out=kf[:], in_=tmp[:], func=AF.Sin), R=[tmp], W=[kf])
            for (cosb, sinb, scale) in outs:
                if which == 0:
                    S.ts("dve", sinb, sinb[:], kf, kf[:], sgn, scale, ALU.mult, ALU.mult, R=[cb])
                else:
                    S.ts("dve", cosb, cosb[:], kf, kf[:], scale, None, ALU.mult)


def stage_P0(S, C, x_dram):
    nc = S.nc
    Sq = C.S
    NG = Sq // GT
    S.begin()
    ident = S.sb([128, 128], BF16, "ident")
    S.dma("sp", ident[:], C.ident_d[:, :], W=[ident])
    cst = S.sb([128, 8], F32, "cst")
    S.dma("sp", cst[:], C.ropec0_d[:, :], W=[cst])
    gv = S.sb([128, NCH], F32, "gv")
    S.dma("sp", gv[:], C.norm_mix_d[0, :].rearrange("(c p) -> p c", p=128), W=[gv], allow_slow_non_contiguous=True)
    negpi = None
    C.epsb = S.sb([128, 1], F32, "epsb")
    S.memset("dve", C.epsb, C.epsb[:], RMS_EPS)
    C.rt_ang = S.sb([128, GT], F32, "rt_ang")
    C.rt_ki = S.sb([128, GT], I32, "rt_ki")
    C.rt_kf = S.sb([128, GT], F32, "rt_kf")
    NFM = 14
    WC = NFM * 256 + 1152
    w = S.sb([128, NCH, WC], BF16, "w0")
    load_weights_bf16(S, C.w0_d, WC, gv, 0, w)

    xt = [S.sb([128, 4, D], F32, "xt")] * 2
    junk = S.sb([128, D], BF16, "junk")
    ssq = S.sb([128, 4], F32, "ssq")
    rstd = S.sb([128, 4], F32, "rstd")
    xn = S.sb([128, 4, D], BF16, "xn")
    xnT = [S.sb([128, NCH, GT], BF16, "xnT") for _ in range(2)]
    pT = [S.ps([128, GT], BF16, "pT") for _ in range(2)]
    pz = [S.ps([128, GT], F32, "pz") for _ in range(2)]
    pr = [S.ps([128, GT], F32, "pr") for _ in range(2)]
    pm = [S.ps([128, GT], F32, "pm") for _ in range(2)]
    posi = S.sb([128, GT], I32, "posi")
    posf = S.sb([128, GT], F32, "posf")
    tmp = S.sb([128, GT], F32, "tmp")
    tabs = {}
    for nm in ("A", "Ak", "B", "Bk"):
        tabs[nm] = (S.sb([128, GT], F32, "cos" + nm), S.sb([128, GT], F32, "sin" + nm))
    t1 = [S.sb([128, GT], F32, "t1") for _ in range(2)]
    t2 = [S.sb([128, GT], F32, "t2") for _ in range(2)]
    ofm = [S.sb([128, GT], BF16, "ofm") for _ in range(3)]
    ova = [S.sb([128, 2, 128], BF16, "ova") for _ in range(2)]
    for b in ova:
        S.memset("pool", b, b[:], 0.0)
        S.memset("pool", b, b[:, :, 0:1], 1.0)
    obv = [S.sb([128, 512], BF16, "obv") for _ in range(2)]
    obg = [S.sb([128, 512], F32, "obg") for _ in range(2)]
    tab_of = ["A"] * 4 + ["Ak"] * 2 + ["B"] * 4 + ["Bk"] * 4
    k = 0
    for g in range(NG):
        X = xnT[g % 2]
        norm_transpose_group(S, C, x_dram, g, xt[g % 2], junk, ssq, rstd, xn, pT, X, ident)
        specs = [(cst, cst[:, 0:1], cst[:, 1:2], [(tabs["A"][0], tabs["A"][1], 1.0), (tabs["Ak"][0], tabs["Ak"][1], 0.125)]),
                 (cst, cst[:, 2:3], cst[:, 3:4], [(tabs["B"][0], tabs["B"][1], 1.0), (tabs["Bk"][0], tabs["Bk"][1], 0.125)])]
        rope_tables(S, C, C.pos_d, g, posi, posf, specs, tmp, negpi)
        for blk in range(NFM):
            z, r = pz[blk % 2], pr[blk % 2]
            c0 = blk * 256
            for c in range(NCH):
                S.mm(z, z[:], w, w[:, c, c0:c0 + 128], X, X[:, c, :], c == 0, c == NCH - 1)
            for c in range(NCH):
                S.mm(r, r[:], w, w[:, c, c0 + 128:c0 + 256], X, X[:, c, :], c == 0, c == NCH - 1)
            cosb, sinb = tabs[tab_of[blk]]
            a, b2, o = t1[blk % 2], t2[blk % 2], ofm[blk % 3]
            S.tt("dve", a, a[:], z, z[:], cosb, cosb[:], ALU.mult)
            S.tt("dve", b2, b2[:], r, r[:], sinb, sinb[:], ALU.mult)
            S.tt("dve" if blk % 4 != 3 else "pool", o, o[:], a, a[:], b2, b2[:], ALU.add)
            S.dma("sp", C.fm0_d[blk, :, g * GT:(g + 1) * GT], o[:], R=[o])
        c0 = NFM * 256
        for j in range(4):
            tok0 = g * GT + j * 128
            p1, p2, p3 = pm[0], pm[1], pz[j % 2]
            for (pp, cc, nn) in ((p1, c0, 128), (p2, c0 + 128, 512), (p3, c0 + 640, 512)):
                for c in range(NCH):
                    S.mm(pp, pp[:, 0:nn], X, X[:, c, j * 128:(j + 1) * 128], w, w[:, c, cc:cc + nn], c == 0, c == NCH - 1)
            va, bv, bg = ova[j % 2], obv[j % 2], obg[j % 2]
            S.copy("act", va, va[:, :, 64:128], p1, p1[:, 0:128].rearrange("p (g d) -> p g d", g=2))
            S.copy("dve", bv, bv[:], p2, p2[:])
            S.op("act", lambda e, bg=bg, p3=p3: e.activation(out=bg[:], in_=p3[:], func=AF.Silu), R=[p3], W=[bg])
            S.dma("pool", C.va0_d[tok0:tok0 + 128, :], va[:].rearrange("p g d -> p (g d)"), R=[va])
            S.dma("pool", C.bv0_d[tok0:tok0 + 128, :], bv[:], R=[bv])
            S.dma("pool", C.gate0_d[tok0:tok0 + 128, :], bg[:], R=[bg])
    S.end()


def host_prep_P0(ev_w_in):
    w = np.asarray(ev_w_in, np.float32)
    aq, ak, av, bq, bk, bv, bg = np.split(w, np.cumsum([512, 128, 128, 512, 512, 512])[:], axis=1)
    pa8 = rot_perm(8, 64, 16)
    pa2 = rot_perm(2, 64, 16)
    pb8 = rot_perm(8, 64, 64)
    cols = []
    aqr = aq[:, pa8]
    for j in range(4):
        cols += [aq[:, j * 128:(j + 1) * 128], aqr[:, j * 128:(j + 1) * 128]]
    akr = ak[:, pa2]
    for gq in range(2):
        kk = ak[:, gq * 64:(gq + 1) * 64]
        kr = akr[:, gq * 64:(gq + 1) * 64]
        cols += [kk, kk, kr, kr]
    bqr = bq[:, pb8]
    for j in range(4):
        cols += [bq[:, j * 128:(j + 1) * 128], bqr[:, j * 128:(j + 1) * 128]]
    bkr = bk[:, pb8]
    for j in range(4):
        cols += [bk[:, j * 128:(j + 1) * 128], bkr[:, j * 128:(j + 1) * 128]]
    cols += [av, bv, bg]
    return np.ascontiguousarray(np.concatenate(cols, axis=1))


def bcast_row_to_parts(S, C, src_b, src_ap, dst_b, dst_ap, ps_b, ncol):
    S.mm(ps_b, ps_b[:, 0:ncol], C.onesf, C.onesf[0:1, :], src_b, src_ap, True, True)
    S.copy("dve", dst_b, dst_ap, ps_b, ps_b[:, 0:ncol])


def load_consts(S, C):
    C.ident = S.sb([128, 128], BF16, "ident")
    S.dma("sp", C.ident[:], C.ident_d[:, :], W=[C.ident])
    C.onesf = S.sb([128, 128], F32, "onesf")
    S.memset("pool", C.onesf, C.onesf[:], 1.0)
    C.epsb = S.sb([128, 1], F32, "epsb")
    S.memset("dve", C.epsb, C.epsb[:], RMS_EPS)


def stage_A(S, C):
    Sq = C.S
    N = Sq // 128
    NG = Sq // GT
    S.begin()
    load_consts(S, C)
    maskA = S.sb([128, 2, 512], BF16, "maskA")
    S.dma("sp", maskA[:], C.maskA_d[:, :, :], W=[maskA])
    sinkb = S.sb([128, 8], F32, "sinkb")
    S.dma("sp", sinkb[:], C.sink_d[0, :].partition_broadcast(128), W=[sinkb])
    Q = [S.sb([128, Sq], BF16, "Q%d" % j) for j in range(4)]
    V = S.sb([128, N, 256], BF16, "V")
    for j in range(4):
        for h in range(0, Sq, 2048):
            w_ = min(2048, Sq - h)
            S.dma("sp" if j % 2 == 0 else "act", Q[j][:, h:h + w_], C.fm0_d[j, :, h:h + w_], W=[Q[j]])
    Kz = [[S.sb([128, Sq], BF16, "Kz%d%d" % (j, r)) for r in range(2)] for j in range(2)]
    for j in range(2):
        for r in range(2):
            S.memset("pool", Kz[j][r], Kz[j][r][(1 - r) * 64:(2 - r) * 64, :], 0.0)
            for h in range(0, Sq, 2048):
                w_ = min(2048, Sq - h)
                S.dma("pool", Kz[j][r][r * 64:(r + 1) * 64, h:h + w_], C.fm0_d[4 + j, r * 64:(r + 1) * 64, h:h + w_], W=[Kz[j][r]])
    K = [Kz[0][0], Kz[1][0]]
    S.dma("sp", V[:], C.va0_d.rearrange("(n p) c -> p n c", p=128), W=[V])
    sq = [S.sb([128, GT], F32, "sq") for _ in range(2)]
    accq = S.sb([1, GT], F32, "accq")
    acck = S.sb([1, GT], F32, "acck")
    S.memset("dve", accq, accq[:], 0.0)
    S.memset("dve", acck, acck[:], 0.0)
    pn = S.ps([128, GT], F32, "pn")
    i = 0
    for (lst, acc, kp) in ((Q, accq, 128), (K, acck, 64)):
        for b in lst:
            for g in range(NG):
                s_ = sq[i % 2]
                i += 1
                S.op("act", lambda e, s_=s_, b=b, g=g: e.activation(out=s_[:], in_=b[:, g * GT:(g + 1) * GT], func=AF.Square),
                     R=[b], W=[s_])
                S.mm(pn, pn[0:1, :], C.onesf, C.onesf[0:kp, 0:1], s_, s_[0:kp, :], True, True)
                S.tt("dve", acc, acc[:], pn, pn[0:1, :], acc, acc[:], ALU.max)
    mq = S.sb([1, 4], F32, "mq")
    S.op("dve", lambda e: e.tensor_reduce(out=mq[:, 0:1], in_=accq[:], axis=AX.X, op=ALU.max), R=[accq], W=[mq])
    S.op("dve", lambda e: e.tensor_reduce(out=mq[:, 1:2], in_=acck[:], axis=AX.X, op=ALU.max), R=[acck], W=[mq])
    S.tt("dve", mq, mq[:, 2:3], mq, mq[:, 0:1], mq, mq[:, 1:2], ALU.mult)
    S.op("act", lambda e: e.activation(out=mq[:, 3:4], in_=mq[:, 2:3], func=AF.Sqrt), R=[mq], W=[mq])
    S.ts("dve", mq, mq[:, 3:4], mq, mq[:, 3:4], -1.0, None, ALU.mult)
    negM = S.sb([128, 1], F32, "negM")
    bcast_row_to_parts(S, C, mq, mq[0:1, 3:4], negM, negM[:], pn, 1)
    sinkexp = S.sb([128, 8], F32, "sinkexp")
    S.op("act", lambda e: e.activation(out=sinkexp[:], in_=sinkb[:], func=AF.Exp, bias=negM[:, 0:1]),
         R=[sinkb, negM], W=[sinkexp])
    pss = [S.ps([128, 512], F32, "pss") for _ in range(4)]
    pso = [S.ps([128, 512], F32, "pso") for _ in range(2)]
    pts = [S.sb([128, 512], BF16, "pt") for _ in range(4)]
    den = [S.sb([1, 512], F32, "den") for _ in range(2)]
    rdb = [S.sb([128, 512], F32, "rdb") for _ in range(2)]
    osb = [S.sb([128, 512], BF16, "osb") for _ in range(2)]
    ks = 0
    it = 0
    for n in range(N):
        for g in range(2):
            po = pso[it % 2]
            ms = [m for m in (n - 1, n, n + 1) if 0 <= m < N]
            ptl = []
            for m in ms:
                ps_ = pss[ks % 4]
                pt = pts[ks % 4]
                ks += 1
                for hh in range(4):
                    h = 4 * g + hh
                    j, r = h // 2, h % 2
                    S.mm(ps_, ps_[:, hh * 128:(hh + 1) * 128], Kz[g][r], Kz[g][r][:, m * 128:(m + 1) * 128],
                         Q[j], Q[j][:, n * 128:(n + 1) * 128], True, True)
                S.op("act", lambda e, pt=pt, ps_=ps_: e.activation(out=pt[:], in_=ps_[:], func=AF.Exp, bias=negM[:, 0:1]),
                     R=[ps_, negM], W=[pt])
                if m != n:
                    mi = 0 if m < n else 1
                    S.tt("pool", pt, pt[:], pt, pt[:], maskA, maskA[:, mi, :], ALU.mult)
                ptl.append((m, pt))
            for idx, (m, pt) in enumerate(ptl):
                S.mm(po, po[:, :], V, V[:, m, g * 128:(g + 1) * 128], pt, pt[:], idx == 0, idx == len(ptl) - 1)
            dn, rb, ob = den[it % 2], rdb[it % 2], osb[it % 2]
            for hh in range(4):
                h = 4 * g + hh
                S.ts("dve", dn, dn[0:1, hh * 128:(hh + 1) * 128], po, po[0:1, hh * 128:(hh + 1) * 128],
                     sinkexp[0:1, h:h + 1], None, ALU.add, R=[sinkexp])
            S.op("dve", lambda e, dn=dn: e.reciprocal(out=dn[0:1, :], in_=dn[0:1, :]), R=[dn], W=[dn])
            pb = pss[ks % 4]
            ks += 1
            S.mm(pb, pb[:, :], C.onesf, C.onesf[0:1, :], dn, dn[0:1, :], True, True)
            S.copy("act", rb, rb[64:128, :], pb, pb[64:128, :])
            S.tt("dve", ob, ob[64:128, :], po, po[64:128, :], rb, rb[64:128, :], ALU.mult)
            S.dma("sp", C.mixT0_d[(4 * g) * 64:(4 * g + 4) * 64, n * 128:(n + 1) * 128].rearrange("(h d) q -> d h q", d=64),
                  ob[64:128, :].rearrange("d (h q) -> d h q", h=4), R=[ob])
            it += 1
    S.end()


def stage_B(S, C):
    Sq = C.S
    N = Sq // 128
    L = 128
    S.begin()
    load_consts(S, C)
    relB = S.sb([128, 2, 128], F32, "relB")
    S.dma("sp", relB[:], C.relB_d[:, :, :], W=[relB])
    maskB = S.sb([128, 2, 128], F32, "maskB")
    S.dma("sp", maskB[:], C.maskB_d[:, :, :], W=[maskB])
    cvec = S.sb([128, 4], F32, "cvec")
    S.dma("sp", cvec[:], C.cvecB_d[:, :], W=[cvec])
    lgb = S.sb([128, 16], F32, "lgb")
    S.dma("sp", lgb[:], C.decay_d.rearrange("a b h -> a (b h)")[0, :].partition_broadcast(128), W=[lgb])
    lgp = S.sb([128, 8], F32, "lgp")
    S.dma("sp", lgp[:], C.decayp_d[:, :], W=[lgp])
    for t in (lgb, lgp):
        S.op("act", lambda e, t=t: e.activation(out=t[:], in_=t[:], func=AF.Exp, scale=-1.0), R=[t], W=[t])
        S.ts("dve", t, t[:], t, t[:], 1.0, None, ALU.add)
        S.op("act", lambda e, t=t: e.activation(out=t[:], in_=t[:], func=AF.Ln), R=[t], W=[t])
        S.ts("dve", t, t[:], t, t[:], -1.0, None, ALU.mult)
    zx = S.sb([128, 4, 8], F32, "zx")
    for k_, (ci, d) in enumerate(((0, 0), (1, 1), (2, 0), (3, 1))):
        S.ts("dve", zx, zx[:, k_, :], lgb, lgb[:, d * 8:(d + 1) * 8], cvec[:, ci:ci + 1], None, ALU.mult, R=[cvec])
    S.op("act", lambda e: e.activation(out=zx[:], in_=zx[:], func=AF.Exp), R=[zx], W=[zx])
    cd = S.sb([128, 8], F32, "cd")
    S.op("act", lambda e: e.activation(out=cd[:], in_=lgp[:], func=AF.Exp, scale=float(L)), R=[lgp], W=[cd])
    dc = S.sb([128, 8, 128], F32, "dc")
    dtmp = S.sb([128, 128], F32, "dtmp")
    for h in range(8):
        S.op("act", lambda e, h=h: e.activation(out=dc[:, h, :], in_=relB[:, 0, :], func=AF.Exp, scale=lgb[:, h:h + 1]),
             R=[relB, lgb], W=[dc])
        S.tt("dve", dc, dc[:, h, :], dc, dc[:, h, :], maskB, maskB[:, 0, :], ALU.mult)
        S.op("act", lambda e, h=h: e.activation(out=dtmp[:], in_=relB[:, 1, :], func=AF.Exp, scale=lgb[:, 8 + h:9 + h]),
             R=[relB, lgb], W=[dtmp])
        S.tt("dve", dtmp, dtmp[:], dtmp, dtmp[:], maskB, maskB[:, 1, :], ALU.mult)
        S.tt("dve", dc, dc[:, h, :], dc, dc[:, h, :], dtmp, dtmp[:], ALU.add)
    Vt = S.sb([128, N, 128], BF16, "Vt")
    bdm = S.sb([128, 128], F32, "bdm")
    S.dma("sp", bdm[:], C.bdm_d[:, :], W=[bdm])
    Kpz = [S.sb([128, Sq], BF16, "Kpz%d" % r) for r in range(2)]
    Gt = S.sb([128, N, 128], F32, "Gt")
    Qp = S.sb([128, Sq], BF16, "Qp")
    Kp = S.sb([128, Sq], BF16, "Kp")
    Rf = S.sb([128, 128], F32, "Rf")
    Rb = S.sb([128, 128], F32, "Rb")
    Rfp = S.sb([128, N, 128], BF16, "Rfp")
    Rbp = S.sb([128, N, 128], BF16, "Rbp")
    pk = [S.ps([128, 128], BF16, "pk") for _ in range(2)]
    pu = [S.ps([128, 256], F32, "pu") for _ in range(2)]
    pss = [S.ps([128, 256], F32, "pss") for _ in range(2)]
    py = [S.ps([128, 384], F32, "py") for _ in range(2)]
    kz = [S.sb([128, 2, 128], BF16, "kz") for _ in range(2)]
    sd = [S.sb([128, 2, 128], BF16, "sd") for _ in range(2)]
    ysb = [S.sb([128, 128], F32, "ysb") for _ in range(2)]
    junk = S.sb([128, 64], F32, "junkB")
    st = [S.sb([128, 8], F32, "st") for _ in range(2)]
    ob = [S.sb([128, 128], BF16, "ob") for _ in range(2)]
    oT = [S.sb([128, 512], BF16, "oT") for _ in range(2)]
    gneps = S.sb([128, 1], F32, "gneps")
    S.memset("dve", gneps, gneps[:], 1e-5)
    for pr_ in range(4):
        for h0 in range(0, Sq, 2048):
            w_ = min(2048, Sq - h0)
            S.dma("sp", Qp[:, h0:h0 + w_], C.fm0_d[6 + pr_, :, h0:h0 + w_], W=[Qp])
            S.dma("act", Kp[:, h0:h0 + w_], C.fm0_d[10 + pr_, :, h0:h0 + w_], W=[Kp])
        S.dma("pool", Gt[:], C.gate0_d[:, pr_ * 128:(pr_ + 1) * 128].rearrange("(n p) c -> p n c", p=128), W=[Gt])
        S.dma("sp", Vt[:], C.bv0_d[:, pr_ * 128:(pr_ + 1) * 128].rearrange("(n p) c -> p n c", p=128), W=[Vt])
        for r in range(2):
            S.copy("pool", Kpz[r], Kpz[r][:], Kp, Kp[:])
            S.memset("pool", Kpz[r], Kpz[r][(1 - r) * 64:(2 - r) * 64, :], 0.0)
        S.memset("dve", Rf, Rf[:], 0.0)
        S.memset("dve", Rb, Rb[:], 0.0)
        for dirn in (0, 1):
            R_, Rp = (Rf, Rfp) if dirn == 0 else (Rb, Rbp)
            order = range(N) if dirn == 0 else range(N - 1, -1, -1)
            for n in order:
                p_, kz_, pu_ = pk[n % 2], kz[n % 2], pu[n % 2]
                S.tr(p_, p_[:], Kp, Kp[:, n * 128:(n + 1) * 128], C.ident, C.ident[:])
                for r in range(2):
                    h = 2 * pr_ + r
                    S.ts("dve", kz_, kz_[:, 0, r * 64:(r + 1) * 64], p_, p_[:, r * 64:(r + 1) * 64],
                         zx[:, dirn, h:h + 1], None, ALU.mult, R=[zx])
                S.mm(pu_, pu_[:, 0:128], kz_, kz_[:, 0, :], Vt, Vt[:, n, :], True, True)
                S.tt("pool", Rp, Rp[:, n, :], R_, R_[:], bdm, bdm[:], ALU.mult)
                S.stt("dve", R_, R_[:], R_, R_[:], cd[:, pr_ * 2 + dirn:pr_ * 2 + dirn + 1], pu_, pu_[:, 0:128],
                      ALU.mult, ALU.add, R=[cd])
        for n in range(N):
            ps_, sd_, py_, y_, st_, ob_ = pss[n % 2], sd[n % 2], py[n % 2], ysb[n % 2], st[n % 2], ob[n % 2]
            tok = slice(n * 128, (n + 1) * 128)
            for r in range(2):
                S.mm(ps_, ps_[:, r * 128:(r + 1) * 128], Kpz[r], Kpz[r][:, tok], Qp, Qp[:, tok], True, True)
            S.tt("dve", sd_, sd_[:].rearrange("p a b -> p (a b)"), ps_, ps_[:],
                 dc, dc[:, 2 * pr_:2 * pr_ + 2, :].rearrange("p a b -> p (a b)"), ALU.mult)
            for r in range(2):
                S.mm(py_, py_[:, r * 64:(r + 1) * 64], sd_, sd_[:, r, :], Vt, Vt[:, n, r * 64:(r + 1) * 64], True, True)
            S.mm(py_, py_[:, 128:256], Qp, Qp[:, tok], Rfp, Rfp[:, n, :], True, True)
            S.mm(py_, py_[:, 256:384], Qp, Qp[:, tok], Rbp, Rbp[:, n, :], True, True)
            S.copy("act", y_, y_[:], py_, py_[:, 0:128])
            for r in range(2):
                h = 2 * pr_ + r
                c_ = slice(r * 64, (r + 1) * 64)
                S.stt("dve", y_, y_[:, c_], py_, py_[:, 128 + r * 64:128 + (r + 1) * 64], zx[:, 2, h:h + 1], y_, y_[:, c_],
                      ALU.mult, ALU.add, R=[zx])
                S.stt("dve", y_, y_[:, c_], py_, py_[:, 256 + r * 64:256 + (r + 1) * 64], zx[:, 3, h:h + 1], y_, y_[:, c_],
                      ALU.mult, ALU.add, R=[zx])
            S.op("dve", lambda e, y_=y_, st_=st_: e.tensor_reduce(out=st_[:, 0:2], in_=y_[:].rearrange("p (a b) -> p a b", a=2),
                                                                 axis=AX.X, op=ALU.add), R=[y_], W=[st_])
            for r in range(2):
                S.op("act", lambda e, y_=y_, st_=st_, r=r: e.activation(out=junk[:], in_=y_[:, r * 64:(r + 1) * 64], func=AF.Square,
                                                                         accum_out=st_[:, 2 + r:3 + r]), R=[y_], W=[junk, st_])
            S.ts("dve", st_, st_[:, 4:6], st_, st_[:, 0:2], 1.0 / 64, None, ALU.mult)
            S.tt("dve", st_, st_[:, 0:2], st_, st_[:, 4:6], st_, st_[:, 4:6], ALU.mult)
            S.stt("dve", st_, st_[:, 6:8], st_, st_[:, 2:4], 1.0 / 64, st_, st_[:, 0:2], ALU.mult, ALU.subtract)
            S.op("act", lambda e, st_=st_: e.activation(out=st_[:, 6:8], in_=st_[:, 6:8], func=AF.Sqrt, bias=gneps[:, 0:1]),
                 R=[st_, gneps], W=[st_])
            S.op("dve", lambda e, st_=st_: e.reciprocal(out=st_[:, 6:8], in_=st_[:, 6:8]), R=[st_], W=[st_])
            for r in range(2):
                c_ = slice(r * 64, (r + 1) * 64)
                S.ts("dve", y_, y_[:, c_], y_, y_[:, c_], st_[:, 4 + r:5 + r], st_[:, 6 + r:7 + r], ALU.subtract, ALU.mult, R=[st_])
            S.tt("pool", ob_, ob_[:], y_, y_[:], Gt, Gt[:, n, :], ALU.mult)
            pt_ = pk[n % 2]
            S.tr(pt_, pt_[:], ob_, ob_[:], C.ident, C.ident[:])
            oT_ = oT[(n // 4) % 2]
            S.copy("act", oT_, oT_[:, (n % 4) * 128:(n % 4 + 1) * 128], pt_, pt_[:])
            if n % 4 == 3:
                S.dma("sp", C.mixT0_d[512 + pr_ * 128:512 + (pr_ + 1) * 128, (n - 3) * 128:(n + 1) * 128], oT_[:], R=[oT_])
    S.end()


def declare(nc, C, Sq, debug=False):
    ks = "ExternalOutput" if debug else "Internal"
    def inp(name, shape, dt):
        return nc.dram_tensor(name, list(shape), dt, kind="ExternalInput").ap()
    def scr(name, shape, dt):
        return nc.dram_tensor(name, list(shape), dt, kind=ks).ap()
    C.S = Sq
    C.x_d = inp("x", [Sq, D], F32)
    C.pos_d = inp("pos", [Sq], I32)
    C.norm_mix_d = inp("norm_mix", [2, D], F32)
    C.w0_d = inp("w0", [D, 14 * 256 + 1152], F32)
    C.ident_d = inp("ident", [128, 128], BF16)
    C.ropec0_d = inp("ropec0", [128, 8], F32)
    C.maskA_d = inp("maskA", [128, 2, 512], BF16)
    C.sink_d = inp("sink", [1, 8], F32)
    C.relB_d = inp("relB", [128, 2, 128], F32)
    C.maskB_d = inp("maskB", [128, 2, 128], F32)
    C.cvecB_d = inp("cvecB", [128, 4], F32)
    C.decay_d = inp("decay", [1, 2, 8], F32)
    C.decayp_d = inp("decayp", [128, 8], F32)
    C.identf_d = inp("identf", [128, 128], F32)
    C.norm_ffn_d = inp("norm_ffn", [2, D], F32)
    C.router_d = inp("router", [2, D, 16], F32)
    C.tokc_d = inp("tokc", [128, Sq // 128], I32)
    C.trib_d = inp("trib", [128, 128], BF16)
    C.ecap_d = inp("ecap", [128, 16], F32)
    C.wout0_d = inp("wout0", [D, D], F32)
    C.wg_d = inp("wg", [2, 16, D, 2048], F32)
    C.wu_d = inp("wu", [2, 16, D, 2048], F32)
    C.wd_d = inp("wd", [2, 16, 2048, D], F32)
    C.norm_final_d = inp("norm_final", [1, D], F32)
    C.y_d = nc.dram_tensor("y", [Sq, D], F32, kind="ExternalOutput").ap()
    C.x1_d = scr("x1", [Sq, D], F32)
    C.hnx_d = scr("hnx", [Sq, XW], I32)
    C.xin_d = scr("xin", [16 * (Sq // 8), XW], I32)
    C.bdm_d = inp("bdm", [128, 128], F32)
    C.w1_d = inp("w1", [D, W1C], F32)
    C.wuq_d = inp("wuq", [512, 1536], F32)
    C.wukv_d = inp("wukv", [256, 1024], F32)
    C.ropec1_d = inp("ropec1", [128, 2], F32)
    C.mla_nq_d = inp("mla_nq", [1, 512], F32)
    C.mla_nkv_d = inp("mla_nkv", [1, 256], F32)
    C.gbias_d = inp("gbias", [4, 4], F32)
    C.wout1_d = inp("wout1", [D, D], F32)
    C.fmq_d = scr("fmq", [8, 96, Sq], BF16)
    C.fmk_d = scr("fmk", [8, 96, Sq], BF16)
    C.vC_d = scr("vC", [Sq, 1024], BF16)
    C.qkraw_d = scr("qkraw", [1024, Sq], F32)
    C.gatesT_d = scr("gatesT", [4, 4, Sq], F32)
    C.vD_d = scr("vD", [Sq, 4 * 129], BF16)
    C.og_d = scr("og", [Sq, 512], F32)
    C.mixT1_d = scr("mixT1", [1024, Sq], BF16)
    C.conv_d = inp("conv", [128, 40], F32)
    C.maskD_d = inp("maskD", [128, 2, 128], F32)
    C.qkc_d = scr("qkc", [8, 128, Sq], BF16)
    C.vec_d = scr("vec", [10, 4, Sq], F32)
    C.vec2_d = scr("vec2", [4, 4, Sq // 128], F32)
    C.chunk_d = scr("chunkd", [4, 4, Sq // 128], F32)
    C.mp_d = scr("mpd", [2, 4, Sq // 128], F32)
    C.x3_d = scr("x3", [Sq, D], F32)
    C.fm0_d = scr("fm0", [14, 128, Sq], BF16)
    C.va0_d = scr("va0", [Sq, 256], BF16)
    C.bv0_d = scr("bv0", [Sq, 512], BF16)
    C.gate0_d = scr("gate0", [Sq, 512], F32)
    C.mixT0_d = scr("mixT0", [1024, Sq], BF16)


def host_consts():
    import ml_dtypes
    bf = ml_dtypes.bfloat16
    c = {}
    c["ident"] = np.eye(128, dtype=np.float32).astype(bf)
    rc = np.zeros((128, 8), np.float32)
    rc[:, 0], rc[:, 1] = rope_consts(64, 16, 500000.0)
    rc[:, 2], rc[:, 3] = rope_consts(64, 64, 10000.0)
    c["ropec0"] = rc
    j = np.arange(128)[:, None]
    i = np.arange(128)[None, :]
    mA = np.zeros((128, 2, 512), np.float32)
    mA[:, 0, :] = np.tile((j >= i).astype(np.float32), (1, 4))
    mA[:, 1, :] = np.tile((j <= i).astype(np.float32), (1, 4))
    c["maskA"] = mA.astype(bf)
    rel = (i - j).astype(np.float32)
    rB = np.zeros((128, 2, 128), np.float32)
    rB[:, 0] = np.maximum(rel, 0)
    rB[:, 1] = np.maximum(-rel, 0)
    c["relB"] = rB
    mB = np.zeros((128, 2, 128), np.float32)
    mB[:, 0] = (rel >= 0)
    mB[:, 1] = (rel < 0)
    c["maskB"] = mB
    c["identf"] = np.eye(128, dtype=np.float32)
    c["bdm"] = ((j // 64) == (i // 64)).astype(np.float32)
    c["trib"] = (j < i).astype(np.float32).astype(bf)
    l = np.arange(128, dtype=np.float32)
    c["cvecB"] = np.stack([127 - l, l, l + 1, 128 - l], axis=1).astype(np.float32)
    return c


def host_inputs(inp, b):
    d = dict(host_consts())
    d["x"] = np.ascontiguousarray(inp["x"][b])
    d["pos"] = np.ascontiguousarray(inp["positions"][b]).astype(np.int32)
    d["norm_mix"] = np.asarray(inp["norm_mix"], np.float32)
    d["w0"] = host_prep_P0(inp["ev_w_in"][0])
    Sq = d["x"].shape[0]
    d["tokc"] = (np.arange(Sq // 128, dtype=np.int32)[None, :] * 128 + np.arange(128, dtype=np.int32)[:, None]).astype(np.int32)
    d["ecap"] = np.tile((np.arange(16, dtype=np.float32) * (Sq // 8))[None, :], (128, 1)).astype(np.float32)
    d["norm_ffn"] = np.asarray(inp["norm_ffn"], np.float32)
    d["router"] = np.asarray(inp["moe_router"], np.float32)
    d["wout0"] = np.asarray(inp["ev_w_out"][0], np.float32)
    d["wg"] = np.asarray(inp["moe_w_gate"], np.float32)
    d["wu"] = np.asarray(inp["moe_w_up"], np.float32)
    d["wd"] = np.asarray(inp["moe_w_down"], np.float32)
    d["norm_final"] = np.asarray(inp["norm_final"], np.float32).reshape(1, D)
    d["w1"], d["wuq"], d["wukv"] = host_prep_P1(inp["od_w_in"][0], inp["mla_w_uq"][0], inp["mla_w_ukv"][0])
    d["ropec1"] = ropec1()
    d["mla_nq"] = np.asarray(inp["mla_norm_q"], np.float32).reshape(1, 512)
    d["mla_nkv"] = np.asarray(inp["mla_norm_kv"], np.float32).reshape(1, 256)
    d["gbias"] = np.ascontiguousarray(np.asarray(inp["mlstm_gate_bias"], np.float32)[0].T)
    d["wout1"] = np.asarray(inp["od_w_out"][0], np.float32)
    d["conv"] = np.ascontiguousarray(np.asarray(inp["mlstm_conv"][0], np.float32).reshape(5, 8, 128).transpose(2, 1, 0).reshape(128, 40))
    jj = np.arange(128)[:, None]; tt_ = np.arange(128)[None, :]
    d["maskD"] = np.stack([(jj <= tt_), (jj > tt_)], axis=1).astype(np.float32)
    d["sink"] = np.asarray(inp["attn_sink"], np.float32).reshape(1, 8)
    dl = np.asarray(inp["ret_decay_logit"], np.float32).reshape(1, 2, 8)
    d["decay"] = dl
    dp = np.zeros((128, 8), np.float32)
    for pr_ in range(4):
        for dr in range(2):
            dp[0:64, pr_ * 2 + dr] = dl[0, dr, 2 * pr_]
            dp[64:128, pr_ * 2 + dr] = dl[0, dr, 2 * pr_ + 1]
    d["decayp"] = dp
    return d

NE = 16
XW = 529


def stage_OUT(S, C, l, mixT_d, wout_d, xin_d, xout_d):
    Sq = C.S
    NG = Sq // GT
    S.begin()
    load_consts(S, C)
    identf = S.sb([128, 128], F32, "identf")
    S.dma("sp", identf[:], C.identf_d[:, :], W=[identf])
    w = S.sb([128, NCH, D], BF16, "wout")
    load_weights_bf16(S, wout_d, D, None, 0, w)
    gb = S.sb([128, D], F32, "gb")
    S.dma("sp", gb[:], C.norm_ffn_d[l, :].partition_broadcast(128), W=[gb])
    wr = S.sb([128, NCH, NE], F32, "wr")
    S.dma("sp", wr[:], C.router_d[l].rearrange("(c p) e -> p c e", p=128), W=[wr])
    tokc = S.sb([128, Sq // 128], I32, "tokc")
    S.dma("sp", tokc[:], C.tokc_d[:, :], W=[tokc])
    mT = [S.sb([128, NCH, GT], BF16, "mT") for _ in range(2)]
    xt = [S.sb([128, D], F32, "xt") for _ in range(2)]
    x1 = [S.sb([128, D], F32, "x1") for _ in range(2)]
    hn = [S.sb([128, D], F32, "hn") for _ in range(2)]
    hT = S.sb([128, NCH, 128], F32, "hT")
    row = [S.sb([128, XW], I32, "row") for _ in range(2)]
    junk = S.sb([128, D], BF16, "junk")
    st = [S.sb([128, 8], F32, "st") for _ in range(2)]
    lg = [S.sb([128, NE], F32, "lg") for _ in range(2)]
    po = [S.ps([128, 512], F32, "po") for _ in range(4)]
    pt = [S.ps([128, 512], F32, "pt") for _ in range(2)]
    pl = S.ps([128, NE], F32, "pl")
    for g in range(NG):
        M = mT[g % 2]
        S.dma("sp", M[:], mixT_d[:, g * GT:(g + 1) * GT].rearrange("(c p) t -> p c t", p=128), W=[M])
        for j in range(4):
            it = g * 4 + j
            tok0 = it * 128
            X, X1, H, R, st_, lg_ = xt[it % 2], x1[it % 2], hn[it % 2], row[it % 2], st[it % 2], lg[it % 2]
            S.dma("act", X[:], xin_d[tok0:tok0 + 128, :], W=[X])
            for hf in range(2):
                p_ = po[(it * 2 + hf) % 4]
                for c in range(NCH):
                    S.mm(p_, p_[:], M, M[:, c, j * 128:(j + 1) * 128], w, w[:, c, hf * 512:(hf + 1) * 512], c == 0, c == NCH - 1)
                S.tt("dve", X1, X1[:, hf * 512:(hf + 1) * 512], p_, p_[:], X, X[:, hf * 512:(hf + 1) * 512], ALU.add)
            S.dma("sp", xout_d[tok0:tok0 + 128, :], X1[:], R=[X1])
            S.op("act", lambda e, X1=X1, st_=st_: e.activation(out=junk[:], in_=X1[:], func=AF.Square, accum_out=st_[:, 0:1]),
                 R=[X1], W=[junk, st_])
            S.op("act", lambda e, st_=st_: e.activation(out=st_[:, 1:2], in_=st_[:, 0:1], func=AF.Sqrt, bias=C.epsb[:, 0:1], scale=1.0 / D),
                 R=[st_, C.epsb], W=[st_])
            S.op("dve", lambda e, st_=st_: e.reciprocal(out=st_[:, 1:2], in_=st_[:, 1:2]), R=[st_], W=[st_])
            S.stt("dve", H, H[:], X1, X1[:], st_[:, 1:2], gb, gb[:], ALU.mult, ALU.mult, R=[st_])
            S.copy("pool", R, R[:, 0:512].bitcast(BF16), H, H[:])
            for c in range(NCH):
                p_ = pt[c % 2]
                S.tr(p_, p_[:, 0:128], H, H[:, c * 128:(c + 1) * 128], identf, identf[:])
                S.copy("act" if c % 2 == 0 else "dve", hT, hT[:, c, :], p_, p_[:, 0:128])
            for c in range(NCH):
                S.mm(pl, pl[:], hT, hT[:, c, :], wr, wr[:, c, :], c == 0, c == NCH - 1)
            S.op("dve", lambda e, st_=st_: e.tensor_reduce(out=st_[:, 2:3], in_=pl[:], axis=AX.X, op=ALU.max), R=[pl], W=[st_])
            S.ts("dve", st_, st_[:, 2:3], st_, st_[:, 2:3], -1.0, None, ALU.mult)
            S.op("act", lambda e, st_=st_, lg_=lg_: e.activation(out=lg_[:], in_=pl[:], func=AF.Exp, bias=st_[:, 2:3], accum_out=st_[:, 3:4]),
                 R=[pl, st_], W=[lg_, st_])
            S.op("dve", lambda e, st_=st_: e.reciprocal(out=st_[:, 4:5], in_=st_[:, 3:4]), R=[st_], W=[st_])
            S.ts("dve", R, R[:, 512:528].bitcast(F32), lg_, lg_[:], st_[:, 4:5], None, ALU.mult, R=[st_])
            S.copy("pool", R, R[:, 528:529], tokc, tokc[:, it:it + 1])
            S.dma("sp", C.hnx_d[tok0:tok0 + 128, :], R[:], R=[R])
    S.end()


def stage_MOE(S, C, l, x_d):
    Sq = C.S
    N = Sq // 128
    cap = Sq // 8
    NT = cap // 128
    BIG = float(NE * cap + 4096)
    S.begin()
    load_consts(S, C)
    aff = S.sb([128, N, NE], F32, "aff")
    S.dma("sp", aff[:], C.hnx_d[:, 512:528].bitcast(F32).rearrange("(n p) e -> p n e", p=128), W=[aff],
          allow_slow_non_contiguous=True)
    lo = S.sb([128, NE], F32, "lo")
    mid = S.sb([128, NE], F32, "mid")
    ge = S.sb([128, NE], F32, "ge")
    cnt = S.sb([128, NE], F32, "cnt")
    cmp_ = S.sb([128, N, NE], F32, "cmp")
    pc = S.ps([128, NE], F32, "pc")
    S.memset("dve", lo, lo[:], 0.0)
    for k in range(1, 29):
        wk = 2.0 ** (-k)
        S.ts("dve", mid, mid[:], lo, lo[:], wk, None, ALU.add)
        S.tt("dve", cmp_, cmp_[:], aff, aff[:], mid, mid[:].unsqueeze(1).to_broadcast([128, N, NE]), ALU.is_ge)
        S.op("dve", lambda e: e.tensor_reduce(out=cnt[:], in_=cmp_[:].rearrange("p n e -> p e n"), axis=AX.X, op=ALU.add),
             R=[cmp_], W=[cnt])
        S.mm(pc, pc[:], C.onesf, C.onesf[:], cnt, cnt[:], True, True)
        S.ts("dve", ge, ge[:], pc, pc[:], float(cap) - 0.5, None, ALU.is_ge)
        S.stt("dve", lo, lo[:], ge, ge[:], wk, lo, lo[:], ALU.mult, ALU.add)
    S.tt("dve", cmp_, cmp_[:], aff, aff[:], lo, lo[:].unsqueeze(1).to_broadcast([128, N, NE]), ALU.is_ge)
    maskb = S.sb([128, N * NE], BF16, "maskb")
    S.copy("dve", maskb, maskb[:], cmp_, cmp_[:].rearrange("p n e -> p (n e)"))
    trib = S.sb([128, 128], BF16, "trib")
    S.dma("sp", trib[:], C.trib_d[:, :], W=[trib])
    onesb = S.sb([128, 128], BF16, "onesb")
    S.memset("pool", onesb, onesb[:], 1.0)
    slot = S.sb([128, N, NE], F32, "slot")
    tot = [S.sb([128, N, NE], F32, "tot") for _ in range(2)]
    pp = [S.ps([128, 512], F32, "pp") for _ in range(2)]
    W_ = N * NE
    for c0 in range(0, W_, 512):
        cw = min(512, W_ - c0)
        S.mm(pp[0], pp[0][:, 0:cw], trib, trib[:], maskb, maskb[:, c0:c0 + cw], True, True)
        S.copy("dve", slot, slot[:].rearrange("p n e -> p (n e)")[:, c0:c0 + cw], pp[0], pp[0][:, 0:cw])
        S.mm(pp[1], pp[1][:, 0:cw], onesb, onesb[:], maskb, maskb[:, c0:c0 + cw], True, True)
        S.copy("dve", tot[0], tot[0][:].rearrange("p n e -> p (n e)")[:, c0:c0 + cw], pp[1], pp[1][:, 0:cw])
    S.tt("dve", slot, slot[:], slot, slot[:], tot[0], tot[0][:], ALU.subtract)
    cur = 0
    s_ = 1
    while s_ < N:
        a, b = tot[cur], tot[1 - cur]
        S.copy("dve", b, b[:, 0:s_, :], a, a[:, 0:s_, :])
        S.tt("dve", b, b[:, s_:N, :], a, a[:, s_:N, :], a, a[:, 0:N - s_, :], ALU.add)
        cur = 1 - cur
        s_ *= 2
    S.tt("dve", slot, slot[:], slot, slot[:], tot[cur], tot[cur][:], ALU.add)
    ecap = S.sb([128, NE], F32, "ecap")
    S.dma("sp", ecap[:], C.ecap_d[:, :], W=[ecap])
    val = tot[1 - cur]
    S.ts("dve", val, val[:], slot, slot[:], float(cap) - 0.5, None, ALU.is_lt)
    S.tt("dve", val, val[:], val, val[:], cmp_, cmp_[:], ALU.mult)
    S.tt("dve", slot, slot[:], slot, slot[:], ecap, ecap[:].unsqueeze(1).to_broadcast([128, N, NE]), ALU.add)
    S.ts("dve", slot, slot[:], slot, slot[:], -BIG, None, ALU.add)
    S.tt("dve", slot, slot[:], slot, slot[:], val, val[:], ALU.mult)
    S.ts("dve", slot, slot[:], slot, slot[:], BIG, None, ALU.add)
    idx = S.sb([128, N, NE], I32, "idx")
    S.copy("dve", idx, idx[:], slot, slot[:])
    rows = [S.sb([128, XW], I32, "rows") for _ in range(3)]
    breg = S.reg("pool", NE * cap - 1)
    for n in range(N):
        R = rows[n % 3]
        S.dma("sp", R[:], C.hnx_d[n * 128:(n + 1) * 128, :], W=[R])
        for e_ in range(NE):
            def fn(eng, R=R, n=n, e_=e_):
                return eng.indirect_dma_start(
                    out=C.xin_d[:, :], out_offset=bass.IndirectOffsetOnAxis(ap=idx[:, n, e_:e_ + 1], axis=0),
                    in_=R[:], in_offset=None, bounds_check=breg["r"], oob_is_err=False)
            S.dma("pool", None, None, R=[R, idx], sem_buf=R, fn=fn)
    S.end()
    S.begin()
    load_consts(S, C)
    wgq = [S.sb([128, NCH, 512], BF16, "wg%d" % q) for q in range(4)]
    wuq_ = [S.sb([128, NCH, 512], BF16, "wu%d" % q) for q in range(4)]
    wd = S.sb([128, 16, D], BF16, "wd")
    stg = [S.sb([128, 512], F32, "wstg") for _ in range(8)]
    xs = [S.sb([128, XW], I32, "xs") for _ in range(2)]
    xT = S.sb([128, NCH, cap], BF16, "xT")
    gates = S.sb([128, NT], F32, "gates")
    toks = S.sb([128, NT], I32, "toks")
    HS = min(512, cap)
    hid = S.sb([128, 16, cap], BF16, "hid")
    sg = [S.sb([128, HS], F32, "sg") for _ in range(2)]
    yo = [S.sb([128, D], F32, "yo") for _ in range(2)]
    ptp = [S.ps([128, 512], BF16, "ptp") for _ in range(2)]
    pg = [S.ps([128, 512], F32, "pg") for _ in range(2)]
    pu = [S.ps([128, 512], F32, "pu") for _ in range(2)]
    py = [S.ps([128, 512], F32, "py") for _ in range(2)]
    kkc = [0]
    breg2 = S.reg("pool", Sq - 1)
    prev_scatter = []

    def emit_wq(e2, q):
        for (wsb, wdr) in ((wgq[q], C.wg_d[l, e2]), (wuq_[q], C.wu_d[l, e2])):
            for c in range(NCH):
                st_ = stg[kkc[0] % 8]
                kkc[0] += 1
                S.dma("sp", st_[:], wdr[c * 128:(c + 1) * 128, q * 512:(q + 1) * 512], W=[st_])
                S.copy("pool", wsb, wsb[:, c, :], st_, st_[:])

    def emit_wdn(e2, c):
        for hc in range(2):
            st_ = stg[kkc[0] % 8]
            kkc[0] += 1
            S.dma("sp", st_[:], C.wd_d[l, e2][c * 128:(c + 1) * 128, hc * 512:(hc + 1) * 512], W=[st_])
            S.copy("act", wd, wd[:, c, hc * 512:(hc + 1) * 512], st_, st_[:])

    for e_ in range(NE):
        if e_ == 0:
            for q in range(4):
                emit_wq(0, q)
        for t in range(NT):
            X = xs[t % 2]
            S.dma("sp", X[:], C.xin_d[e_ * cap + t * 128:e_ * cap + (t + 1) * 128, :], W=[X])
            S.copy("dve", gates, gates[:, t:t + 1], X, X[:, 512 + e_:513 + e_].bitcast(F32))
            S.copy("dve", toks, toks[:, t:t + 1], X, X[:, 528:529])
            for c in range(NCH):
                p_ = ptp[c % 2]
                S.tr(p_, p_[:, 0:128], X, X[:, 0:512].bitcast(BF16)[:, c * 128:(c + 1) * 128], C.ident, C.ident[:])
                S.copy("act" if c % 2 == 0 else "dve", xT, xT[:, c, t * 128:(t + 1) * 128], p_, p_[:, 0:128])
        new_scatter = []
        for fb in range(16):
            q, fo = fb // 4, (fb % 4) * 128
            for s0 in range(0, cap, HS):
                g_, u_, sg_ = pg[(fb * 2 + s0 // HS) % 2], pu[(fb * 2 + s0 // HS) % 2], sg[(fb * 2 + s0 // HS) % 2]
                for c in range(NCH):
                    S.mm(g_, g_[:, 0:HS], wgq[q], wgq[q][:, c, fo:fo + 128], xT, xT[:, c, s0:s0 + HS], c == 0, c == NCH - 1)
                for c in range(NCH):
                    S.mm(u_, u_[:, 0:HS], wuq_[q], wuq_[q][:, c, fo:fo + 128], xT, xT[:, c, s0:s0 + HS], c == 0, c == NCH - 1)
                S.op("act", lambda e, sg_=sg_, g_=g_: e.activation(out=sg_[:], in_=g_[:, 0:HS], func=AF.Silu), R=[g_], W=[sg_])
                S.tt("dve", hid, hid[:, fb, s0:s0 + HS], u_, u_[:, 0:HS], sg_, sg_[:], ALU.mult)
            emit_wdn(e_, fb)
            if fb % 4 == 3 and e_ + 1 < NE:
                emit_wq(e_ + 1, fb // 4)
        if True:
            for tt_ in range(NT):
                Y = yo[tt_ % 2]
                for hf in range(2):
                    p_ = py[hf]
                    for fb in range(16):
                        S.mm(p_, p_[:], hid, hid[:, fb, tt_ * 128:(tt_ + 1) * 128], wd, wd[:, fb, hf * 512:(hf + 1) * 512], fb == 0, fb == 15)
                    S.ts("dve", Y, Y[:, hf * 512:(hf + 1) * 512], p_, p_[:], gates[:, tt_:tt_ + 1], None, ALU.mult, R=[gates])

                def fn(eng, Y=Y, tt_=tt_):
                    return eng.indirect_dma_start(
                        out=x_d[:, :], out_offset=bass.IndirectOffsetOnAxis(ap=toks[:, tt_:tt_ + 1], axis=0),
                        in_=Y[:], in_offset=None, bounds_check=breg2["r"], oob_is_err=False, compute_op=ALU.add)
                tk = S.dma("pool", None, None, R=[Y, toks], sem_buf=Y, fn=fn, extra=prev_scatter)
                new_scatter.append(tk)
        prev_scatter = new_scatter
    S.end()


def stage_FIN(S, C, x_d, out_d):
    Sq = C.S
    S.begin()
    load_consts(S, C)
    gb = S.sb([128, D], F32, "gbf")
    S.dma("sp", gb[:], C.norm_final_d[0, :].partition_broadcast(128), W=[gb])
    xt = [S.sb([128, D], F32, "xf") for _ in range(2)]
    yo = [S.sb([128, D], F32, "yf") for _ in range(2)]
    junk = S.sb([128, D], BF16, "junkf")
    st = [S.sb([128, 2], F32, "stf") for _ in range(2)]
    for t in range(Sq // 128):
        X, Y, st_ = xt[t % 2], yo[t % 2], st[t % 2]
        S.dma("sp", X[:], x_d[t * 128:(t + 1) * 128, :], W=[X])
        S.op("act", lambda e, X=X, st_=st_: e.activation(out=junk[:], in_=X[:], func=AF.Square, accum_out=st_[:, 0:1]),
             R=[X], W=[junk, st_])
        S.op("act", lambda e, st_=st_: e.activation(out=st_[:, 1:2], in_=st_[:, 0:1], func=AF.Sqrt, bias=C.epsb[:, 0:1], scale=1.0 / D),
             R=[st_, C.epsb], W=[st_])
        S.op("dve", lambda e, st_=st_: e.reciprocal(out=st_[:, 1:2], in_=st_[:, 1:2]), R=[st_], W=[st_])
        S.stt("dve", Y, Y[:], X, X[:], st_[:, 1:2], gb, gb[:], ALU.mult, ALU.mult, R=[st_])
        S.dma("act", out_d[t * 128:(t + 1) * 128, :], Y[:], R=[Y])
    S.end()


def run_stage(S, C, st):
    if st == "P0": stage_P0(S, C, C.x_d)
    elif st == "A": stage_A(S, C)
    elif st == "B": stage_B(S, C)
    elif st == "OUT0": stage_OUT(S, C, 0, C.mixT0_d, C.wout0_d, C.x_d, C.x1_d)
    elif st == "MOE0": stage_MOE(S, C, 0, C.x1_d)
    elif st == "P1": stage_P1(S, C, C.x1_d)
    elif st == "C": stage_C(S, C)
    elif st == "D": stage_D(S, C)
    elif st == "OUT1": stage_OUT(S, C, 1, C.mixT1_d, C.wout1_d, C.x1_d, C.x3_d)
    elif st == "MOE1": stage_MOE(S, C, 1, C.x3_d)
    elif st == "FIN": stage_FIN(S, C, C.x3_d, C.y_d)
    else: raise ValueError(st)


W1C = 3024
O_KR, O_KRR, O_DQ, O_DK, O_G, O_CQ, O_CKV, O_DV, O_DO = 0, 96, 192, 704, 1216, 1232, 1744, 2000, 2512


def host_prep_P1(od_w_in, w_uq, w_ukv):
    w = np.asarray(od_w_in, np.float32)
    cq, ckv, kr, dq, dk, dv, do, gt = np.split(w, np.cumsum([512, 256, 32, 512, 512, 512, 512])[:], axis=1)
    p32 = np.array([(d + 16 if d < 16 else d - 16) for d in range(32)])
    w1 = np.concatenate([cq[:, 0:64], kr, cq[:, 0:64], kr[:, p32], dq, dk, gt, cq, ckv, dv, do], axis=1)
    assert w1.shape[1] == W1C
    uq = np.asarray(w_uq, np.float32)
    perm = np.arange(768)
    for h in range(8):
        for dd in range(32):
            perm[h * 96 + 64 + dd] = h * 96 + 64 + (dd + 16 if dd < 16 else dd - 16)
    wuq = np.concatenate([uq, uq[:, perm]], axis=1)
    ukv = np.asarray(w_ukv, np.float32).reshape(256, 8, 128)
    wukv = np.concatenate([ukv[:, :, 0:64].reshape(256, 512), ukv[:, :, 64:128].reshape(256, 512)], axis=1)
    return np.ascontiguousarray(w1), np.ascontiguousarray(wuq), np.ascontiguousarray(wukv)


def ropec1():
    inv = np.zeros(128, np.float32)
    sgn = np.zeros(128, np.float32)
    fr = (10000.0 ** (-np.arange(16, dtype=np.float32) * 2.0 / 32)).astype(np.float32)
    for p in range(64, 96):
        d = p - 64
        inv[p] = fr[d % 16]
        sgn[p] = -1.0 if d < 16 else 1.0
    return np.stack([inv, sgn], axis=1).astype(np.float32)


def stage_P1(S, C, x_dram):
    Sq = C.S
    NG = Sq // GT
    S.begin()
    load_consts(S, C)
    ident = C.ident
    cst = S.sb([128, 2], F32, "cst1")
    S.dma("sp", cst[:], C.ropec1_d[:, :], W=[cst])
    gv = S.sb([128, NCH], F32, "gv1")
    S.dma("sp", gv[:], C.norm_mix_d[1, :].rearrange("(c p) -> p c", p=128), W=[gv], allow_slow_non_contiguous=True)
    gq = S.sb([128, 4], F32, "gq")
    S.dma("sp", gq[:], C.mla_nq_d[0, :].rearrange("(c p) -> p c", p=128), W=[gq], allow_slow_non_contiguous=True)
    gkv = S.sb([128, 2], F32, "gkv")
    S.dma("sp", gkv[:], C.mla_nkv_d[0, :].rearrange("(c p) -> p c", p=128), W=[gkv], allow_slow_non_contiguous=True)
    gb4 = S.sb([4, 4], F32, "gb4")
    S.dma("sp", gb4[:], C.gbias_d[:, :], W=[gb4])
    C.rt_ang = S.sb([128, GT], F32, "rt_ang")
    C.rt_ki = S.sb([128, GT], I32, "rt_ki")
    C.rt_kf = S.sb([128, GT], F32, "rt_kf")
    w = S.sb([128, NCH, W1C], BF16, "w1")
    stg = load_weights_bf16(S, C.w1_d, W1C, gv, 0, w)
    wuq = S.sb([128, 4, 1536], BF16, "wuq")
    load_weights_bf16(S, C.wuq_d, 1536, gq, 0, wuq, kchunks=4, stg=stg)
    wukv = S.sb([128, 2, 1024], BF16, "wukv")
    load_weights_bf16(S, C.wukv_d, 1024, gkv, 0, wukv, kchunks=2, stg=stg)

    xt = [S.sb([128, 4, D], F32, "xt")] * 2
    junk = S.sb([128, D], BF16, "junk")
    ssq = S.sb([128, 4], F32, "ssq")
    rstd = S.sb([128, 4], F32, "rstd")
    xn = S.sb([128, 4, D], BF16, "xn")
    xnT = [S.sb([128, NCH, GT], BF16, "xnT") for _ in range(2)]
    pT = [S.ps([128, GT], BF16, "pT") for _ in range(2)]
    pz = [S.ps([128, GT], F32, "pz") for _ in range(2)]
    pr = [S.ps([128, GT], F32, "pr") for _ in range(2)]
    pm = [S.ps([128, GT], F32, "pm") for _ in range(2)]
    posi = S.sb([128, GT], I32, "posi")
    posf = S.sb([128, GT], F32, "posf")
    tmp = S.sb([128, GT], F32, "tmp")
    cosC = S.sb([128, GT], F32, "cosC")
    sinC = S.sb([128, GT], F32, "sinC")
    t1 = [S.sb([128, GT], F32, "t1") for _ in range(2)]
    t2 = [S.sb([128, GT], F32, "t2") for _ in range(2)]
    ofm = [S.sb([128, GT], BF16, "ofm") for _ in range(3)]
    oraw = [S.sb([128, GT], F32, "oraw") for _ in range(2)]
    og4 = [S.sb([4, GT], F32, "og4") for _ in range(2)]
    ovd = [S.sb([128, 4, 129], BF16, "ovd") for _ in range(2)]
    for b in ovd:
        S.memset("pool", b, b[:], 1.0)
    ovc = [S.sb([128, 8, 128], BF16, "ovc") for _ in range(2)]
    for b in ovc:
        S.memset("pool", b, b[:], 0.0)
        S.memset("pool", b, b[:, :, 0:1], 1.0)
    ogo = [S.sb([128, 512], F32, "ogo") for _ in range(2)]
    cn4 = S.sb([128, 4, 768], BF16, "cn4")
    cnT = S.sb([128, 6, GT], BF16, "cnT")
    st2 = [S.sb([128, 4], F32, "st2") for _ in range(2)]
    eps2 = C.epsb
    kf = 0
    for g in range(NG):
        X = xnT[g % 2]
        gs = slice(g * GT, (g + 1) * GT)
        norm_transpose_group(S, C, x_dram, g, xt[0], junk, ssq, rstd, xn, pT, X, ident)
        rope_tables(S, C, C.pos_d, g, posi, posf, [(cst, cst[:, 0:1], cst[:, 1:2], [(cosC, sinC, 1.0)])], tmp, None)
        z, r = pz[kf % 2], pr[kf % 2]
        for c in range(NCH):
            S.mm(z, z[0:96, :], w, w[:, c, O_KR:O_KR + 96], X, X[:, c, :], c == 0, c == NCH - 1)
        for c in range(NCH):
            S.mm(r, r[0:96, :], w, w[:, c, O_KRR:O_KRR + 96], X, X[:, c, :], c == 0, c == NCH - 1)
        a, b2, o = t1[kf % 2], t2[kf % 2], ofm[kf % 3]
        kf += 1
        S.tt("dve", a, a[64:96, :], z, z[64:96, :], cosC, cosC[64:96, :], ALU.mult)
        S.tt("dve", b2, b2[64:96, :], r, r[64:96, :], sinC, sinC[64:96, :], ALU.mult)
        S.tt("pool", o, o[64:96, :], a, a[64:96, :], b2, b2[64:96, :], ALU.add)
        for h in range(8):
            S.dma("sp" if h % 2 == 0 else "act", C.fmk_d[h, 64:96, gs], o[64:96, :], R=[o])
        for blk in range(8):
            z = pz[kf % 2]
            orw = oraw[kf % 2]
            kf += 1
            c0 = O_DQ + blk * 128
            for c in range(NCH):
                S.mm(z, z[:], w, w[:, c, c0:c0 + 128], X, X[:, c, :], c == 0, c == NCH - 1)
            S.copy("act" if blk % 2 == 0 else "dve", orw, orw[:], z, z[:])
            S.dma("sp", C.qkraw_d[blk * 128:(blk + 1) * 128, gs], orw[:], R=[orw])
        for ty in range(4):
            z = pr[ty % 2]
            o4 = og4[ty % 2]
            c0 = O_G + ty * 4
            for c in range(NCH):
                S.mm(z, z[0:4, :], w, w[:, c, c0:c0 + 4], X, X[:, c, :], c == 0, c == NCH - 1)
            S.ts("dve", o4, o4[:], z, z[0:4, :], gb4[:, ty:ty + 1], None, ALU.add, R=[gb4])
            S.dma("sp", C.gatesT_d[ty, :, gs], o4[:], R=[o4])
        for j in range(4):
            tok0 = g * GT + j * 128
            pcq, pckv, pdv, pdo = pm[0], pm[1], pz[j % 2], pr[j % 2]
            for (pp, cc, nn) in ((pcq, O_CQ, 512), (pckv, O_CKV, 256), (pdv, O_DV, 512), (pdo, O_DO, 512)):
                for c in range(NCH):
                    S.mm(pp, pp[:, 0:nn], X, X[:, c, j * 128:(j + 1) * 128], w, w[:, c, cc:cc + nn], c == 0, c == NCH - 1)
            s2 = st2[j % 2]
            S.op("act", lambda e, s2=s2, pcq=pcq: e.activation(out=junk[:, 0:512], in_=pcq[:, 0:512], func=AF.Square, accum_out=s2[:, 0:1]),
                 R=[pcq], W=[junk, s2])
            S.op("act", lambda e, s2=s2, pckv=pckv: e.activation(out=junk[:, 0:256], in_=pckv[:, 0:256], func=AF.Square, accum_out=s2[:, 1:2]),
                 R=[pckv], W=[junk, s2])
            S.op("act", lambda e, s2=s2: e.activation(out=s2[:, 2:3], in_=s2[:, 0:1], func=AF.Sqrt, bias=eps2[:, 0:1], scale=1.0 / 512),
                 R=[s2, eps2], W=[s2])
            S.op("act", lambda e, s2=s2: e.activation(out=s2[:, 3:4], in_=s2[:, 1:2], func=AF.Sqrt, bias=eps2[:, 0:1], scale=1.0 / 256),
                 R=[s2, eps2], W=[s2])
            S.op("dve", lambda e, s2=s2: e.reciprocal(out=s2[:, 2:4], in_=s2[:, 2:4]), R=[s2], W=[s2])
            S.ts("dve", cn4, cn4[:, j, 0:512], pcq, pcq[:, 0:512], s2[:, 2:3], None, ALU.mult, R=[s2])
            S.ts("dve", cn4, cn4[:, j, 512:768], pckv, pckv[:, 0:256], s2[:, 3:4], None, ALU.mult, R=[s2])
            vd, go = ovd[j % 2], ogo[j % 2]
            S.copy("act", vd, vd[:, :, 0:128], pdv, pdv[:].rearrange("p (h d) -> p h d", h=4))
            S.op("act", lambda e, go=go, pdo=pdo: e.activation(out=go[:], in_=pdo[:], func=AF.Sigmoid), R=[pdo], W=[go])
            S.dma("pool", C.vD_d[tok0:tok0 + 128, :], vd[:].rearrange("p h d -> p (h d)"), R=[vd])
            S.dma("pool", C.og_d[tok0:tok0 + 128, :], go[:], R=[go])
        for c in range(6):
            p = pT[c % 2]
            for j in range(4):
                S.tr(p, p[:, j * 128:(j + 1) * 128], cn4, cn4[:, j, c * 128:(c + 1) * 128], ident, ident[:])
            S.copy("act" if c % 2 == 0 else "dve", cnT, cnT[:, c, :], p, p[:])
        for h in range(8):
            z, r = pz[kf % 2], pr[kf % 2]
            a, b2, o = t1[kf % 2], t2[kf % 2], ofm[kf % 3]
            kf += 1
            for c in range(4):
                S.mm(z, z[0:96, :], wuq, wuq[:, c, h * 96:(h + 1) * 96], cnT, cnT[:, c, :], c == 0, c == 3)
            for c in range(4):
                S.mm(r, r[0:96, :], wuq, wuq[:, c, 768 + h * 96:768 + (h + 1) * 96], cnT, cnT[:, c, :], c == 0, c == 3)
            S.copy("act", o, o[0:64, :], z, z[0:64, :])
            S.tt("dve", a, a[64:96, :], z, z[64:96, :], cosC, cosC[64:96, :], ALU.mult)
            S.tt("dve", b2, b2[64:96, :], r, r[64:96, :], sinC, sinC[64:96, :], ALU.mult)
            S.tt("dve", o, o[64:96, :], a, a[64:96, :], b2, b2[64:96, :], ALU.add)
            S.dma("sp", C.fmq_d[h, :, gs], o[0:96, :], R=[o])
        for h in range(8):
            z = pm[h % 2]
            o = ofm[kf % 3]
            kf += 1
            for c in range(2):
                S.mm(z, z[0:64, :], wukv, wukv[:, c, h * 64:(h + 1) * 64], cnT, cnT[:, 4 + c, :], c == 0, c == 1)
            S.copy("act" if h % 2 == 0 else "dve", o, o[0:64, :], z, z[0:64, :])
            S.dma("act", C.fmk_d[h, 0:64, gs], o[0:64, :], R=[o])
        for j in range(4):
            tok0 = g * GT + j * 128
            z = pz[j % 2]
            vc = ovc[j % 2]
            for c in range(2):
                S.mm(z, z[:], cnT, cnT[:, 4 + c, j * 128:(j + 1) * 128], wukv, wukv[:, c, 512:1024], c == 0, c == 1)
            S.copy("dve", vc, vc[:, :, 64:128], z, z[:].rearrange("p (h d) -> p h d", h=8))
            S.dma("pool", C.vC_d[tok0:tok0 + 128, :], vc[:].rearrange("p h d -> p (h d)"), R=[vc])
    S.end()


def stage_C(S, C):
    Sq = C.S
    N = Sq // 128
    NG = Sq // GT
    scale = 96 ** -0.5
    S.begin()
    load_consts(S, C)
    Qh = [S.sb([96, Sq], BF16, "Qh") for _ in range(2)]
    Kh = [S.sb([96, Sq], BF16, "Kh") for _ in range(2)]
    Vh = [S.sb([128, N, 128], BF16, "Vh") for _ in range(2)]
    sq = [S.sb([96, GT], F32, "sqc") for _ in range(2)]
    acc = S.sb([1, 2, GT], F32, "accc")
    mq = S.sb([1, 4], F32, "mqc")
    negM = [S.sb([128, 1], F32, "negMc") for _ in range(2)]
    pn = S.ps([128, GT], F32, "pnc")
    pss = [S.ps([128, 512], F32, "pssc") for _ in range(4)]
    pso = [S.ps([128, 512], F32, "psoc") for _ in range(2)]
    pts = [S.sb([128, 512], BF16, "ptc") for _ in range(4)]
    den = [S.sb([1, 512], F32, "denc") for _ in range(2)]
    rdb = [S.sb([128, 512], F32, "rdbc") for _ in range(2)]
    osb = [S.sb([128, 512], BF16, "osbc") for _ in range(2)]
    ks = 0
    it = 0
    for h in range(8):
        Q, K, V, nM = Qh[h % 2], Kh[h % 2], Vh[h % 2], negM[h % 2]
        for h0 in range(0, Sq, 2048):
            w_ = min(2048, Sq - h0)
            S.dma("sp", Q[:, h0:h0 + w_], C.fmq_d[h, :, h0:h0 + w_], W=[Q])
            S.dma("act", K[:, h0:h0 + w_], C.fmk_d[h, :, h0:h0 + w_], W=[K])
        S.dma("pool", V[:], C.vC_d[:, h * 128:(h + 1) * 128].rearrange("(n p) c -> p n c", p=128), W=[V])
        S.memset("dve", acc, acc[:], 0.0)
        i = 0
        for ai, T in enumerate((Q, K)):
            for g in range(NG):
                s_ = sq[i % 2]
                i += 1
                S.op("act", lambda e, s_=s_, T=T, g=g: e.activation(out=s_[:], in_=T[:, g * GT:(g + 1) * GT], func=AF.Square),
                     R=[T], W=[s_])
                S.mm(pn, pn[0:1, :], C.onesf, C.onesf[0:96, 0:1], s_, s_[:, :], True, True)
                S.tt("dve", acc, acc[:, ai, :], pn, pn[0:1, :], acc, acc[:, ai, :], ALU.max)
        S.op("dve", lambda e: e.tensor_reduce(out=mq[:, 0:2], in_=acc[:], axis=AX.X, op=ALU.max), R=[acc], W=[mq])
        S.tt("dve", mq, mq[:, 2:3], mq, mq[:, 0:1], mq, mq[:, 1:2], ALU.mult)
        S.op("act", lambda e: e.activation(out=mq[:, 3:4], in_=mq[:, 2:3], func=AF.Sqrt), R=[mq], W=[mq])
        S.ts("dve", mq, mq[:, 3:4], mq, mq[:, 3:4], -scale, None, ALU.mult)
        bcast_row_to_parts(S, C, mq, mq[0:1, 3:4], nM, nM[:], pn, 1)
        for g in range(NG):
            po = pso[it % 2]
            pend = []
            LAG = 2
            for m in range(N):
                ps_ = pss[ks % 4]
                pt = pts[ks % 4]
                ks += 1
                S.mm(ps_, ps_[:], K, K[:, m * 128:(m + 1) * 128], Q, Q[:, g * GT:(g + 1) * GT], True, True)
                S.op("act", lambda e, pt=pt, ps_=ps_, nM=nM: e.activation(out=pt[:], in_=ps_[:], func=AF.Exp, bias=nM[:, 0:1], scale=scale),
                     R=[ps_, nM], W=[pt])
                pend.append((m, pt))
                if len(pend) > LAG:
                    m2, pt2 = pend.pop(0)
                    S.mm(po, po[:, :], V, V[:, m2, :], pt2, pt2[:], m2 == 0, m2 == N - 1)
            for (m2, pt2) in pend:
                S.mm(po, po[:, :], V, V[:, m2, :], pt2, pt2[:], m2 == 0, m2 == N - 1)
            dn, rb, ob = den[it % 2], rdb[it % 2], osb[it % 2]
            S.op("dve", lambda e, dn=dn, po=po: e.reciprocal(out=dn[0:1, :], in_=po[0:1, :]), R=[po], W=[dn])
            pb = pss[ks % 4]
            ks += 1
            S.mm(pb, pb[:, :], C.onesf, C.onesf[0:1, :], dn, dn[0:1, :], True, True)
            S.copy("act", rb, rb[64:128, :], pb, pb[64:128, :])
            S.tt("dve", ob, ob[64:128, :], po, po[64:128, :], rb, rb[64:128, :], ALU.mult)
            S.dma("sp", C.mixT1_d[h * 64:(h + 1) * 64, g * GT:(g + 1) * GT], ob[64:128, :], R=[ob])
            it += 1
    S.end()


def _v3(ap):
    return ap.rearrange("p (n l) -> p n l", l=128)


def logstep3(S, bufs, cur, op, suffix, L=128):
    s = 1
    while s < L:
        a, b = bufs[cur], bufs[1 - cur]
        a3, b3 = _v3(a[:]), _v3(b[:])
        if not suffix:
            S.copy("pool", b, b3[:, :, 0:s], a, a3[:, :, 0:s])
            S.tt("dve", b, b3[:, :, s:L], a, a3[:, :, s:L], a, a3[:, :, 0:L - s], op)
        else:
            S.copy("pool", b, b3[:, :, L - s:L], a, a3[:, :, L - s:L])
            S.tt("dve", b, b3[:, :, 0:L - s], a, a3[:, :, 0:L - s], a, a3[:, :, s:L], op)
        cur = 1 - cur
        s *= 2
    return cur


def logstep2(S, bufs, cur, op, suffix, N):
    s = 1
    while s < N:
        a, b = bufs[cur], bufs[1 - cur]
        if not suffix:
            S.copy("pool", b, b[:, 0:s], a, a[:, 0:s])
            S.tt("dve", b, b[:, s:N], a, a[:, s:N], a, a[:, 0:N - s], op)
        else:
            S.copy("pool", b, b[:, N - s:N], a, a[:, N - s:N])
            S.tt("dve", b, b[:, 0:N - s], a, a[:, 0:N - s], a, a[:, s:N], op)
        cur = 1 - cur
        s *= 2
    return cur


def stage_D(S, C):
    Sq = C.S
    N = Sq // 128
    L = 128
    NEGB = -1.0e30
    S.begin()
    load_consts(S, C)
    cw = S.sb([128, 8, 5], F32, "cw")
    S.dma("sp", cw[:].rearrange("p b w -> p (b w)"), C.conv_d[:, :], W=[cw])
    raw = [S.sb([128, Sq], F32, "raw") for _ in range(2)]
    acc = S.sb([128, Sq], F32, "cacc")
    sil = S.sb([128, Sq], F32, "sil")
    ocv = [S.sb([128, Sq], BF16, "ocv") for _ in range(2)]
    for blk in range(8):
        R, O = raw[blk % 2], ocv[blk % 2]
        for h0 in range(0, Sq, 2048):
            w_ = min(2048, Sq - h0)
            S.dma("sp" if (h0 // 2048) % 2 == 0 else "act", R[:, h0:h0 + w_], C.qkraw_d[blk * 128:(blk + 1) * 128, h0:h0 + w_], W=[R])
        S.ts("dve", acc, acc[:], R, R[:], cw[:, blk, 2:3], None, ALU.mult, R=[cw])
        for wi in (0, 1, 3, 4):
            sh = wi - 2
            if sh > 0:
                S.stt("dve", acc, acc[:, 0:Sq - sh], R, R[:, sh:Sq], cw[:, blk, wi:wi + 1], acc, acc[:, 0:Sq - sh], ALU.mult, ALU.add, R=[cw])
            else:
                S.stt("dve", acc, acc[:, -sh:Sq], R, R[:, 0:Sq + sh], cw[:, blk, wi:wi + 1], acc, acc[:, -sh:Sq], ALU.mult, ALU.add, R=[cw])
        if blk < 4:
            S.op("act", lambda e, O=O: e.activation(out=O[:], in_=acc[:], func=AF.Silu), R=[acc], W=[O])
        else:
            S.op("act", lambda e: e.activation(out=sil[:], in_=acc[:], func=AF.Silu), R=[acc], W=[sil])
            S.ts("dve", O, O[:], sil, sil[:], 128 ** -0.5, None, ALU.mult)
        for h0 in range(0, Sq, 2048):
            w_ = min(2048, Sq - h0)
            S.dma("sp", C.qkc_d[blk, :, h0:h0 + w_], O[:, h0:h0 + w_], R=[O])
    S.end()
    S.begin()
    load_consts(S, C)
    TPG = 256
    G = Sq // TPG
    P_ = 4 * G
    J = TPG // L

    def fold(ap2):
        return ap2.rearrange("h (g t) -> (h g) t", t=TPG)

    def foldn(ap2):
        return ap2.rearrange("h (g j) -> (h g) j", j=J)

    def bc(t):
        return t[:].unsqueeze(2).to_broadcast([P_, J, L])
    gi = S.sb([P_, TPG], F32, "gi")
    gf = S.sb([P_, TPG], F32, "gf")
    bb = [S.sb([P_, TPG], F32, "bb") for _ in range(2)]
    cc = S.sb([P_, TPG], F32, "cc")
    cm = [S.sb([P_, TPG], F32, "cm") for _ in range(2)]
    aa = S.sb([P_, TPG], F32, "aa")
    mmx = S.sb([P_, TPG], F32, "mmx")
    ov = [S.sb([P_, TPG], F32, "ov") for _ in range(3)]
    g_ = S.sb([P_, J], F32, "g_")
    amax = S.sb([P_, J], F32, "amax")
    mPf = S.sb([P_, J], F32, "mPf")
    g4 = S.sb([4, N], F32, "g4")
    am4 = S.sb([4, N], F32, "am4")
    GG = [S.sb([4, N], F32, "GG") for _ in range(2)]
    PP = [S.sb([4, N], F32, "PP") for _ in range(2)]
    mN = S.sb([4, N], F32, "mN")
    mP = S.sb([4, N], F32, "mP")
    spc = [S.sb([4, N], F32, "spc") for _ in range(2)]
    kv = 0
    for dirn in (0, 1):
        suffix = (dirn == 1)
        dch = Buf(None, "dch%d" % dirn)
        dmp = Buf(None, "dmp%d" % dirn)
        S.dma("sp", gi[:], fold(C.gatesT_d[2 * dirn, :, :]), W=[gi])
        S.dma("act", gf[:], fold(C.gatesT_d[2 * dirn + 1, :, :]), W=[gf])
        S.op("act", lambda e: e.activation(out=bb[0][:], in_=gf[:], func=AF.Exp, scale=-1.0), R=[gf], W=[bb[0]])
        S.ts("dve", bb[0], bb[0][:], bb[0], bb[0][:], 1.0, None, ALU.add)
        S.op("act", lambda e: e.activation(out=bb[0][:], in_=bb[0][:], func=AF.Ln), R=[bb[0]], W=[bb[0]])
        S.ts("dve", bb[0], bb[0][:], bb[0], bb[0][:], -1.0, None, ALU.mult)
        cb = logstep3(S, bb, 0, ALU.add, suffix)
        B_ = bb[cb]
        B3 = _v3(B_[:])
        gcol = (L - 1) if not suffix else 0
        S.copy("dve", g_, g_[:], B_, B3[:, :, gcol])
        S.tt("dve", cc, cc[:], gi, gi[:], B_, B_[:], ALU.subtract)
        S.tt("dve", aa, _v3(aa[:]), cc, _v3(cc[:]), g_, bc(g_), ALU.add)
        S.op("dve", lambda e: e.tensor_reduce(out=amax[:], in_=_v3(aa[:]), axis=AX.X, op=ALU.max), R=[aa], W=[amax])
        S.dma("sp", foldn(C.chunk_d[dirn * 2 + 0, :, :]), g_[:], R=[g_], W=[dch])
        S.dma("sp", foldn(C.chunk_d[dirn * 2 + 1, :, :]), amax[:], R=[amax], W=[dch])
        wv = ov[kv % 3]
        kv += 1
        S.tt("dve", aa, _v3(aa[:]), aa, _v3(aa[:]), amax, bc(amax), ALU.subtract)
        S.op("act", lambda e, wv=wv: e.activation(out=wv[:], in_=aa[:], func=AF.Exp), R=[aa], W=[wv])
        S.dma("sp", fold(C.vec_d[dirn * 5 + 0, :, :]), wv[:], R=[wv])
        e1 = ov[kv % 3]
        kv += 1
        S.op("act", lambda e, e1=e1: e.activation(out=e1[:], in_=cc[:], func=AF.Exp), R=[cc], W=[e1])
        S.dma("sp", fold(C.vec_d[dirn * 5 + 1, :, :]), e1[:], R=[e1])
        S.copy("pool", cm[0], cm[0][:], cc, cc[:])
        ci = logstep3(S, cm, 0, ALU.max, suffix)
        if suffix:
            a3, b3 = _v3(cm[ci][:]), _v3(cm[1 - ci][:])
            S.copy("pool", cm[1 - ci], b3[:, :, 0:L - 1], cm[ci], a3[:, :, 1:L])
            S.memset("pool", cm[1 - ci], b3[:, :, L - 1:L], NEGB)
            ci = 1 - ci
        CM = cm[ci]
        S.dma("sp", g4[:], C.chunk_d[dirn * 2 + 0, :, :], R=[dch], W=[g4])
        S.dma("sp", am4[:], C.chunk_d[dirn * 2 + 1, :, :], R=[dch], W=[am4])
        S.copy("pool", GG[0], GG[0][:], g4, g4[:])
        gi_ = logstep2(S, GG, 0, ALU.add, suffix, N)
        G_ = GG[gi_]
        S.tt("dve", PP[0], PP[0][:], am4, am4[:], G_, G_[:], ALU.subtract)
        pi_ = logstep2(S, PP, 0, ALU.max, suffix, N)
        S.ts("dve", mN, mN[:], PP[pi_], PP[pi_][:], 0.0, None, ALU.max)
        S.tt("dve", mN, mN[:], mN, mN[:], G_, G_[:], ALU.add)
        S.memset("pool", mP, mP[:], 0.0)
        if not suffix:
            S.copy("pool", mP, mP[:, 1:N], mN, mN[:, 0:N - 1])
        else:
            S.copy("pool", mP, mP[:, 0:N - 1], mN, mN[:, 1:N])
        S.tt("dve", spc[0], spc[0][:], g4, g4[:], mP, mP[:], ALU.add)
        S.tt("dve", spc[0], spc[0][:], spc[0], spc[0][:], mN, mN[:], ALU.subtract)
        S.op("act", lambda e: e.activation(out=spc[0][:], in_=spc[0][:], func=AF.Exp), R=[spc[0]], W=[spc[0]])
        S.tt("dve", spc[1], spc[1][:], am4, am4[:], mN, mN[:], ALU.subtract)
        S.op("act", lambda e: e.activation(out=spc[1][:], in_=spc[1][:], func=AF.Exp), R=[spc[1]], W=[spc[1]])
        S.dma("sp", C.vec2_d[dirn * 2 + 0, :, :], spc[0][:], R=[spc[0]])
        S.dma("sp", C.vec2_d[dirn * 2 + 1, :, :], spc[1][:], R=[spc[1]])
        S.dma("sp", C.mp_d[dirn, :, :], mP[:], R=[mP], W=[dmp])
        S.dma("sp", mPf[:], foldn(C.mp_d[dirn, :, :]), R=[dmp], W=[mPf])
        S.tt("dve", mmx, _v3(mmx[:]), CM, _v3(CM[:]), mPf, bc(mPf), ALU.max)
        e2 = ov[kv % 3]
        kv += 1
        S.op("act", lambda e, e2=e2: e.activation(out=e2[:], in_=mmx[:], func=AF.Exp, scale=-1.0), R=[mmx], W=[e2])
        S.dma("sp", fold(C.vec_d[dirn * 5 + 2, :, :]), e2[:], R=[e2])
        iw = ov[kv % 3]
        kv += 1
        S.tt("dve", iw, _v3(iw[:]), mmx, _v3(mmx[:]), mPf, bc(mPf), ALU.subtract)
        S.op("act", lambda e, iw=iw: e.activation(out=iw[:], in_=iw[:], func=AF.Exp, scale=-1.0), R=[iw], W=[iw])
        S.dma("sp", fold(C.vec_d[dirn * 5 + 3, :, :]), iw[:], R=[iw])
        em = ov[kv % 3]
        kv += 1
        S.tt("dve", em, em[:], mmx, mmx[:], B_, B_[:], ALU.add)
        S.op("act", lambda e, em=em: e.activation(out=em[:], in_=em[:], func=AF.Exp, scale=-1.0), R=[em], W=[em])
        S.dma("sp", fold(C.vec_d[dirn * 5 + 4, :, :]), em[:], R=[em])
    S.end()
    S.begin()
    load_consts(S, C)
    identf = S.sb([128, 128], F32, "identfD")
    S.dma("sp", identf[:], C.identf_d[:, :], W=[identf])
    mk2 = S.sb([128, 2, 128], F32, "mk2")
    S.dma("sp", mk2[:], C.maskD_d[:, :, :], W=[mk2])
    VW = min(2048, Sq)
    VT = [S.sb([40, VW], F32, "VT") for _ in range(2)]
    tv = S.sb([128, N, 40], F32, "tv")
    pu = [S.ps([128, 129], F32, "pud") for _ in range(2)]
    for h0 in range(0, Sq, VW):
        V_ = VT[(h0 // VW) % 2]
        S.dma("sp", V_[:], C.vec_d.rearrange("a h s -> (a h) s")[:, h0:h0 + VW], W=[V_])
        for nn in range(VW // 128):
            n = h0 // 128 + nn
            p_ = pu[n % 2]
            S.tr(p_, p_[:, 0:40], V_, V_[:, nn * 128:(nn + 1) * 128], identf, identf[0:40, 0:40])
            S.copy("act" if n % 2 == 0 else "dve", tv, tv[:, n, :], p_, p_[:, 0:40])
    spsc = S.sb([128, 16 * N], F32, "spsc")
    S.dma("sp", spsc[:], C.vec2_d.rearrange("a h n -> (a h n)").partition_broadcast(128), W=[spsc])

    def vcol(dirn, k, h):
        return (dirn * 5 + k) * 4 + h

    qT = S.sb([128, Sq], BF16, "qTd")
    kT = S.sb([128, Sq], BF16, "kTd")
    Kt = S.sb([128, N, 128], BF16, "Ktd")
    Va = S.sb([128, N, 129], BF16, "Vad")
    Og4 = [S.sb([128, 4, 128], F32, "Ogd") for _ in range(2)]
    Cst = S.sb([128, 129], F32, "Cst")
    Cp = [S.sb([128, N, 129], BF16, "Cp%d" % d) for d in range(2)]
    pk = [S.ps([128, 128], BF16, "pkd") for _ in range(2)]
    pS = S.ps([128, 128], F32, "pSd")
    pA = [S.ps([128, 2, 129], F32, "pAd") for _ in range(2)]
    vw = [S.sb([128, 129], BF16, "vw") for _ in range(2)]
    tU = [S.sb([128, 129], F32, "tU") for _ in range(2)]
    sF = [S.sb([128, 128], BF16, "sF") for _ in range(2)]
    sB = [S.sb([128, 128], BF16, "sB") for _ in range(2)]
    vF = [S.sb([128, 129], BF16, "vF") for _ in range(2)]
    vB = [S.sb([128, 129], BF16, "vB") for _ in range(2)]
    t1 = [S.sb([128, 129], F32, "t1d") for _ in range(2)]
    tot = [S.sb([128, 129], F32, "totd") for _ in range(2)]
    dn = [S.sb([128, 2], F32, "dnd") for _ in range(2)]
    hacc = [S.sb([128, 128], F32, "hacc") for _ in range(2)]
    ob = [S.sb([128, 128], BF16, "obd") for _ in range(2)]
    oT = [S.sb([128, 512], BF16, "oTd") for _ in range(2)]
    for h in range(4):
        for h0 in range(0, Sq, 2048):
            w_ = min(2048, Sq - h0)
            S.dma("sp", qT[:, h0:h0 + w_], C.qkc_d[h, :, h0:h0 + w_], W=[qT])
            S.dma("act", kT[:, h0:h0 + w_], C.qkc_d[4 + h, :, h0:h0 + w_], W=[kT])
        S.dma("pool", Va[:], C.vD_d[:, h * 129:(h + 1) * 129].rearrange("(n p) c -> p n c", p=128), W=[Va])
        for n in range(N):
            p_ = pk[n % 2]
            S.tr(p_, p_[:], kT, kT[:, n * 128:(n + 1) * 128], C.ident, C.ident[:])
            S.copy("act" if n % 2 == 0 else "dve", Kt, Kt[:, n, :], p_, p_[:])
        for dirn in (0, 1):
            S.memset("dve", Cst, Cst[:], 0.0)
            order = range(N) if dirn == 0 else range(N - 1, -1, -1)
            for n in order:
                vw_, pu_, tU_ = vw[n % 2], pu[n % 2], tU[n % 2]
                cw_ = vcol(dirn, 0, h)
                S.op("act", lambda e, vw_=vw_, n=n, cw_=cw_: e.activation(out=vw_[:], in_=Va[:, n, :], func=AF.Copy, scale=tv[:, n, cw_:cw_ + 1]),
                     R=[Va, tv], W=[vw_])
                S.mm(pu_, pu_[:], Kt, Kt[:, n, :], vw_, vw_[:], True, True)
                S.copy("pool", Cp[dirn], Cp[dirn][:, n, :], Cst, Cst[:])
                isp = ((dirn * 2 + 0) * 4 + h) * N + n
                isc = ((dirn * 2 + 1) * 4 + h) * N + n
                S.ts("dve", tU_, tU_[:], pu_, pu_[:], spsc[:, isc:isc + 1], None, ALU.mult, R=[spsc])
                S.stt("dve", Cst, Cst[:], Cst, Cst[:], spsc[:, isp:isp + 1], tU_, tU_[:], ALU.mult, ALU.add, R=[spsc])
        for n in range(N):
            tok = slice(n * 128, (n + 1) * 128)
            i2 = n % 2
            Og = Og4[(n // 4) % 2]
            if n % 4 == 0:
                S.dma("pool", Og[:], C.og_d[n * 128:(n + 4) * 128, h * 128:(h + 1) * 128].rearrange("(n p) c -> p n c", p=128), W=[Og])
            S.mm(pS, pS[:], kT, kT[:, tok], qT, qT[:, tok], True, True)
            S.tt("dve", sF[i2], sF[i2][:], pS, pS[:], mk2, mk2[:, 0, :], ALU.mult)
            S.tt("dve", sB[i2], sB[i2][:], pS, pS[:], mk2, mk2[:, 1, :], ALU.mult)
            ha = hacc[i2]
            for dirn in (0, 1):
                s_ = (sF if dirn == 0 else sB)[i2]
                v_ = (vF if dirn == 0 else vB)[i2]
                pa = pA[dirn]
                c1, c2, c3, c4 = vcol(dirn, 1, h), vcol(dirn, 2, h), vcol(dirn, 3, h), vcol(dirn, 4, h)
                S.op("act", lambda e, v_=v_, n=n, c1=c1: e.activation(out=v_[:], in_=Va[:, n, :], func=AF.Copy, scale=tv[:, n, c1:c1 + 1]),
                     R=[Va, tv], W=[v_])
                S.mm(pa, pa[:, 0, :], s_, s_[:], v_, v_[:], True, True)
                S.mm(pa, pa[:, 1, :], qT, qT[:, tok], Cp[dirn], Cp[dirn][:, n, :], True, True)
                t_, T_, d_ = t1[dirn], tot[dirn], dn[dirn]
                S.ts("dve", t_, t_[:], pa, pa[:, 1, :], tv[:, n, c3:c3 + 1], None, ALU.mult, R=[tv])
                S.stt("dve", T_, T_[:], pa, pa[:, 0, :], tv[:, n, c2:c2 + 1], t_, t_[:], ALU.mult, ALU.add, R=[tv])
                S.stt("dve", d_, d_[:, 0:1], T_, T_[:, 128:129], -1.0, T_, T_[:, 128:129], ALU.mult, ALU.max)
                S.tt("dve", d_, d_[:, 0:1], d_, d_[:, 0:1], tv, tv[:, n, c4:c4 + 1], ALU.max)
                S.op("dve", lambda e, d_=d_: e.reciprocal(out=d_[:, 1:2], in_=d_[:, 0:1]), R=[d_], W=[d_])
                if dirn == 0:
                    S.ts("dve", ha, ha[:], T_, T_[:, 0:128], d_[:, 1:2], None, ALU.mult, R=[d_])
                else:
                    S.stt("dve", ha, ha[:], T_, T_[:, 0:128], d_[:, 1:2], ha, ha[:], ALU.mult, ALU.add, R=[d_])
            o_ = ob[i2]
            S.tt("pool", o_, o_[:], ha, ha[:], Og, Og[:, n % 4, :], ALU.mult)
            pt_ = pk[i2]
            S.tr(pt_, pt_[:], o_, o_[:], C.ident, C.ident[:])
            oT_ = oT[(n // 4) % 2]
            S.copy("act", oT_, oT_[:, (n % 4) * 128:(n % 4 + 1) * 128], pt_, pt_[:])
            if n % 4 == 3:
                S.dma("sp", C.mixT1_d[512 + h * 128:512 + (h + 1) * 128, (n - 3) * 128:(n + 1) * 128], oT_[:], R=[oT_])
    S.end()


STAGES = ("P0", "A", "B", "OUT0", "MOE0", "P1", "C", "D", "OUT1", "MOE1", "FIN")


def build_program(Sq):
    nc = bass.Bass("TRN2", target_bir_lowering=False)
    C = Ctx()
    declare(nc, C, Sq, debug=False)
    S = Sched(nc)
    for st in STAGES:
        run_stage(S, C, st)
    S.close()
    return nc


def kernel(**inputs):
    from concourse.bass_utils import run_bass_kernel_spmd
    inp = {k: np.asarray(v) for k, v in inputs.items()}
    B, Sq, _ = inp["x"].shape
    nc = build_program(Sq)
    shared = host_inputs(inp, 0)
    in_maps = []
    for b in range(B):
        d = dict(shared)
        d["x"] = np.ascontiguousarray(inp["x"][b], dtype=np.float32)
        d["pos"] = np.ascontiguousarray(inp["positions"][b]).astype(np.int32)
        in_maps.append(d)
    res = run_bass_kernel_spmd(nc, in_maps, core_ids=list(range(B)))
    return np.stack([np.asarray(r["y"], dtype=np.float32) for r in res.results], axis=0)
```
